# Optimizing a Trainium2 kernel written in Bass

```python
import jax, jax.numpy as jnp
from jax import lax
import numpy as np

D_MODEL = 2048
BATCH = 1
SEQ = 16384
DEPTH = 1

EPS = 1e-6
GMLP_WIDTH = 1024
GMLP_GROUPS = 8
GMLP_GROUP_DIM = GMLP_WIDTH // GMLP_GROUPS
GMLP_CHUNK = 128
HGRN_HEADS = 8
HGRN_KEY_DIM = 128
HGRN_VAL_DIM = 128
HGRN_QK_WIDTH = HGRN_HEADS * HGRN_KEY_DIM
HGRN_V_WIDTH = HGRN_HEADS * HGRN_VAL_DIM
HGRN_CHUNK = 16
IN_SIZES = (GMLP_WIDTH, GMLP_WIDTH, HGRN_QK_WIDTH, HGRN_QK_WIDTH, HGRN_V_WIDTH, HGRN_V_WIDTH, D_MODEL, D_MODEL)
IN_SPLITS = tuple(int(s) for s in np.cumsum(IN_SIZES)[:-1])
D_IN_PROJ = sum(IN_SIZES)
N_GROUPS = 8
EXPERTS_PER_GROUP = 8
N_EXPERTS = N_GROUPS * EXPERTS_PER_GROUP
EXPERT_TOP_K = 2
D_EXPERT = 1024
MOE_BLOCK = 128

kernel_name = "hybrid_gmlp_hgrn2_hiermoe_block"


def rms_norm(x, g):
    xf = x.astype(jnp.float32)
    y = xf * lax.rsqrt(jnp.mean(xf * xf, axis=-1, keepdims=True) + EPS)
    return (y * g.astype(jnp.float32)).astype(x.dtype)


def layer_norm(x, g, b):
    xf = x.astype(jnp.float32)
    mu = jnp.mean(xf, axis=-1, keepdims=True)
    var = jnp.mean(jnp.square(xf - mu), axis=-1, keepdims=True)
    y = (xf - mu) * lax.rsqrt(var + EPS)
    return (y * g.astype(jnp.float32) + b.astype(jnp.float32)).astype(x.dtype)


def gmlp_branch(u, v, ln_g, ln_b, w_s, b_s):
    bn, s, _ = u.shape
    nc = s // GMLP_CHUNK
    u = jax.nn.gelu(u)
    v = layer_norm(jax.nn.gelu(v), ln_g, ln_b)
    v = v.reshape(bn, nc, GMLP_CHUNK, GMLP_GROUPS, GMLP_GROUP_DIM)
    causal = jnp.tril(jnp.ones((GMLP_CHUNK, GMLP_CHUNK), dtype=bool))
    w = jnp.where(causal[None], w_s, jnp.zeros((), w_s.dtype))
    sv = jnp.einsum('gts,bnsgd->bntgd', w, v) + b_s.T[None, None, :, :, None]
    return u * sv.reshape(bn, s, GMLP_WIDTH)


def hgrn2_branch(q, f_logit, inp, g, lb, norm_g):
    bn, s, _ = q.shape
    H, K, V, C = HGRN_HEADS, HGRN_KEY_DIM, HGRN_VAL_DIM, HGRN_CHUNK
    nc = s // C
    f = lb + (1.0 - lb) * jax.nn.sigmoid(f_logit.astype(jnp.float32))
    log_f = jnp.log(f)
    k = 1.0 - f

    def chunks(t, d):
        return t.reshape(bn, nc, C, H, d).transpose(1, 0, 3, 2, 4)

    qc = chunks(q.astype(jnp.float32), K)
    kc = chunks(k, K)
    ic = chunks(inp.astype(jnp.float32), V)
    a_cum = jnp.cumsum(chunks(log_f, K), axis=3)
    a_last = a_cum[:, :, :, -1, :]

    q_dec = qc * jnp.exp(a_cum)
    k_inv = kc * jnp.exp(-a_cum)
    causal = jnp.tril(jnp.ones((C, C), dtype=bool))
    scores = jnp.einsum('nbhtk,nbhsk->nbhts', q_dec, k_inv)
    scores = jnp.where(causal, scores, 0.0)
    o_intra = jnp.einsum('nbhts,nbhsv->nbhtv', scores, ic)

    k_dec = kc * jnp.exp(a_last[:, :, :, None, :] - a_cum)

    def step(state, xs):
        q_c, k_c, i_c, al = xs
        o = jnp.einsum('bhtk,bhkv->bhtv', q_c, state)
        state = jnp.exp(al)[..., None] * state + jnp.einsum('bhsk,bhsv->bhkv', k_c, i_c)
        return state, o

    s0 = jnp.zeros((bn, H, K, V), jnp.float32)
    _, o_inter = lax.scan(step, s0, (q_dec, k_dec, ic, a_last))
    o = (o_intra + o_inter).transpose(1, 0, 3, 2, 4).reshape(bn, s, H, V)
    o = rms_norm(o, norm_g.reshape(H, V)).reshape(bn, s, H * V)
    return (o * jax.nn.silu(g.astype(jnp.float32))).astype(g.dtype)


def hier_moe(h, w_rg, b_rg, w_re, b_re, w_gate, w_up, w_down):
    bn, s, d = h.shape
    T = bn * s
    ht = h.reshape(T, d)
    g_logits = (ht @ w_rg).astype(jnp.float32) + b_rg.astype(jnp.float32)
    p_group = jax.nn.softmax(g_logits, axis=-1)
    g_sel = jnp.argmax(g_logits, axis=-1).astype(jnp.int32)
    p_g = jnp.take_along_axis(p_group, g_sel[:, None], axis=-1)
    e_logits = ((ht @ w_re).astype(jnp.float32) + b_re.astype(jnp.float32)).reshape(T, N_GROUPS, EXPERTS_PER_GROUP)
    e_in = jnp.take_along_axis(e_logits, g_sel[:, None, None], axis=1)[:, 0]
    top_v, top_i = lax.top_k(e_in, EXPERT_TOP_K)
    gate = jax.nn.softmax(top_v, axis=-1) * p_g

    n_assign = T * EXPERT_TOP_K
    eid = (g_sel[:, None] * EXPERTS_PER_GROUP + top_i.astype(jnp.int32)).reshape(-1)
    tok = jnp.repeat(jnp.arange(T, dtype=jnp.int32), EXPERT_TOP_K)
    wt = gate.reshape(-1)
    order = jnp.argsort(eid)
    e_sorted, tok_sorted, w_sorted = eid[order], tok[order], wt[order]

    counts = jax.ops.segment_sum(jnp.ones_like(eid), eid, num_segments=N_EXPERTS)
    start = jnp.cumsum(counts) - counts
    padded = ((counts + MOE_BLOCK - 1) // MOE_BLOCK) * MOE_BLOCK
    pad_start = jnp.cumsum(padded) - padded
    pad_end = pad_start + padded
    dest = pad_start[e_sorted] + (jnp.arange(n_assign, dtype=jnp.int32) - start[e_sorted])

    n_blocks = (n_assign + N_EXPERTS * (MOE_BLOCK - 1) + MOE_BLOCK - 1) // MOE_BLOCK
    P = n_blocks * MOE_BLOCK
    tok_buf = jnp.full((P,), T, dtype=jnp.int32).at[dest].set(tok_sorted)
    w_buf = jnp.zeros((P,), jnp.float32).at[dest].set(w_sorted)
    block_start = jnp.arange(n_blocks, dtype=jnp.int32) * MOE_BLOCK
    block_e = jnp.minimum(jnp.sum(pad_end[None, :] <= block_start[:, None], axis=1), N_EXPERTS - 1)

    h_pad = jnp.concatenate([ht, jnp.zeros((1, d), ht.dtype)], axis=0)
    xb = h_pad[tok_buf].reshape(n_blocks, MOE_BLOCK, d)

    def expert_block(args):
        xblk, e = args
        a = xblk @ w_gate[e]
        b = xblk @ w_up[e]
        return (jax.nn.silu(a) * b) @ w_down[e]

    yb = lax.map(expert_block, (xb, block_e)).reshape(P, d)
    y = jax.ops.segment_sum(yb * w_buf[:, None].astype(yb.dtype), tok_buf, num_segments=T + 1)[:T]
    return y.reshape(bn, s, d).astype(h.dtype)


def setup_inputs(seed: int = 0) -> dict:
    key = jax.random.key(seed)
    ks = jax.random.split(key, 24)
    f32 = jnp.float32
    L = DEPTH

    def nrm(k, shape, scale):
        return (jax.random.normal(k, shape, f32) * scale).astype(f32)

    return {
        "x": nrm(ks[0], (BATCH, SEQ, D_MODEL), 1.0),
        "norm_mix_g": 1.0 + nrm(ks[1], (L, D_MODEL), 0.02),
        "w_in": nrm(ks[2], (L, D_MODEL, D_IN_PROJ), D_MODEL ** -0.5),
        "gmlp_ln_g": 1.0 + nrm(ks[3], (L, GMLP_WIDTH), 0.02),
        "gmlp_ln_b": nrm(ks[4], (L, GMLP_WIDTH), 0.02),
        "w_spatial": nrm(ks[5], (L, GMLP_GROUPS, GMLP_CHUNK, GMLP_CHUNK), GMLP_CHUNK ** -0.5),
        "b_spatial": 1.0 + nrm(ks[6], (L, GMLP_GROUPS, GMLP_CHUNK), 0.1),
        "hgrn_lb_logits": nrm(ks[7], (L + 1, HGRN_QK_WIDTH), 0.5),
        "hgrn_norm_g": 1.0 + nrm(ks[8], (L, HGRN_V_WIDTH), 0.02),
        "w_branch_a": nrm(ks[9], (L, GMLP_WIDTH, D_MODEL), GMLP_WIDTH ** -0.5),
        "w_branch_b": nrm(ks[10], (L, HGRN_V_WIDTH, D_MODEL), HGRN_V_WIDTH ** -0.5),
        "w_out": nrm(ks[11], (L, D_MODEL, D_MODEL), D_MODEL ** -0.5),
        "norm_ffn_g": 1.0 + nrm(ks[12], (L, D_MODEL), 0.02),
        "w_router_group": nrm(ks[13], (L, D_MODEL, N_GROUPS), D_MODEL ** -0.5),
        "b_router_group": nrm(ks[14], (L, N_GROUPS), 0.01),
        "w_router_expert": nrm(ks[15], (L, D_MODEL, N_EXPERTS), D_MODEL ** -0.5),
        "b_router_expert": nrm(ks[16], (L, N_EXPERTS), 0.01),
        "w_expert_gate": nrm(ks[17], (L, N_EXPERTS, D_MODEL, D_EXPERT), D_MODEL ** -0.5),
        "w_expert_up": nrm(ks[18], (L, N_EXPERTS, D_MODEL, D_EXPERT), D_MODEL ** -0.5),
        "w_expert_down": nrm(ks[19], (L, N_EXPERTS, D_EXPERT, D_MODEL), D_EXPERT ** -0.5),
        "norm_final_g": 1.0 + nrm(ks[20], (D_MODEL,), 0.02),
    }


def reference(x, norm_mix_g, w_in, gmlp_ln_g, gmlp_ln_b, w_spatial, b_spatial, hgrn_lb_logits,
              hgrn_norm_g, w_branch_a, w_branch_b, w_out, norm_ffn_g, w_router_group, b_router_group,
              w_router_expert, b_router_expert, w_expert_gate, w_expert_up, w_expert_down, norm_final_g):
    lb_table = jnp.cumsum(jax.nn.softmax(hgrn_lb_logits.astype(jnp.float32), axis=0), axis=0)
    for l in range(DEPTH):
        h = rms_norm(x, norm_mix_g[l])
        z = h @ w_in[l]
        u, v, q, f_logit, i_in, g_out, gate_a, gate_b = jnp.split(z, IN_SPLITS, axis=-1)
        y_a = gmlp_branch(u, v, gmlp_ln_g[l], gmlp_ln_b[l], w_spatial[l], b_spatial[l])
        y_b = hgrn2_branch(q, f_logit, i_in, g_out, lb_table[l], hgrn_norm_g[l])
        merged = (jax.nn.sigmoid(gate_a) * (y_a @ w_branch_a[l])
                  + jax.nn.sigmoid(gate_b) * (y_b @ w_branch_b[l]))
        x = x + (merged @ w_out[l]).astype(x.dtype)
        h = rms_norm(x, norm_ffn_g[l])
        x = x + hier_moe(h, w_router_group[l], b_router_group[l], w_router_expert[l], b_router_expert[l],
                         w_expert_gate[l], w_expert_up[l], w_expert_down[l])
    return rms_norm(x, norm_final_g)
```

```python
import numpy as np
from contextlib import ExitStack
import concourse.bass as bass
import concourse.mybir as mybir
from concourse.bass_utils import run_bass_kernel_spmd

F32 = mybir.dt.float32
BF16 = mybir.dt.bfloat16
AF = mybir.ActivationFunctionType
ALU = mybir.AluOpType
AX = mybir.AxisListType

D = 2048
KC = 16
EPS = 1e-6
ENGS = ("pe", "act", "dve", "pool", "sp")


class Res:
    __slots__ = ("lw", "rd", "name", "excl")

    def __init__(self, name="", excl=False):
        self.lw = None
        self.rd = []
        self.name = name
        self.excl = excl


class Op:
    __slots__ = ("eng", "fn", "waits", "dma", "needed", "val")

    def __init__(self, eng, fn, waits, dma):
        self.eng = eng
        self.fn = fn
        self.waits = waits
        self.dma = dma
        self.needed = False
        self.val = 0


class Prog:
    def __init__(self):
        self.q = {e: [] for e in ENGS}
        self.waited = {e: {} for e in ENGS}
        self.fence = {e: [] for e in ENGS}
        self.dma_cnt = {}
        self.last_dma = {}
        self.last_c = {}

    def op(self, eng, fn, reads=(), writes=(), dma=None, nofence=False, extra=()):
        deps = list(extra)
        ex = [r for r in reads if r.excl]
        if ex:
            reads = [r for r in reads if not r.excl]
            writes = list(writes) + ex
        for r in reads:
            if r.lw is not None:
                deps.append(r.lw)
        for w in writes:
            if w.lw is not None:
                deps.append(w.lw)
            deps.extend(w.rd)
        if not nofence and self.fence[eng]:
            deps.extend(self.fence[eng])
            self.fence[eng] = []
        deps.sort(key=lambda d: -d[2])
        waits = []
        wd = self.waited[eng]
        for d in deps:
            if d[0] == "c":
                _, pe_, pidx = d
                if pe_ == "pe" and eng == "pe":
                    continue
                key = ("c", pe_)
                if wd.get(key, -1) >= pidx:
                    continue
                wd[key] = pidx
                self.q[pe_][pidx].needed = True
                waits.append(d)
            else:
                _, stream, val = d
                key = ("d", stream)
                if wd.get(key, 0) >= val:
                    continue
                wd[key] = val
                waits.append(d)
        idx = len(self.q[eng])
        o = Op(eng, fn, waits, dma)
        if dma is not None:
            cnt = self.dma_cnt.get(dma, 0) + 1
            self.dma_cnt[dma] = cnt
            ev = ("d", dma, cnt * 16)
            self.last_dma[dma] = ev
        else:
            ev = ("c", eng, idx)
            self.last_c[eng] = ev
        self.q[eng].append(o)
        for r in reads:
            r.rd.append(ev)
        for w in writes:
            w.lw = ev
            w.rd = []
        return ev

    def barrier(self):
        evs = list(self.last_c.values()) + list(self.last_dma.values())
        for e in ENGS:
            self.fence[e] = list(evs)

    def emit(self, nc, es):
        for e in ENGS:
            c = 0
            for o in self.q[e]:
                if o.dma is None and o.needed:
                    c += 1
                    o.val = c
        sem_c = {e: es.enter_context(nc.semaphore("c_" + e)) for e in ENGS}
        sem_d = {s: es.enter_context(nc.semaphore("d_" + s)) for s in self.dma_cnt}
        q = self.q

        def run(name, e):
            for o in q[name]:
                for d in o.waits:
                    if d[0] == "c":
                        e.wait_ge(sem_c[d[1]], q[d[1]][d[2]].val)
                    else:
                        e.wait_ge(sem_d[d[1]], d[2])
                if o.fn is None:
                    continue
                ins = o.fn(e)
                if o.dma is not None:
                    ins.then_inc(sem_d[o.dma], 16)
                elif o.needed:
                    ins.then_inc(sem_c[name], 1)

        with nc.Block() as block:
            @block.tensor
            def _(e):
                run("pe", e)

            @block.scalar
            def _(e):
                run("act", e)

            @block.vector
            def _(e):
                run("dve", e)

            @block.gpsimd
            def _(e):
                run("pool", e)

            @block.sync
            def _(e):
                run("sp", e)


def bc(ap, shape):
    return ap.broadcast_to(list(shape))


def build(cfg):
    TPC = cfg["tpc"]
    NH = TPC // 1024
    NPH = cfg["nph"]
    NG, EPG, DE = cfg["ng"], cfg["epg"], cfg["de"]
    NE = NG * EPG
    NR = NG + NE
    FFC = DE // 128
    NFB = DE // 256

    nc = bass.Bass("TRN2", target_bir_lowering=False)

    def din(name, shape, dt=F32):
        return nc.dram_tensor(name, list(shape), dt, kind="ExternalInput").ap()

    x_d = din("x", [TPC, D])
    xp_d = din("xprev", [max(NPH, 1) * 1024, D])
    g1_d = din("norm_mix_g", [D])
    win_d = din("w_in", [D, 10240])
    lng_d = din("gmlp_ln_g", [1024])
    lnb_d = din("gmlp_ln_b", [1024])
    wsp_d = din("w_spatial", [8, 128, 128])
    bsp_d = din("b_spatial", [1, 1024])
    lbl_d = din("hgrn_lb", [2, 1024])
    hng_d = din("hgrn_norm_g", [1024])
    wpa_d = din("w_branch_a", [1024, D])
    wpb_d = din("w_branch_b", [1024, D])
    wout_d = din("w_out", [D, D])
    g2_d = din("norm_ffn_g", [D])
    wr_d = din("w_router", [D, NR])
    br_d = din("b_router", [NR])
    weg_d = din("w_eg", [NE * D, DE])
    weu_d = din("w_eu", [NE * D, DE])
    wed_d = din("w_ed", [NE * DE, D])
    gf_d = din("norm_final_g", [D])
    cst_d = din("consts", [128, 1408])
    out_d = nc.dram_tensor("out", [TPC, D], F32, kind="ExternalOutput").ap()
    DBG = cfg.get("dbg", False)
    STOP = cfg.get("stop", 99)
    if DBG:
        dout = lambda n, sh, dt: nc.dram_tensor(n, list(sh), dt, kind="ExternalOutput").ap()
        dbg_h = dout("dbg_h", [128, 16384], BF16)
        dbg_ya = dout("dbg_ya", [128, 8192], BF16)
        dbg_yb = dout("dbg_yb", [128, 8192], BF16)
        dbg_m = dout("dbg_m", [128, 16384], BF16)
        dbg_x2 = dout("dbg_x2", [1024, D], F32)
        dbg_G = dout("dbg_G", [128, 8 * NE], F32)
        dbg_S = dout("dbg_S", [128, 1024], F32)

    P = Prog()
    es = ExitStack()
    with es:
        ARENA_BYTES = 211968
        arena = es.enter_context(nc.sbuf_tensor("arena", [128, ARENA_BYTES // 4], F32))
        arena_bf = arena.bitcast(BF16)
        banks = [es.enter_context(nc.psum_tensor("bank%d" % i, [128, 512], F32)) for i in range(8)]
        banks_bf = [b.bitcast(BF16) for b in banks]
        bankR = [Res("bank%d" % i, excl=True) for i in range(8)]

        def view(off, shape, dt):
            n = 1
            for s in shape[1:]:
                n *= s
            if dt == F32:
                assert off % 4 == 0
                ap = arena[:, off // 4: off // 4 + n]
            else:
                assert off % 2 == 0
                ap = arena_bf[:, off // 2: off // 2 + n]
            if shape[0] != 128:
                ap = ap[0:shape[0], :]
            if len(shape) == 3:
                ap = ap.rearrange("p (a b) -> p a b", a=shape[1])
            elif len(shape) == 4:
                ap = ap.rearrange("p (a b c) -> p a b c", a=shape[1], b=shape[2])
            return ap

        cur = [0]

        def alloc(shape, dt, nbytes=None):
            n = 1
            for s in shape[1:]:
                n *= s
            sz = n * (4 if dt == F32 else 2)
            sz = (sz + 63) // 64 * 64
            off = cur[0]
            cur[0] += sz if nbytes is None else nbytes
            return view(off, shape, dt), off

        cst, _ = alloc([128, 1408], F32)
        ident_f = cst[:, 0:128]
        LT = cst[:, 128:256]
        UT = cst[:, 256:384]
        rmask = cst[:, 384:1408]
        identb, _ = alloc([128, 128], BF16)
        onesb, _ = alloc([128, 128], BF16)
        g1T, _ = alloc([128, 16], F32)
        g2T, _ = alloc([128, 16], F32)
        ngT, _ = alloc([128, 8], F32)
        lbT, _ = alloc([128, 8], F32)
        omlT, _ = alloc([128, 8], F32)
        brt, _ = alloc([128, NR], F32)
        bshi, _ = alloc([1, 1024], BF16)
        bslo, _ = alloc([1, 1024], BF16)
        wsT, _ = alloc([128, 8, 128], BF16)
        wrhi, _ = alloc([128, 16, NR], BF16)
        wrlo, _ = alloc([128, 16, NR], BF16)
        S_f, _ = alloc([128, 8, 128], F32)
        S_b, _ = alloc([128, 8, 128], BF16)
        Aj, _ = alloc([128, 8, 8], F32)
        stat, _ = alloc([128, 64], F32)
        SPARE_SZ = 12288
        _, SPARE = alloc([128, SPARE_SZ // 4], F32)
        NST, NRB = 2, 6
        stg = [alloc([128, 8, 256], F32)[0] for _ in range(NST)]
        ring = [alloc([128, 8, 256], BF16)[0] for _ in range(NRB)]
        stgR = [Res("stg%d" % i) for i in range(NST)]
        ringR = [Res("ring%d" % i) for i in range(NRB)]
        _, RA = alloc([128, 16384], F32)
        _, RB = alloc([128, 8192], F32)
        _, RC = alloc([128, 8192], F32)
        assert cur[0] <= ARENA_BYTES, cur[0]

        constR = Res("const")
        SR = Res("S")
        SbR = Res("Sb")
        statR = Res("stat")

        wctr = [0, 0]

        def wchunk(W2d, kc0, c0, nk=8, ncols=256):
            s = wctr[0] % NST
            b = wctr[1] % NRB
            wctr[0] += 1
            wctr[1] += 1
            src = W2d[kc0 * 128:(kc0 + nk) * 128, c0:c0 + ncols].rearrange("(kc p) c -> p kc c", p=128)
            sv = stg[s][:, 0:nk, 0:ncols]
            rv = ring[b][:, 0:nk, 0:ncols]
            P.op("sp", lambda e, sv=sv, src=src: e.dma_start(out=sv, in_=src),
                 writes=[stgR[s]], dma="st%d" % s, nofence=True)
            P.op("pool", lambda e, sv=sv, rv=rv: e.tensor_copy(out=rv, in_=sv),
                 reads=[stgR[s]], writes=[ringR[b]], nofence=True)
            return ring[b], ringR[b]

        bctr = [0]

        def nbank():
            b = bctr[0] % 8
            bctr[0] += 1
            return b

        def setup():
            tmpC_f = view(RC, [128, 16, NR], F32)
            tmpC_w = view(RC + 8192, [128, 8, 128], F32)
            tmpC_wb = view(RC + 8192 + 4096, [128, 8, 128], BF16)
            bsrow = view(RC + 16384, [1, 1024], F32)
            bstmp = view(RC + 16384 + 4096, [1, 1024], F32)
            l01 = view(RC + 30720, [128, 2, 8], F32)
            g2b = g2T.rearrange("p (a o) -> p a o", o=1)
            sp = "cst"

            def ld(dst, src, slow=False):
                if slow:
                    P.op("sp", lambda e: e.dma_start(out=dst, in_=src, allow_slow_non_contiguous=True),
                         writes=[constR], dma=sp)
                else:
                    P.op("sp", lambda e: e.dma_start(out=dst, in_=src), writes=[constR], dma=sp)

            ld(cst, cst_d[:, :])
            ld(g1T, g1_d.rearrange("(kc p) -> p kc", p=128), True)
            ld(g2T, g2_d.rearrange("(kc p) -> p kc", p=128), True)
            ld(ngT, hng_d.rearrange("(h v) -> v h", v=128), True)
            ld(l01, lbl_d.rearrange("l (h k) -> k l h", k=128), True)
            ld(brt, br_d.partition_broadcast(128))
            ld(bsrow, bsp_d[:, :])
            ld(tmpC_w, wsp_d.rearrange("g t s -> t g s"))
            ld(tmpC_f, wr_d.rearrange("(kc p) c -> p kc c", p=128))
            P.op("dve", lambda e: e.tensor_copy(out=identb, in_=ident_f), reads=[constR], writes=[constR])
            P.op("dve", lambda e: e.memset(onesb, 1.0), writes=[constR])
            P.op("dve", lambda e: e.memset(S_f, 0.0), writes=[SR])
            P.op("dve", lambda e: e.memset(S_b, 0.0), writes=[SbR])
            P.op("dve", lambda e: e.memset(Aj, 0.0), writes=[constR])
            P.op("dve", lambda e: e.tensor_tensor(out=lbT, in0=l01[:, 0, :], in1=l01[:, 1, :], op=ALU.subtract),
                 reads=[constR], writes=[constR])
            P.op("act", lambda e: e.activation(out=lbT, in_=lbT, func=AF.Sigmoid), reads=[constR], writes=[constR])
            P.op("dve", lambda e: e.tensor_scalar(out=omlT, in0=lbT, scalar1=-1.0, scalar2=1.0, op0=ALU.mult, op1=ALU.add),
                 reads=[constR], writes=[constR])
            P.op("dve", lambda e: e.tensor_copy(out=bshi, in_=bsrow), reads=[constR], writes=[constR])
            P.op("dve", lambda e: e.tensor_tensor(out=bstmp, in0=bsrow, in1=bshi, op=ALU.subtract), reads=[constR], writes=[constR])
            P.op("dve", lambda e: e.tensor_copy(out=bslo, in_=bstmp), reads=[constR], writes=[constR])
            P.op("dve", lambda e: e.tensor_tensor(out=tmpC_wb, in0=tmpC_w, in1=bc(LT.rearrange("p (o f) -> p o f", o=1), [128, 8, 128]), op=ALU.mult),
                 reads=[constR], writes=[constR])
            b = nbank()

            def tr8(e):
                ins = None
                for g in range(8):
                    ins = e.transpose(banks_bf[b][:, g * 128:(g + 1) * 128], tmpC_wb[:, g, :], identb)
                return ins
            P.op("pe", tr8, reads=[constR], writes=[bankR[b]])
            P.op("dve", lambda e: e.tensor_copy(out=wsT, in_=banks_bf[b][:, 0:1024].rearrange("p (g t) -> p g t", g=8)),
                 reads=[bankR[b]], writes=[constR])
            P.op("dve", lambda e: e.tensor_tensor(out=tmpC_f, in0=tmpC_f, in1=bc(g2b, [128, 16, NR]), op=ALU.mult),
                 reads=[constR], writes=[constR])
            P.op("dve", lambda e: e.tensor_copy(out=wrhi, in_=tmpC_f), reads=[constR], writes=[constR])
            tmp2 = view(RC + 24576, [128, 16, NR], F32) if 16 * NR * 4 <= 8192 else None
            P.op("dve", lambda e: e.tensor_tensor(out=tmp2, in0=tmpC_f, in1=wrhi, op=ALU.subtract), reads=[constR], writes=[constR])
            P.op("dve", lambda e: e.tensor_copy(out=wrlo, in_=tmp2), reads=[constR], writes=[constR])
            P.barrier()

        def norm_transpose(src_rows, gT, dstT, tmp_off, store_x=None):
            xin = [view(tmp_off + i * 8192, [128, D], F32) for i in range(2)]
            xs = [view(tmp_off + 16384 + i * 4096, [128, D], BF16) for i in range(2)]
            junk = view(tmp_off + 24576, [128, D], BF16)
            xinR = [Res(), Res()]
            xsR = [Res(), Res()]
            junkR = Res()
            g3 = gT.rearrange("p (a o) -> p a o", o=1)
            for t in range(8):
                s = t % 2
                xt = xin[s]
                src = src_rows(t)
                P.op("sp", lambda e, xt=xt, src=src: e.dma_start(out=xt, in_=src), writes=[xinR[s]], dma="xin%d" % s)
                ssq = stat[:, 2 * s:2 * s + 1]
                rt = stat[:, 2 * s + 1:2 * s + 2]
                P.op("act", lambda e, xt=xt, ssq=ssq: e.activation(out=junk, in_=xt, func=AF.Square, accum_out=ssq),
                     reads=[xinR[s]], writes=[junkR, statR])
                P.op("act", lambda e, ssq=ssq, rt=rt: e.activation(out=rt, in_=ssq, func=AF.Sqrt, bias=EPS, scale=1.0 / D),
                     reads=[statR], writes=[statR])
                P.op("dve", lambda e, rt=rt: e.reciprocal(out=rt, in_=rt), reads=[statR], writes=[statR])
                xst = xs[s]
                P.op("dve", lambda e, xt=xt, xst=xst, rt=rt: e.tensor_scalar(out=xst, in0=xt, scalar1=rt, scalar2=None, op0=ALU.mult),
                     reads=[xinR[s], statR], writes=[xsR[s]])
                b0, b1 = nbank(), nbank()

                def tr(e, xst=xst, b0=b0, b1=b1):
                    ins = None
                    for j in range(16):
                        bb = b0 if j < 8 else b1
                        ins = e.transpose(banks_bf[bb][:, (j % 8) * 128:(j % 8 + 1) * 128], xst[:, j * 128:(j + 1) * 128], identb)
                    return ins
                P.op("pe", tr, reads=[xsR[s]], writes=[bankR[b0], bankR[b1]])
                for hf, bb in ((0, b0), (1, b1)):
                    o = dstT[:, hf * 8:(hf + 1) * 8, t * 128:(t + 1) * 128]
                    i0 = banks_bf[bb][:, 0:1024].rearrange("p (a b) -> p a b", a=8)
                    i1 = bc(g3[:, hf * 8:(hf + 1) * 8, :], [128, 8, 128])
                    P.op("dve", lambda e, o=o, i0=i0, i1=i1: e.tensor_tensor(out=o, in0=i0, in1=i1, op=ALU.mult),
                         reads=[bankR[bb]], writes=[])

        def mm_group(b, bcols, lhs_list, rhs_list, reads):
            n = len(lhs_list)

            def fn(e):
                ins = None
                for i in range(n):
                    ins = e.matmul(banks[b][:, bcols[0]:bcols[1]], lhs_list[i], rhs_list[i], start=(i == 0), stop=(i == n - 1))
                return ins
            return P.op("pe", fn, reads=reads, writes=[bankR[b]])

        def hgrn_tile_common(fs_t, kT_t, T, TR):
            a = T["a"]
            P.op("dve", lambda e: e.tensor_scalar(out=kT_t, in0=fs_t, scalar1=-1.0, scalar2=1.0, op0=ALU.mult, op1=ALU.add),
                 reads=[TR["fs"]], writes=[TR["k"]])
            P.op("act", lambda e: e.activation(out=fs_t, in_=fs_t, func=AF.Ln), reads=[TR["fs"], TR["k"]], writes=[TR["fs"]])
            a2 = a.rearrange("p a b -> p (a b)")
            lf2 = fs_t.rearrange("p a b -> p (a b)")
            P.op("dve", lambda e: e.tensor_tensor_scan(out=a2, data0=rmask, data1=lf2, initial=0.0, op0=ALU.mult, op1=ALU.add),
                 reads=[TR["fs"]], writes=[TR["a"]])

        def hgrn_state_update(kT_t, itok_t, T, TR):
            a = T["a"]
            tA, tC, kdecT, kdtok, dS = T["tA"], T["tC"], T["kdecT"], T["kdtok"], T["dS"]
            aend = a[:, :, 127:128]
            P.op("dve", lambda e: e.tensor_tensor(out=tA, in0=bc(aend, [128, 8, 128]), in1=a, op=ALU.subtract),
                 reads=[TR["a"]], writes=[TR["tA"]])
            P.op("act", lambda e: e.activation(out=tC, in_=tA, func=AF.Exp), reads=[TR["tA"]], writes=[TR["tC"]])
            P.op("dve", lambda e: e.tensor_tensor(out=kdecT, in0=kT_t, in1=tC, op=ALU.mult),
                 reads=[TR["k"], TR["tC"]], writes=[TR["kdecT"]])
            P.op("act", lambda e: e.activation(out=dS, in_=aend, func=AF.Exp), reads=[TR["a"]], writes=[TR["dS"]])
            b = nbank()

            def tr(e):
                ins = None
                for h in range(8):
                    ins = e.transpose(banks_bf[b][:, h * 128:(h + 1) * 128], kdecT[:, h, :], identb)
                return ins
            P.op("pe", tr, reads=[TR["kdecT"]], writes=[bankR[b]])
            P.op("act", lambda e: e.copy(out=kdtok, in_=banks_bf[b][:, 0:1024]), reads=[bankR[b]], writes=[TR["kdtok"]])
            for hb in range(2):
                b2 = nbank()

                def mmB(e, hb=hb, b2=b2):
                    ins = None
                    for hq in range(4):
                        h = hb * 4 + hq
                        ins = e.matmul(banks[b2][:, hq * 128:(hq + 1) * 128], kdtok[:, h * 128:(h + 1) * 128],
                                       itok_t[:, h * 128:(h + 1) * 128], start=True, stop=True)
                    return ins
                P.op("pe", mmB, reads=[TR["kdtok"], TR["itok"]], writes=[bankR[b2]])
                Sv = S_f[:, hb * 4:(hb + 1) * 4, :]
                dSv = bc(dS[:, hb * 4:(hb + 1) * 4, :], [128, 4, 128])
                Bv = banks[b2][:, 0:512].rearrange("p (a b) -> p a b", a=4)
                P.op("dve", lambda e, Sv=Sv, dSv=dSv: e.tensor_tensor(out=Sv, in0=Sv, in1=dSv, op=ALU.mult),
                     reads=[TR["dS"]], writes=[SR])
                P.op("dve", lambda e, Sv=Sv, Bv=Bv: e.tensor_tensor(out=Sv, in0=Sv, in1=Bv, op=ALU.add),
                     reads=[bankR[b2]], writes=[SR])
            P.op("act", lambda e: e.copy(out=S_b, in_=S_f), reads=[SR], writes=[SbR])

        def hgrn_temps(base_offs):
            T, TR = {}, {}
            return T, TR

        hT = view(RB, [128, 16, 1024], BF16)
        yaT = view(RA, [128, 8, 1024], BF16)
        ybT = view(RA + 16384, [128, 8, 1024], BF16)
        mT = view(RC, [128, 16, 1024], BF16)
        yacc = view(RA, [128, 8, D], F32)

        def proj_fm(W2d, c0, actT, nkc, ntok_blocks, tokw, consume, tok0=0):
            chunks = []
            for k0 in range(0, nkc, 8):
                chunks.append(wchunk(W2d, k0, c0))
            for cb in range(2):
                for tb in range(ntok_blocks):
                    b = nbank()
                    lhs = [chunks[kc // 8][0][:, kc % 8, cb * 128:(cb + 1) * 128] for kc in range(nkc)]
                    rhs = [actT[:, kc, tok0 + tb * tokw: tok0 + (tb + 1) * tokw] for kc in range(nkc)]
                    mm_group(b, (0, tokw), lhs, rhs, [c[1] for c in chunks])
                    consume(cb, tb, b)

        def proj_tm(W2d, c0, actT, nkc, tiles, consume):
            chunks = []
            for k0 in range(0, nkc, 8):
                chunks.append(wchunk(W2d, k0, c0))
            for t in tiles:
                b = nbank()
                lhs = [actT[:, kc, t * 128:(t + 1) * 128] for kc in range(nkc)]
                rhs = [chunks[kc // 8][0][:, kc % 8, 0:256] for kc in range(nkc)]
                mm_group(b, (0, 256), lhs, rhs, [c[1] for c in chunks])
                consume(t, b)

        def hgrn_alloc(off_list):
            pools = [[o, o + s] for o, s in off_list]

            def take(shape, dt):
                n = 1
                for s_ in shape[1:]:
                    n *= s_
                sz = n * (4 if dt == F32 else 2)
                sz = (sz + 63) // 64 * 64
                for p in pools:
                    if p[1] - p[0] >= sz:
                        o = p[0]
                        p[0] += sz
                        return view(o, shape, dt)
                raise RuntimeError("hgrn temp alloc failed")
            return take

        def prologue_half(ph):
            norm_transpose(lambda t: xp_d[ph * 1024 + t * 128: ph * 1024 + (t + 1) * 128, :], g1T, hT, RA)
            P.barrier()
            take = hgrn_alloc([(RA, 65536), (RC, 32768)])
            fs = take([128, 2, 8, 128], F32)
            kT = take([128, 2, 8, 128], BF16)
            itok = take([128, 2, 1024], BF16)
            T = {"a": take([128, 8, 128], F32), "tA": take([128, 8, 128], F32), "tC": take([128, 8, 128], F32),
                 "kdecT": take([128, 8, 128], BF16), "kdtok": take([128, 1024], BF16), "dS": take([128, 8, 1], F32)}
            TR = {k: Res(k) for k in ("fs", "k", "a", "tA", "tC", "kdecT", "kdtok", "dS", "itok")}
            for tb4 in range(4):
                tok0 = tb4 * 256
                hgrn_project_f_i(tok0, fs, itok, TR)
                hgrn_f_affine(fs, TR)
                for tl in range(2):
                    hgrn_tile_common(fs[:, tl], kT[:, tl], T, TR)
                    hgrn_state_update(kT[:, tl], itok[:, tl, :], T, TR)
            P.barrier()


        def hgrn_project_f_i(tok0, fs, itok, TR, qT=None, sgn=None, sgtmp=None):
            sections = [("f", 3072)]
            if qT is not None:
                sections = [("q", 2048), ("f", 3072), ("g", 5120)]
            for name, cbase in sections:
                for cg in range(4):
                    def consume(cb, tb, b, name=name, cg=cg):
                        h = cg * 2 + cb
                        src = banks[b][:, 0:256].rearrange("p (t c) -> p t c", t=2)
                        if name == "f":
                            o = fs[:, :, h, :]
                            P.op("act", lambda e: e.activation(out=o, in_=src, func=AF.Sigmoid), reads=[bankR[b]], writes=[TR["fs"]])
                        elif name == "q":
                            o = qT[:, :, h, :]
                            P.op("act", lambda e: e.copy(out=o, in_=src), reads=[bankR[b]], writes=[TR["q"]])
                        else:
                            P.op("act", lambda e: e.activation(out=sgtmp, in_=banks[b][:, 0:256], func=AF.Silu),
                                 reads=[bankR[b]], writes=[TR["sgtmp"]])
                            o = sgn[:, :, h, :]
                            sv = sgtmp.rearrange("p (t c) -> p t c", t=2)
                            P.op("dve", lambda e: e.tensor_scalar(out=o, in0=sv, scalar1=ngT[:, h:h + 1], scalar2=None, op0=ALU.mult),
                                 reads=[TR["sgtmp"]], writes=[TR["sgn"]])
                    proj_fm(win_d, cbase + cg * 256, hT, 16, 1, 256, consume, tok0=tok0)
            for cg in range(4):
                def consume_i(t, b, cg=cg):
                    tl = t - tok0 // 128
                    o = itok[:, tl, cg * 256:(cg + 1) * 256]
                    P.op("act", lambda e: e.copy(out=o, in_=banks[b][:, 0:256]), reads=[bankR[b]], writes=[TR["itok"]])
                proj_tm(win_d, 4096 + cg * 256, hT, 16, [tok0 // 128, tok0 // 128 + 1], consume_i)

        def hgrn_f_affine(fs, TR):
            oml3 = omlT.rearrange("p (o h c) -> p o h c", o=1, c=1)
            lb3 = lbT.rearrange("p (o h c) -> p o h c", o=1, c=1)
            P.op("dve", lambda e: e.tensor_tensor(out=fs, in0=fs, in1=bc(oml3, [128, 2, 8, 128]), op=ALU.mult),
                 reads=[TR["fs"]], writes=[TR["fs"]])
            P.op("dve", lambda e: e.tensor_tensor(out=fs, in0=fs, in1=bc(lb3, [128, 2, 8, 128]), op=ALU.add),
                 reads=[TR["fs"]], writes=[TR["fs"]])

        def dump(dst, src, name):
            P.op("sp", lambda e: e.dma_start(out=dst, in_=src), dma=name)
            P.barrier()

        def main_half(hf):
            xbase = hf * 1024
            dbg = DBG and hf == 0
            if dbg:
                dump(dbg_S, S_f.rearrange("p a b -> p (a b)"), "dbgS")
            norm_transpose(lambda t: x_d[xbase + t * 128: xbase + (t + 1) * 128, :], g1T, hT, RA)
            P.barrier()
            if dbg:
                dump(dbg_h, hT.rearrange("p a b -> p (a b)"), "dbgh")
            if STOP <= 1:
                return
            vg = view(RC, [128, 8, 1024], F32)
            vln = view(RA + 32768, [128, 8, 1024], BF16)
            lnG = view(RA + 49152, [128, 1024], F32)
            lnB = view(RA + 53248, [128, 1024], F32)
            tmpv = view(RA + 57344, [128, 1024], F32)
            junkv = view(RA + 61440, [128, 1024], BF16)
            lnR, lnR2 = Res(), Res()
            P.op("sp", lambda e: e.dma_start(out=lnG, in_=lng_d.partition_broadcast(128)), writes=[lnR], dma="lng")
            P.op("sp", lambda e: e.dma_start(out=lnB, in_=lnb_d.partition_broadcast(128)), writes=[lnR2], dma="lnb")
            uR = Res()
            for cg in range(4):
                def cons_u(cb, tb, b, cg=cg):
                    o = yaT[:, cg * 2 + cb, tb * 512:(tb + 1) * 512]
                    P.op("act", lambda e: e.activation(out=o, in_=banks[b][:, 0:512], func=AF.Gelu_apprx_tanh),
                         reads=[bankR[b]], writes=[uR])
                proj_fm(win_d, cg * 256, hT, 16, 2, 512, cons_u)
            vgR = [Res() for _ in range(8)]
            for cg in range(4):
                def cons_v(t, b, cg=cg):
                    o = vg[:, t, cg * 256:(cg + 1) * 256]
                    P.op("act", lambda e: e.activation(out=o, in_=banks[b][:, 0:256], func=AF.Gelu_apprx_tanh),
                         reads=[bankR[b]], writes=[vgR[t]])
                proj_tm(win_d, 1024 + cg * 256, hT, 16, range(8), cons_v)
            tmpR, junkR2, vlnR = Res(), Res(), [Res() for _ in range(8)]
            yaR = Res()
            for t in range(8):
                vt = vg[:, t, :]
                s1, s2, mean, msq, var = (stat[:, 8 + i:9 + i] for i in range(5))
                P.op("dve", lambda e, vt=vt: e.tensor_reduce(out=s1, in_=vt, axis=AX.X, op=ALU.add), reads=[vgR[t]], writes=[statR])
                P.op("act", lambda e, vt=vt: e.activation(out=junkv, in_=vt, func=AF.Square, accum_out=s2),
                     reads=[vgR[t]], writes=[junkR2, statR])
                P.op("dve", lambda e: e.tensor_scalar(out=mean, in0=s1, scalar1=1.0 / 1024, scalar2=None, op0=ALU.mult), reads=[statR], writes=[statR])
                P.op("dve", lambda e: e.tensor_tensor(out=msq, in0=mean, in1=mean, op=ALU.mult), reads=[statR], writes=[statR])
                P.op("dve", lambda e: e.tensor_scalar(out=var, in0=s2, scalar1=1.0 / 1024, scalar2=msq, op0=ALU.mult, op1=ALU.subtract),
                     reads=[statR], writes=[statR])
                P.op("act", lambda e: e.activation(out=var, in_=var, func=AF.Sqrt, bias=EPS, scale=1.0), reads=[statR], writes=[statR])
                P.op("dve", lambda e: e.reciprocal(out=var, in_=var), reads=[statR], writes=[statR])
                P.op("dve", lambda e, vt=vt: e.tensor_scalar(out=tmpv, in0=vt, scalar1=mean, scalar2=var, op0=ALU.subtract, op1=ALU.mult),
                     reads=[vgR[t], statR], writes=[tmpR])
                P.op("dve", lambda e: e.tensor_tensor(out=tmpv, in0=tmpv, in1=lnG, op=ALU.mult), reads=[tmpR, lnR], writes=[tmpR])
                vl = vln[:, t, :]
                P.op("dve", lambda e, vl=vl: e.tensor_tensor(out=vl, in0=tmpv, in1=lnB, op=ALU.add), reads=[tmpR, lnR2], writes=[vlnR[t]])
                for gb in range(2):
                    b = nbank()

                    def sp_mm(e, t=t, gb=gb, b=b):
                        ins = None
                        for gq in range(4):
                            g = gb * 4 + gq
                            o = banks[b][:, gq * 128:(gq + 1) * 128]
                            e.matmul(o, vln[:, t, g * 128:(g + 1) * 128], wsT[:, g, :], start=True, stop=False)
                            e.matmul(o, onesb[0:1, :], bshi[0:1, g * 128:(g + 1) * 128], start=False, stop=False)
                            ins = e.matmul(o, onesb[0:1, :], bslo[0:1, g * 128:(g + 1) * 128], start=False, stop=True)
                        return ins
                    P.op("pe", sp_mm, reads=[vlnR[t]], writes=[bankR[b]])
                    o = yaT[:, gb * 4:(gb + 1) * 4, t * 128:(t + 1) * 128]
                    pv = banks[b][:, 0:512].rearrange("p (a b) -> p a b", a=4)
                    P.op("dve", lambda e, o=o, pv=pv: e.tensor_tensor(out=o, in0=pv, in1=o, op=ALU.mult),
                         reads=[bankR[b], uR], writes=[yaR])
            P.barrier()
            if dbg:
                dump(dbg_ya, yaT.rearrange("p a b -> p (a b)"), "dbgya")
            if STOP <= 2:
                return
            take = hgrn_alloc([(RA + 32768, 32768), (RC, 32768), (SPARE, SPARE_SZ)])
            fs = take([128, 2, 8, 128], F32)
            kT = take([128, 2, 8, 128], BF16)
            qT = take([128, 2, 8, 128], BF16)
            sgn = take([128, 2, 8, 128], BF16)
            itok = take([128, 2, 1024], BF16)
            sgtmp = take([128, 256], F32)
            T = {"a": take([128, 8, 128], F32), "tA": take([128, 8, 128], F32), "tB": take([128, 8, 128], F32),
                 "tC": take([128, 8, 128], F32), "qd": take([128, 8, 128], BF16), "qdt": take([128, 8, 128], BF16),
                 "kinv": take([128, 8, 128], BF16), "kdecT": take([128, 8, 128], BF16), "kdtok": take([128, 1024], BF16),
                 "dS": take([128, 8, 1], F32), "Ef": take([128, 8, 8, 8], F32), "E": take([128, 8, 8, 8], BF16),
                 "kk0": take([128, 8, 8, 16], BF16), "kk1": take([128, 8, 8, 16], BF16),
                 "scm": take([128, 8, 128], BF16), "osq": take([128, 8, 128], BF16)}
            TR = {k: Res(k) for k in list(T.keys()) + ["fs", "k", "q", "sgn", "sgtmp", "itok", "scm0", "scm1", "osq0", "osq1"]}
            ybR = Res()
            for tb4 in range(4):
                tok0 = tb4 * 256
                hgrn_project_f_i(tok0, fs, itok, TR, qT=qT, sgn=sgn, sgtmp=sgtmp)
                hgrn_f_affine(fs, TR)
                for tl in range(2):
                    tokt = tok0 + tl * 128
                    fs_t, kT_t, qT_t, sgn_t, itok_t = fs[:, tl], kT[:, tl], qT[:, tl], sgn[:, tl], itok[:, tl, :]
                    hgrn_tile_common(fs_t, kT_t, T, TR)
                    a, tA, tB, tC = T["a"], T["tA"], T["tB"], T["tC"]
                    a4 = a.rearrange("p h (j r) -> p h j r", r=16)
                    Aj4 = Aj.rearrange("p h (j o) -> p h j o", o=1)
                    P.op("dve", lambda e, a4=a4, Aj4=Aj4: e.tensor_copy(out=Aj4[:, :, 1:8, :], in_=a4[:, :, 0:7, 15:16]),
                         reads=[TR["a"]], writes=[TR["Ef"]])
                    tA4 = tA.rearrange("p h (j r) -> p h j r", r=16)
                    P.op("dve", lambda e, a4=a4, Aj4=Aj4, tA4=tA4: e.tensor_tensor(out=tA4, in0=a4, in1=bc(Aj4, [128, 8, 8, 16]), op=ALU.subtract),
                         reads=[TR["a"], TR["Ef"]], writes=[TR["tA"]])
                    P.op("act", lambda e, tA=tA, tB=tB: e.activation(out=tB, in_=tA, func=AF.Exp), reads=[TR["tA"]], writes=[TR["tB"]])
                    P.op("dve", lambda e, qT_t=qT_t, tB=tB: e.tensor_tensor(out=T["qd"], in0=qT_t, in1=tB, op=ALU.mult),
                         reads=[TR["q"], TR["tB"]], writes=[TR["qd"]])
                    P.op("act", lambda e, tA=tA, tC=tC: e.activation(out=tC, in_=tA, func=AF.Exp, scale=-1.0), reads=[TR["tA"]], writes=[TR["tC"]])
                    P.op("dve", lambda e, kT_t=kT_t, tC=tC: e.tensor_tensor(out=T["kinv"], in0=kT_t, in1=tC, op=ALU.mult),
                         reads=[TR["k"], TR["tC"]], writes=[TR["kinv"]])
                    P.op("act", lambda e, a=a, tB=tB: e.activation(out=tB, in_=a, func=AF.Exp), reads=[TR["a"]], writes=[TR["tB"]])
                    P.op("dve", lambda e, qT_t=qT_t, tB=tB: e.tensor_tensor(out=T["qdt"], in0=qT_t, in1=tB, op=ALU.mult),
                         reads=[TR["q"], TR["tB"]], writes=[TR["qdt"]])
                    Ef, Eb = T["Ef"], T["E"]
                    Ajj = Aj.rearrange("p h (j o) -> p h j o", o=1)
                    Ajc = Aj.rearrange("p h (o c) -> p h o c", o=1)
                    P.op("dve", lambda e, Ef=Ef: e.tensor_tensor(out=Ef, in0=bc(Ajj, [128, 8, 8, 8]), in1=bc(Ajc, [128, 8, 8, 8]), op=ALU.subtract),
                         reads=[TR["Ef"]], writes=[TR["E"]])
                    P.op("dve", lambda e, Ef=Ef: e.tensor_scalar(out=Ef, in0=Ef, scalar1=0.0, scalar2=None, op0=ALU.min),
                         reads=[TR["E"]], writes=[TR["E"]])
                    P.op("act", lambda e, Ef=Ef, Eb=Eb: e.activation(out=Eb, in_=Ef, func=AF.Exp), reads=[TR["E"]], writes=[TR["E"]])
                    obanks = []
                    for hb in range(2):
                        bs_ = nbank()
                        for hq in range(4):
                            h = hb * 4 + hq
                            kk = T["kk%d" % (h % 2)]
                            kkR = TR["kk%d" % (h % 2)]
                            kin = T["kinv"][:, h, :].rearrange("p (o c r) -> p o c r", o=1, r=16)
                            Eh = Eb[:, h].rearrange("p j (c o) -> p j c o", o=1)
                            P.op("dve", lambda e, kk=kk, kin=kin, Eh=Eh: e.tensor_tensor(out=kk, in0=bc(kin, [128, 8, 8, 16]), in1=bc(Eh, [128, 8, 8, 16]), op=ALU.mult),
                                 reads=[TR["kinv"], TR["E"]], writes=[kkR])

                            def sc_mm(e, kk=kk, h=h, hq=hq, bs_=bs_):
                                ins = None
                                for j in range(8):
                                    ins = e.matmul(banks[bs_][:, hq * 128 + j * 16: hq * 128 + (j + 1) * 16],
                                                   kk[:, j].rearrange("p c r -> p (c r)"), T["qd"][:, h, j * 16:(j + 1) * 16],
                                                   start=True, stop=True)
                                return ins
                            P.op("pe", sc_mm, reads=[kkR, TR["qd"]], writes=[bankR[bs_]])
                        scm = T["scm"][:, hb * 4:(hb + 1) * 4, :]
                        scR = TR["scm%d" % hb]
                        UT3 = bc(UT.rearrange("p (o f) -> p o f", o=1), [128, 4, 128])
                        sv = banks[bs_][:, 0:512].rearrange("p (a b) -> p a b", a=4)
                        P.op("dve", lambda e, scm=scm, sv=sv, UT3=UT3: e.tensor_tensor(out=scm, in0=sv, in1=UT3, op=ALU.mult),
                             reads=[bankR[bs_]], writes=[scR])
                        bo = nbank()
                        obanks.append(bo)

                        def o_mm(e, hb=hb, bo=bo, itok_t=itok_t):
                            ins = None
                            for hq in range(4):
                                h = hb * 4 + hq
                                o = banks[bo][:, hq * 128:(hq + 1) * 128]
                                e.matmul(o, itok_t[:, h * 128:(h + 1) * 128], T["scm"][:, h, :], start=True, stop=False)
                                ins = e.matmul(o, S_b[:, h, :], T["qdt"][:, h, :], start=False, stop=True)
                            return ins
                        P.op("pe", o_mm, reads=[scR, TR["itok"], SbR, TR["qdt"]], writes=[bankR[bo]])
                    hgrn_state_update(kT_t, itok_t, T, TR)
                    for hb in range(2):
                        bo = obanks[hb]
                        osq = T["osq"][:, hb * 4:(hb + 1) * 4, :]
                        oR = TR["osq%d" % hb]
                        ov = banks[bo][:, 0:512].rearrange("p (a b) -> p a b", a=4)
                        P.op("act", lambda e, osq=osq, ov=ov: e.activation(out=osq, in_=ov, func=AF.Square), reads=[bankR[bo]], writes=[oR])
                        bq = nbank()
                        mm_group(bq, (0, 512), [onesb], [osq.rearrange("p a b -> p (a b)")], [oR])
                        tq = (tA if hb == 0 else tC)[:, 0:4, :]
                        tqR = TR["tA"] if hb == 0 else TR["tC"]
                        qv = banks[bq][:, 0:512].rearrange("p (a b) -> p a b", a=4)
                        P.op("act", lambda e, tq=tq, qv=qv: e.activation(out=tq, in_=qv, func=AF.Sqrt, bias=EPS, scale=1.0 / 128),
                             reads=[bankR[bq]], writes=[tqR])
                        P.op("dve", lambda e, tq=tq: e.reciprocal(out=tq, in_=tq), reads=[tqR], writes=[tqR])
                        P.op("dve", lambda e, tq=tq, ov=ov: e.tensor_tensor(out=tq, in0=ov, in1=tq, op=ALU.mult),
                             reads=[bankR[bo], tqR], writes=[tqR])
                        yo = ybT[:, hb * 4:(hb + 1) * 4, tokt:tokt + 128]
                        sg4 = sgn_t[:, hb * 4:(hb + 1) * 4, :]
                        P.op("dve", lambda e, yo=yo, tq=tq, sg4=sg4: e.tensor_tensor(out=yo, in0=tq, in1=sg4, op=ALU.mult),
                             reads=[tqR, TR["sgn"]], writes=[ybR])
            P.barrier()
            if dbg:
                dump(dbg_yb, ybT.rearrange("p a b -> p (a b)"), "dbgyb")
            if STOP <= 3:
                return
            sA = [view(RA + 32768 + i * 2048, [128, 512], F32) for i in range(4)]
            sAR = [Res() for _ in range(4)]
            sctr = [0]
            for cg in range(8):
                parts = []
                for (goff, Wp, yT) in ((6144, wpa_d, yaT), (8192, wpb_d, ybT)):
                    cw = [wchunk(win_d, 0, goff + cg * 256), wchunk(win_d, 8, goff + cg * 256)]
                    cp = wchunk(Wp, 0, cg * 256)
                    parts.append((cw, cp, yT))
                for cb in range(2):
                    for tb in range(2):
                        tsl = slice(tb * 512, (tb + 1) * 512)
                        prods = []
                        for (cw, cp, yT) in parts:
                            bg = nbank()
                            mm_group(bg, (0, 512), [cw[kc // 8][0][:, kc % 8, cb * 128:(cb + 1) * 128] for kc in range(16)],
                                     [hT[:, kc, tsl] for kc in range(16)], [cw[0][1], cw[1][1]])
                            si = sctr[0] % 4
                            sctr[0] += 1
                            sg_ = sA[si]
                            P.op("act", lambda e, sg_=sg_, bg=bg: e.activation(out=sg_, in_=banks[bg][:, 0:512], func=AF.Sigmoid),
                                 reads=[bankR[bg]], writes=[sAR[si]])
                            bp = nbank()
                            mm_group(bp, (0, 512), [cp[0][:, kc, cb * 128:(cb + 1) * 128] for kc in range(8)],
                                     [yT[:, kc, tsl] for kc in range(8)], [cp[1]])
                            P.op("dve", lambda e, sg_=sg_, bp=bp: e.tensor_tensor(out=sg_, in0=sg_, in1=banks[bp][:, 0:512], op=ALU.mult),
                                 reads=[bankR[bp], sAR[si]], writes=[sAR[si]])
                            prods.append((sg_, sAR[si]))
                        o = mT[:, cg * 2 + cb, tsl]
                        P.op("dve", lambda e, o=o, p0=prods[0][0], p1=prods[1][0]: e.tensor_tensor(out=o, in0=p0, in1=p1, op=ALU.add),
                             reads=[prods[0][1], prods[1][1]], writes=[])
            P.barrier()
            if dbg:
                dump(dbg_m, mT.rearrange("p a b -> p (a b)"), "dbgm")
            if STOP <= 4:
                return
            yR = [Res() for _ in range(8)]
            for t in range(8):
                yt = yacc[:, t, :]
                src = x_d[xbase + t * 128: xbase + (t + 1) * 128, :]
                P.op("sp", lambda e, yt=yt, src=src: e.dma_start(out=yt, in_=src), writes=[yR[t]], dma="xres%d" % t)
            for cg in range(8):
                def cons_o(t, b, cg=cg):
                    o = yacc[:, t, cg * 256:(cg + 1) * 256]
                    P.op("dve", lambda e: e.tensor_tensor(out=o, in0=o, in1=banks[b][:, 0:256], op=ALU.add),
                         reads=[bankR[b]], writes=[yR[t]])
                proj_tm(wout_d, cg * 256, mT, 16, range(8), cons_o)
            P.barrier()
            if dbg:
                dump(dbg_x2.rearrange("(t p) d -> p t d", p=128), yacc, "dbgx2")
            if STOP <= 5:
                return
            h2T = hT
            G = view(RC, [128, 8, NE], F32)
            xs32 = view(RC + 4096, [128, D], F32)
            hi = view(RC + 12288, [128, D], BF16)
            lo = view(RC + 16384, [128, D], BF16)
            junk = view(RC + 20480, [128, D], BF16)
            hiT = view(RC + 24576, [128, 16, 128], BF16)
            loT = view(RC + 28672, [128, 16, 128], BF16)
            rs = view(SPARE, [128, 512], F32)
            xsR, hiR, loR, jkR, hiTR, loTR, rsR, GR = (Res() for _ in range(8))
            g3 = g2T.rearrange("p (a o) -> p a o", o=1)
            for t in range(8):
                yt = yacc[:, t, :]
                ssq, rt = stat[:, 16:17], stat[:, 17:18]
                P.op("act", lambda e, yt=yt: e.activation(out=junk, in_=yt, func=AF.Square, accum_out=ssq), reads=[yR[t]], writes=[jkR, statR])
                P.op("act", lambda e: e.activation(out=rt, in_=ssq, func=AF.Sqrt, bias=EPS, scale=1.0 / D), reads=[statR], writes=[statR])
                P.op("dve", lambda e: e.reciprocal(out=rt, in_=rt), reads=[statR], writes=[statR])
                P.op("dve", lambda e, yt=yt: e.tensor_scalar(out=xs32, in0=yt, scalar1=rt, scalar2=None, op0=ALU.mult),
                     reads=[yR[t], statR], writes=[xsR])
                if STOP < 5.01:
                    continue
                P.op("dve", lambda e: e.tensor_copy(out=hi, in_=xs32), reads=[xsR], writes=[hiR])
                P.op("dve", lambda e: e.tensor_tensor(out=xs32, in0=xs32, in1=hi, op=ALU.subtract), reads=[xsR, hiR], writes=[xsR])
                P.op("dve", lambda e: e.tensor_copy(out=lo, in_=xs32), reads=[xsR], writes=[loR])
                if STOP < 5.02:
                    continue
                for (srcb, srcR, dstT_, dR) in ((hi, hiR, hiT, hiTR), (lo, loR, loT, loTR)):
                    b0, b1 = nbank(), nbank()

                    def tr(e, srcb=srcb, b0=b0, b1=b1):
                        ins = None
                        for j in range(16):
                            bb = b0 if j < 8 else b1
                            ins = e.transpose(banks_bf[bb][:, (j % 8) * 128:(j % 8 + 1) * 128], srcb[:, j * 128:(j + 1) * 128], identb)
                        return ins
                    P.op("pe", tr, reads=[srcR], writes=[bankR[b0], bankR[b1]])
                    if STOP < 5.03:
                        continue
                    for hf_, bb in ((0, b0), (1, b1)):
                        pv = banks_bf[bb][:, 0:1024].rearrange("p (a b) -> p a b", a=8)
                        o = dstT_[:, hf_ * 8:(hf_ + 1) * 8, :]
                        P.op("act", lambda e, o=o, pv=pv: e.copy(out=o, in_=pv), reads=[bankR[bb]], writes=[dR])
                        if dstT_ is hiT:
                            o2 = h2T[:, hf_ * 8:(hf_ + 1) * 8, t * 128:(t + 1) * 128]
                            i1 = bc(g3[:, hf_ * 8:(hf_ + 1) * 8, :], [128, 8, 128])
                            P.op("dve", lambda e, o2=o2, o=o, i1=i1: e.tensor_tensor(out=o2, in0=o, in1=i1, op=ALU.mult),
                                 reads=[dR], writes=[])
                if STOP < 5.2:
                    continue
                br_ = nbank()
                lhs = [hiT[:, kc, :] for kc in range(16)] + [loT[:, kc, :] for kc in range(16)] + [hiT[:, kc, :] for kc in range(16)]
                rhs = [wrhi[:, kc, :] for kc in range(16)] + [wrhi[:, kc, :] for kc in range(16)] + [wrlo[:, kc, :] for kc in range(16)]
                mm_group(br_, (0, NR), lhs, rhs, [hiTR, loTR])
                lg = rs[:, 0:NR]
                gl = rs[:, 0:NG]
                el = rs[:, NG:NR]
                msk = rs[:, 128:128 + NE]
                ohg = rs[:, 256:256 + NG]
                oh1 = rs[:, 320:320 + NE]
                oh2 = rs[:, 384:384 + NE]
                sm = rs[:, 448:464]
                gmax, ngmax, sume, pg, m1, m2, dd, w1, w2, g1v, g2v = (sm[:, i:i + 1] for i in range(11))
                ejunk = rs[:, 464:464 + NG]

                def R(fn, eng="dve"):
                    P.op(eng, fn, reads=[rsR], writes=[rsR])
                P.op("dve", lambda e, br_=br_: e.tensor_tensor(out=lg, in0=banks[br_][:, 0:NR], in1=brt, op=ALU.add), reads=[bankR[br_], rsR], writes=[rsR])
                if STOP < 5.3:
                    continue
                R(lambda e: e.tensor_reduce(out=gmax, in_=gl, axis=AX.X, op=ALU.max))
                R(lambda e: e.tensor_scalar(out=ohg, in0=gl, scalar1=gmax, scalar2=None, op0=ALU.is_equal))
                R(lambda e: e.tensor_scalar(out=ngmax, in0=gmax, scalar1=-1.0, scalar2=None, op0=ALU.mult))
                if STOP < 5.4:
                    continue
                R(lambda e: e.activation(out=ejunk, in_=gl, func=AF.Exp, bias=ngmax, scale=1.0, accum_out=sume), "act")
                R(lambda e: e.reciprocal(out=pg, in_=sume))
                if STOP < 5.5:
                    continue
                R(lambda e: e.tensor_scalar(out=ohg, in0=ohg, scalar1=-1.0, scalar2=1e30, op0=ALU.add, op1=ALU.mult))
                el3 = el.rearrange("p (g c) -> p g c", g=NG)
                msk3 = msk.rearrange("p (g c) -> p g c", g=NG)
                ohg3 = ohg.rearrange("p (g o) -> p g o", o=1)
                R(lambda e: e.tensor_tensor(out=msk3, in0=el3, in1=bc(ohg3, [128, NG, EPG]), op=ALU.add))
                R(lambda e: e.tensor_reduce(out=m1, in_=msk, axis=AX.X, op=ALU.max))
                R(lambda e: e.tensor_scalar(out=oh1, in0=msk, scalar1=m1, scalar2=None, op0=ALU.is_equal))
                if STOP < 5.6:
                    continue
                R(lambda e: e.scalar_tensor_tensor(out=msk, in0=oh1, scalar=-1e30, in1=msk, op0=ALU.mult, op1=ALU.add))
                R(lambda e: e.tensor_reduce(out=m2, in_=msk, axis=AX.X, op=ALU.max))
                R(lambda e: e.tensor_scalar(out=oh2, in0=msk, scalar1=m2, scalar2=None, op0=ALU.is_equal))
                if STOP < 5.7:
                    continue
                R(lambda e: e.tensor_tensor(out=dd, in0=m2, in1=m1, op=ALU.subtract))
                R(lambda e: e.activation(out=dd, in_=dd, func=AF.Exp), "act")
                R(lambda e: e.tensor_scalar(out=w1, in0=dd, scalar1=1.0, scalar2=None, op0=ALU.add))
                R(lambda e: e.reciprocal(out=w1, in_=w1))
                R(lambda e: e.tensor_tensor(out=w2, in0=dd, in1=w1, op=ALU.mult))
                R(lambda e: e.tensor_tensor(out=g1v, in0=w1, in1=pg, op=ALU.mult))
                R(lambda e: e.tensor_tensor(out=g2v, in0=w2, in1=pg, op=ALU.mult))
                if STOP < 5.8:
                    continue
                Gt = G[:, t, :]
                R(lambda e: e.tensor_scalar(out=oh1, in0=oh1, scalar1=g1v, scalar2=None, op0=ALU.mult))
                P.op("dve", lambda e, Gt=Gt: e.scalar_tensor_tensor(out=Gt, in0=oh2, scalar=g2v, in1=oh1, op0=ALU.mult, op1=ALU.add),
                     reads=[rsR], writes=[GR])
            P.barrier()
            if dbg:
                dump(dbg_G, G.rearrange("p a b -> p (a b)"), "dbgG")
            if STOP <= 6:
                return
            actT = view(RC + 4096, [128, FFC, 1024], BF16)
            sil = [view(RC + 4096 + FFC * 2048 + i * 2048, [128, 512], F32) for i in range(2)]
            silR = [Res(), Res()]
            actR = Res()
            sc2 = [0]
            for ex in range(NE):
                for fb in range(NFB):
                    cgs = [wchunk(weg_d, ex * 16 + 0, fb * 256), wchunk(weg_d, ex * 16 + 8, fb * 256)]
                    cus = [wchunk(weu_d, ex * 16 + 0, fb * 256), wchunk(weu_d, ex * 16 + 8, fb * 256)]
                    for cb in range(2):
                        for tb in range(2):
                            tsl = slice(tb * 512, (tb + 1) * 512)
                            ba = nbank()
                            mm_group(ba, (0, 512), [cgs[kc // 8][0][:, kc % 8, cb * 128:(cb + 1) * 128] for kc in range(16)],
                                     [h2T[:, kc, tsl] for kc in range(16)], [cgs[0][1], cgs[1][1]])
                            si = sc2[0] % 2
                            sc2[0] += 1
                            sl = sil[si]
                            P.op("act", lambda e, sl=sl, ba=ba: e.activation(out=sl, in_=banks[ba][:, 0:512], func=AF.Silu),
                                 reads=[bankR[ba]], writes=[silR[si]])
                            bu = nbank()
                            mm_group(bu, (0, 512), [cus[kc // 8][0][:, kc % 8, cb * 128:(cb + 1) * 128] for kc in range(16)],
                                     [h2T[:, kc, tsl] for kc in range(16)], [cus[0][1], cus[1][1]])
                            o = actT[:, fb * 2 + cb, tsl]
                            P.op("dve", lambda e, o=o, sl=sl, bu=bu: e.tensor_tensor(out=o, in0=sl, in1=banks[bu][:, 0:512], op=ALU.mult),
                                 reads=[bankR[bu], silR[si]], writes=[actR])
                for cg in range(8):
                    cd = wchunk(wed_d, ex * FFC, cg * 256, nk=FFC)
                    for t in range(8):
                        b = nbank()
                        mm_group(b, (0, 256), [actT[:, kc, t * 128:(t + 1) * 128] for kc in range(FFC)],
                                 [cd[0][:, kc, 0:256] for kc in range(FFC)], [cd[1], actR])
                        o = yacc[:, t, cg * 256:(cg + 1) * 256]
                        gsc = G[:, t, ex:ex + 1]
                        P.op("dve", lambda e, o=o, b=b, gsc=gsc: e.scalar_tensor_tensor(out=o, in0=banks[b][:, 0:256], scalar=gsc, in1=o, op0=ALU.mult, op1=ALU.add),
                             reads=[bankR[b], GR], writes=[yR[t]])
            P.barrier()
            if STOP <= 7:
                return
            gfin = view(RC, [128, D], F32)
            ot = [view(RC + 8192 + i * 8192, [128, D], F32) for i in range(2)]
            junk7 = view(RC + 24576, [128, D], BF16)
            otR = [Res(), Res()]
            gfR, j7R = Res(), Res()
            P.op("sp", lambda e: e.dma_start(out=gfin, in_=gf_d.partition_broadcast(128)), writes=[gfR], dma="gfin")
            for t in range(8):
                yt = yacc[:, t, :]
                s = t % 2
                ssq, rt = stat[:, 20 + 2 * s:21 + 2 * s], stat[:, 21 + 2 * s:22 + 2 * s]
                P.op("act", lambda e, yt=yt, ssq=ssq: e.activation(out=junk7, in_=yt, func=AF.Square, accum_out=ssq), reads=[yR[t]], writes=[j7R, statR])
                P.op("act", lambda e, ssq=ssq, rt=rt: e.activation(out=rt, in_=ssq, func=AF.Sqrt, bias=EPS, scale=1.0 / D), reads=[statR], writes=[statR])
                P.op("dve", lambda e, rt=rt: e.reciprocal(out=rt, in_=rt), reads=[statR], writes=[statR])
                o = ot[s]
                P.op("dve", lambda e, o=o, yt=yt, rt=rt: e.scalar_tensor_tensor(out=o, in0=yt, scalar=rt, in1=gfin, op0=ALU.mult, op1=ALU.mult),
                     reads=[yR[t], statR, gfR], writes=[otR[s]])
                dst = out_d[xbase + t * 128: xbase + (t + 1) * 128, :]
                P.op("sp", lambda e, o=o, dst=dst: e.dma_start(out=dst, in_=o), reads=[otR[s]], dma="out%d" % s)
            P.barrier()

        setup()
        for ph in range(NPH if STOP > 0 else 0):
            prologue_half(ph)
        for hf in range(NH if STOP > 0 else 0):
            main_half(hf)
        P.barrier()
        P.op("sp", None)
        P.emit(nc, es)
    return nc


CFG = dict(tpc=2048, nph=14, ng=8, epg=8, de=1024)
N_CORES = 8


def make_consts():
    c = np.zeros((128, 1408), np.float32)
    p = np.arange(128)[:, None]
    f = np.arange(128)[None, :]
    c[:, 0:128] = (p == f)
    c[:, 128:256] = (f <= p)
    c[:, 256:384] = (f >= p)
    rm = np.ones((128, 1024), np.float32)
    rm[:, ::128] = 0.0
    c[:, 384:1408] = rm
    return c


def make_in_maps(cfg, n_cores, x, norm_mix_g, w_in, gmlp_ln_g, gmlp_ln_b, w_spatial, b_spatial, hgrn_lb_logits,
                 hgrn_norm_g, w_branch_a, w_branch_b, w_out, norm_ffn_g, w_router_group, b_router_group,
                 w_router_expert, b_router_expert, w_expert_gate, w_expert_up, w_expert_down, norm_final_g):
    f = lambda a: np.ascontiguousarray(np.asarray(a, dtype=np.float32))
    TPC = cfg["tpc"]
    NPH = cfg["nph"]
    NE = cfg["ng"] * cfg["epg"]
    DE = cfg["de"]
    xf = f(x).reshape(-1, D)
    shared = dict(
        norm_mix_g=f(norm_mix_g).reshape(D), w_in=f(w_in).reshape(D, 10240),
        gmlp_ln_g=f(gmlp_ln_g).reshape(1024), gmlp_ln_b=f(gmlp_ln_b).reshape(1024),
        w_spatial=f(w_spatial).reshape(8, 128, 128), b_spatial=f(b_spatial).reshape(1, 1024),
        hgrn_lb=f(hgrn_lb_logits).reshape(2, 1024), hgrn_norm_g=f(hgrn_norm_g).reshape(1024),
        w_branch_a=f(w_branch_a).reshape(1024, D), w_branch_b=f(w_branch_b).reshape(1024, D),
        w_out=f(w_out).reshape(D, D), norm_ffn_g=f(norm_ffn_g).reshape(D),
        w_router=np.ascontiguousarray(np.concatenate([f(w_router_group).reshape(D, -1), f(w_router_expert).reshape(D, -1)], axis=1)),
        b_router=np.ascontiguousarray(np.concatenate([f(b_router_group).reshape(-1), f(b_router_expert).reshape(-1)])),
        w_eg=f(w_expert_gate).reshape(NE * D, DE), w_eu=f(w_expert_up).reshape(NE * D, DE),
        w_ed=f(w_expert_down).reshape(NE * DE, D), norm_final_g=f(norm_final_g).reshape(D),
        consts=make_consts(),
    )
    maps = []
    npt = max(NPH, 1) * 1024
    for c in range(n_cores):
        m = dict(shared)
        m["x"] = xf[c * TPC:(c + 1) * TPC]
        xp = np.zeros((npt, D), np.float32)
        prev = xf[0:c * TPC]
        if NPH > 0 and prev.shape[0] > 0:
            xp[npt - prev.shape[0]:] = prev
        m["xprev"] = xp
        maps.append(m)
    return maps


def kernel(**inputs):
    nc = build(CFG)
    maps = make_in_maps(CFG, N_CORES, **inputs)
    res = run_bass_kernel_spmd(nc, maps, core_ids=list(range(N_CORES)))
    out = np.concatenate([r["out"] for r in res.results], axis=0)
    return out.reshape(1, N_CORES * CFG["tpc"], D).astype(np.float32)
```

```python
import numpy as np
from contextlib import ExitStack
import concourse.bass as bass
import concourse.mybir as mybir
from concourse.bass_utils import run_bass_kernel_spmd

F32 = mybir.dt.float32
BF16 = mybir.dt.bfloat16
AF = mybir.ActivationFunctionType
ALU = mybir.AluOpType
AX = mybir.AxisListType

D = 2048
KC = 16
EPS = 1e-6
ENGS = ("pe", "act", "dve", "pool", "sp")


class Res:
    __slots__ = ("lw", "rd", "name", "excl")

    def __init__(self, name="", excl=False):
        self.lw = None
        self.rd = []
        self.name = name
        self.excl = excl


class Op:
    __slots__ = ("eng", "fn", "waits", "dma", "needed", "val")

    def __init__(self, eng, fn, waits, dma):
        self.eng = eng
        self.fn = fn
        self.waits = waits
        self.dma = dma
        self.needed = False
        self.val = 0


class Prog:
    def __init__(self):
        self.q = {e: [] for e in ENGS}
        self.waited = {e: {} for e in ENGS}
        self.fence = {e: [] for e in ENGS}
        self.dma_cnt = {}
        self.last_dma = {}
        self.last_c = {}

    def op(self, eng, fn, reads=(), writes=(), dma=None, nofence=False, extra=()):
        deps = list(extra)
        ex = [r for r in reads if r.excl]
        if ex:
            reads = [r for r in reads if not r.excl]
            writes = list(writes) + ex
        for r in reads:
            if r.lw is not None:
                deps.append(r.lw)
        for w in writes:
            if w.lw is not None:
                deps.append(w.lw)
            deps.extend(w.rd)
        if not nofence and self.fence[eng]:
            deps.extend(self.fence[eng])
            self.fence[eng] = []
        deps.sort(key=lambda d: -d[2])
        waits = []
        wd = self.waited[eng]
        for d in deps:
            if d[0] == "c":
                _, pe_, pidx = d
                if pe_ == "pe" and eng == "pe":
                    continue
                key = ("c", pe_)
                if wd.get(key, -1) >= pidx:
                    continue
                wd[key] = pidx
                self.q[pe_][pidx].needed = True
                waits.append(d)
            else:
                _, stream, val = d
                key = ("d", stream)
                if wd.get(key, 0) >= val:
                    continue
                wd[key] = val
                waits.append(d)
        idx = len(self.q[eng])
        o = Op(eng, fn, waits, dma)
        if dma is not None:
            cnt = self.dma_cnt.get(dma, 0) + 1
            self.dma_cnt[dma] = cnt
            ev = ("d", dma, cnt * 16)
            self.last_dma[dma] = ev
        else:
            ev = ("c", eng, idx)
            self.last_c[eng] = ev
        self.q[eng].append(o)
        for r in reads:
            r.rd.append(ev)
        for w in writes:
            w.lw = ev
            w.rd = []
        return ev

    def barrier(self):
        evs = list(self.last_c.values()) + list(self.last_dma.values())
        for e in ENGS:
            self.fence[e] = list(evs)

    def emit(self, nc, es):
        for e in ENGS:
            c = 0
            for o in self.q[e]:
                if o.dma is None and o.needed:
                    c += 1
                    o.val = c
        sem_c = {e: es.enter_context(nc.semaphore("c_" + e)) for e in ENGS}
        sem_d = {s: es.enter_context(nc.semaphore("d_" + s)) for s in self.dma_cnt}
        q = self.q

        def run(name, e):
            for o in q[name]:
                for d in o.waits:
                    if d[0] == "c":
                        e.wait_ge(sem_c[d[1]], q[d[1]][d[2]].val)
                    else:
                        e.wait_ge(sem_d[d[1]], d[2])
                if o.fn is None:
                    continue
                ins = o.fn(e)
                if o.dma is not None:
                    ins.then_inc(sem_d[o.dma], 16)
                elif o.needed:
                    ins.then_inc(sem_c[name], 1)

        with nc.Block() as block:
            @block.tensor
            def _(e):
                run("pe", e)

            @block.scalar
            def _(e):
                run("act", e)

            @block.vector
            def _(e):
                run("dve", e)

            @block.gpsimd
            def _(e):
                run("pool", e)

            @block.sync
            def _(e):
                run("sp", e)


def bc(ap, shape):
    return ap.broadcast_to(list(shape))


def build(cfg):
    TPC = cfg["tpc"]
    NH = TPC // 1024
    NPH = cfg["nph"]
    NG, EPG, DE = cfg["ng"], cfg["epg"], cfg["de"]
    NE = NG * EPG
    NR = NG + NE
    FFC = DE // 128
    NFB = DE // 256

    nc = bass.Bass("TRN2", target_bir_lowering=False)

    def din(name, shape, dt=F32):
        return nc.dram_tensor(name, list(shape), dt, kind="ExternalInput").ap()

    x_d = din("x", [TPC, D])
    xp_d = din("xprev", [max(NPH, 1) * 1024, D])
    g1_d = din("norm_mix_g", [D])
    win_d = din("w_in", [D, 10240])
    lng_d = din("gmlp_ln_g", [1024])
    lnb_d = din("gmlp_ln_b", [1024])
    wsp_d = din("w_spatial", [8, 128, 128])
    bsp_d = din("b_spatial", [1, 1024])
    lbl_d = din("hgrn_lb", [2, 1024])
    hng_d = din("hgrn_norm_g", [1024])
    wpa_d = din("w_branch_a", [1024, D])
    wpb_d = din("w_branch_b", [1024, D])
    wout_d = din("w_out", [D, D])
    g2_d = din("norm_ffn_g", [D])
    wr_d = din("w_router", [D, NR])
    br_d = din("b_router", [NR])
    weg_d = din("w_eg", [NE * D, DE])
    weu_d = din("w_eu", [NE * D, DE])
    wed_d = din("w_ed", [NE * DE, D])
    gf_d = din("norm_final_g", [D])
    cst_d = din("consts", [128, 1648])
    out_d = nc.dram_tensor("out", [TPC, D], F32, kind="ExternalOutput").ap()
    DBG = cfg.get("dbg", False)
    STOP = cfg.get("stop", 99)
    NT = TPC // 128
    NA = 2 * NT
    NOV = NA
    dint = lambda n, sh, dt: nc.dram_tensor(n, list(sh), dt, kind="Internal").ap()
    x2s_d = dint("x2s", [TPC, D], F32)
    h2s_d = dint("h2s", [TPC, D], BF16)
    xsort_d = dint("xsort", [(NE + NOV) * 128, D], BF16)
    ysort_d = dint("ysort", [(NE + NOV) * 128, D], F32)
    if DBG:
        dout = lambda n, sh, dt: nc.dram_tensor(n, list(sh), dt, kind="ExternalOutput").ap()
        dbg_h = dout("dbg_h", [128, 16384], BF16)
        dbg_ya = dout("dbg_ya", [128, 8192], BF16)
        dbg_yb = dout("dbg_yb", [128, 8192], BF16)
        dbg_m = dout("dbg_m", [128, 16384], BF16)
        dbg_x2 = dout("dbg_x2", [1024, D], F32)
        dbg_G = dout("dbg_G", [128, 8 * NE], F32)
        dbg_S = dout("dbg_S", [128, 1024], F32)

    P = Prog()
    es = ExitStack()
    with es:
        ARENA_BYTES = 211968
        arena = es.enter_context(nc.sbuf_tensor("arena", [128, ARENA_BYTES // 4], F32))
        arena_bf = arena.bitcast(BF16)
        banks = [es.enter_context(nc.psum_tensor("bank%d" % i, [128, 512], F32)) for i in range(8)]
        banks_bf = [b.bitcast(BF16) for b in banks]
        bankR = [Res("bank%d" % i, excl=True) for i in range(8)]

        def view(off, shape, dt):
            n = 1
            for s in shape[1:]:
                n *= s
            if dt == F32:
                assert off % 4 == 0
                ap = arena[:, off // 4: off // 4 + n]
            else:
                assert off % 2 == 0
                ap = arena_bf[:, off // 2: off // 2 + n]
            if shape[0] != 128:
                ap = ap[0:shape[0], :]
            if len(shape) == 3:
                ap = ap.rearrange("p (a b) -> p a b", a=shape[1])
            elif len(shape) == 4:
                ap = ap.rearrange("p (a b c) -> p a b c", a=shape[1], b=shape[2])
            return ap

        cur = [0]

        def alloc(shape, dt, nbytes=None):
            n = 1
            for s in shape[1:]:
                n *= s
            sz = n * (4 if dt == F32 else 2)
            sz = (sz + 63) // 64 * 64
            off = cur[0]
            cur[0] += sz if nbytes is None else nbytes
            return view(off, shape, dt), off

        cst, _ = alloc([128, 1648], F32)
        ident_f = cst[:, 0:128]
        LT = cst[:, 128:256]
        UT = cst[:, 256:384]
        rmask = cst[:, 384:1408]
        SUT = cst[:, 1408:1536]
        identb, _ = alloc([128, 128], BF16)
        sutb, _ = alloc([128, 128], BF16)
        onesb, _ = alloc([128, 128], BF16)
        g1T, _ = alloc([128, 16], F32)
        g2T, _ = alloc([128, 16], F32)
        ngT, _ = alloc([128, 8], F32)
        lbT, _ = alloc([128, 8], F32)
        omlT, _ = alloc([128, 8], F32)
        brt, _ = alloc([128, NR], F32)
        bshi, _ = alloc([1, 1024], BF16)
        bslo, _ = alloc([1, 1024], BF16)
        wsT, _ = alloc([128, 8, 128], BF16)
        wrhi, _ = alloc([128, 16, NR], BF16)
        wrlo, _ = alloc([128, 16, NR], BF16)
        S_f, _ = alloc([128, 8, 128], F32)
        S_b, _ = alloc([128, 8, 128], BF16)
        Aj, _ = alloc([128, 8, 8], F32)
        stat, _ = alloc([128, 64], F32)
        SPARE_SZ = 12288
        _, SPARE = alloc([128, SPARE_SZ // 4], F32)
        NST, NRB = 2, 6
        stg = [alloc([128, 8, 256], F32)[0] for _ in range(NST)]
        ring = [alloc([128, 8, 256], BF16)[0] for _ in range(NRB)]
        stgR = [Res("stg%d" % i) for i in range(NST)]
        ringR = [Res("ring%d" % i) for i in range(NRB)]
        _, RA = alloc([128, 16384], F32)
        _, RB = alloc([128, 8192], F32)
        _, RC = alloc([128, 8192], F32)
        assert cur[0] <= ARENA_BYTES, cur[0]

        I32 = mybir.dt.int32
        rs = view(SPARE, [128, 512], F32)
        OH = view(SPARE + 2048, [128, NT, 2, NE], BF16)
        o_ = SPARE + 2048 + NT * 2 * NE * 2
        GT = view(o_, [128, NT, 2], F32)
        rank = view(o_ + 128, [128, NA], F32)
        desti = arena.bitcast(I32)[:, (o_ + 256) // 4:(o_ + 256) // 4 + NA]
        basev = view(o_ + 384, [128, NA], F32)
        osv = view(o_ + 512, [128, NA], F32)
        isov = view(o_ + 640, [128, NA], F32)
        cntall = view(o_ + 768, [128, NE], F32)
        opad = view(o_ + 1024, [128, NE], F32)
        oend = view(o_ + 1280, [128, NE], F32)
        ostart = view(o_ + 1536, [128, NE], F32)
        tmp64 = view(o_ + 1792, [128, NE], F32)
        ones64 = view(o_ + 2048, [128, NE], F32)
        obe = view(o_ + 2304, [128, NOV], F32)
        o2_ = o_ + 2304 + 128
        oig = arena.bitcast(I32)[:, o2_ // 4:o2_ // 4 + NOV * 16].rearrange("p (j k) -> p j k", k=16)
        o3_ = o2_ + NOV * 16 * 4
        oid = arena.bitcast(I32)[:, o3_ // 4:o3_ // 4 + NOV * FFC].rearrange("p (j k) -> p j k", k=FFC)
        assert o3_ + NOV * FFC * 4 <= SPARE + SPARE_SZ, (o3_ + NOV * FFC * 4 - SPARE)
        routeR, x2sR, h2sR, xsortR, ysortR = (Res() for _ in range(5))
        constR = Res("const")
        SR = Res("S")
        SbR = Res("Sb")
        statR = Res("stat")

        wctr = [0, 0]

        def wchunk(W2d, kc0, c0, nk=8, ncols=256):
            s = wctr[0] % NST
            b = wctr[1] % NRB
            wctr[0] += 1
            wctr[1] += 1
            src = W2d[kc0 * 128:(kc0 + nk) * 128, c0:c0 + ncols].rearrange("(kc p) c -> p kc c", p=128)
            sv = stg[s][:, 0:nk, 0:ncols]
            rv = ring[b][:, 0:nk, 0:ncols]
            P.op("sp", lambda e, sv=sv, src=src: e.dma_start(out=sv, in_=src),
                 writes=[stgR[s]], dma="st%d" % s, nofence=True)
            P.op("pool", lambda e, sv=sv, rv=rv: e.tensor_copy(out=rv, in_=sv),
                 reads=[stgR[s]], writes=[ringR[b]], nofence=True)
            return ring[b], ringR[b]

        bctr = [0]

        def nbank():
            b = bctr[0] % 8
            bctr[0] += 1
            return b

        def setup():
            tmpC_f = view(RC, [128, 16, NR], F32)
            tmpC_w = view(RC + 8192, [128, 8, 128], F32)
            tmpC_wb = view(RC + 8192 + 4096, [128, 8, 128], BF16)
            bsrow = view(RC + 16384, [1, 1024], F32)
            bstmp = view(RC + 16384 + 4096, [1, 1024], F32)
            l01 = view(RC + 30720, [128, 2, 8], F32)
            g2b = g2T.rearrange("p (a o) -> p a o", o=1)
            sp = "cst"

            def ld(dst, src, slow=False):
                if slow:
                    P.op("sp", lambda e: e.dma_start(out=dst, in_=src, allow_slow_non_contiguous=True),
                         writes=[constR], dma=sp)
                else:
                    P.op("sp", lambda e: e.dma_start(out=dst, in_=src), writes=[constR], dma=sp)

            ld(cst, cst_d[:, :])
            ld(g1T, g1_d.rearrange("(kc p) -> p kc", p=128), True)
            ld(g2T, g2_d.rearrange("(kc p) -> p kc", p=128), True)
            ld(ngT, hng_d.rearrange("(h v) -> v h", v=128), True)
            ld(l01, lbl_d.rearrange("l (h k) -> k l h", k=128), True)
            ld(brt, br_d.partition_broadcast(128))
            ld(bsrow, bsp_d[:, :])
            ld(tmpC_w, wsp_d.rearrange("g t s -> t g s"))
            ld(tmpC_f, wr_d.rearrange("(kc p) c -> p kc c", p=128))
            P.op("dve", lambda e: e.tensor_copy(out=identb, in_=ident_f), reads=[constR], writes=[constR])
            P.op("dve", lambda e: e.memset(onesb, 1.0), writes=[constR])
            P.op("dve", lambda e: e.tensor_copy(out=sutb, in_=SUT), reads=[constR], writes=[constR])
            P.op("dve", lambda e: e.memset(S_f, 0.0), writes=[SR])
            P.op("dve", lambda e: e.memset(S_b, 0.0), writes=[SbR])
            P.op("dve", lambda e: e.memset(Aj, 0.0), writes=[constR])
            P.op("dve", lambda e: e.tensor_tensor(out=lbT, in0=l01[:, 0, :], in1=l01[:, 1, :], op=ALU.subtract),
                 reads=[constR], writes=[constR])
            P.op("act", lambda e: e.activation(out=lbT, in_=lbT, func=AF.Sigmoid), reads=[constR], writes=[constR])
            P.op("dve", lambda e: e.tensor_scalar(out=omlT, in0=lbT, scalar1=-1.0, scalar2=1.0, op0=ALU.mult, op1=ALU.add),
                 reads=[constR], writes=[constR])
            P.op("dve", lambda e: e.tensor_copy(out=bshi, in_=bsrow), reads=[constR], writes=[constR])
            P.op("dve", lambda e: e.tensor_tensor(out=bstmp, in0=bsrow, in1=bshi, op=ALU.subtract), reads=[constR], writes=[constR])
            P.op("dve", lambda e: e.tensor_copy(out=bslo, in_=bstmp), reads=[constR], writes=[constR])
            P.op("dve", lambda e: e.tensor_tensor(out=tmpC_wb, in0=tmpC_w, in1=bc(LT.rearrange("p (o f) -> p o f", o=1), [128, 8, 128]), op=ALU.mult),
                 reads=[constR], writes=[constR])
            b = nbank()

            def tr8(e):
                ins = None
                for g in range(8):
                    ins = e.transpose(banks_bf[b][:, g * 128:(g + 1) * 128], tmpC_wb[:, g, :], identb)
                return ins
            P.op("pe", tr8, reads=[constR], writes=[bankR[b]])
            P.op("dve", lambda e: e.tensor_copy(out=wsT, in_=banks_bf[b][:, 0:1024].rearrange("p (g t) -> p g t", g=8)),
                 reads=[bankR[b]], writes=[constR])
            P.op("dve", lambda e: e.tensor_tensor(out=tmpC_f, in0=tmpC_f, in1=bc(g2b, [128, 16, NR]), op=ALU.mult),
                 reads=[constR], writes=[constR])
            P.op("dve", lambda e: e.tensor_copy(out=wrhi, in_=tmpC_f), reads=[constR], writes=[constR])
            tmp2 = view(RC + 24576, [128, 16, NR], F32) if 16 * NR * 4 <= 8192 else None
            P.op("dve", lambda e: e.tensor_tensor(out=tmp2, in0=tmpC_f, in1=wrhi, op=ALU.subtract), reads=[constR], writes=[constR])
            P.op("dve", lambda e: e.tensor_copy(out=wrlo, in_=tmp2), reads=[constR], writes=[constR])
            P.barrier()

        def norm_transpose(src_rows, gT, dstT, tmp_off, store_x=None):
            xin = [view(tmp_off + i * 8192, [128, D], F32) for i in range(2)]
            xs = [view(tmp_off + 16384 + i * 4096, [128, D], BF16) for i in range(2)]
            junk = view(tmp_off + 24576, [128, D], BF16)
            xinR = [Res(), Res()]
            xsR = [Res(), Res()]
            junkR = Res()
            g3 = gT.rearrange("p (a o) -> p a o", o=1)
            for t in range(8):
                s = t % 2
                xt = xin[s]
                src = src_rows(t)
                P.op("sp", lambda e, xt=xt, src=src: e.dma_start(out=xt, in_=src), writes=[xinR[s]], dma="xin%d" % s)
                ssq = stat[:, 2 * s:2 * s + 1]
                rt = stat[:, 2 * s + 1:2 * s + 2]
                P.op("act", lambda e, xt=xt, ssq=ssq: e.activation(out=junk, in_=xt, func=AF.Square, accum_out=ssq),
                     reads=[xinR[s]], writes=[junkR, statR])
                P.op("act", lambda e, ssq=ssq, rt=rt: e.activation(out=rt, in_=ssq, func=AF.Sqrt, bias=EPS, scale=1.0 / D),
                     reads=[statR], writes=[statR])
                P.op("dve", lambda e, rt=rt: e.reciprocal(out=rt, in_=rt), reads=[statR], writes=[statR])
                xst = xs[s]
                P.op("dve", lambda e, xt=xt, xst=xst, rt=rt: e.tensor_scalar(out=xst, in0=xt, scalar1=rt, scalar2=None, op0=ALU.mult),
                     reads=[xinR[s], statR], writes=[xsR[s]])
                b0, b1 = nbank(), nbank()

                def tr(e, xst=xst, b0=b0, b1=b1):
                    ins = None
                    for j in range(16):
                        bb = b0 if j < 8 else b1
                        ins = e.transpose(banks_bf[bb][:, (j % 8) * 128:(j % 8 + 1) * 128], xst[:, j * 128:(j + 1) * 128], identb)
                    return ins
                P.op("pe", tr, reads=[xsR[s]], writes=[bankR[b0], bankR[b1]])
                for hf, bb in ((0, b0), (1, b1)):
                    o = dstT[:, hf * 8:(hf + 1) * 8, t * 128:(t + 1) * 128]
                    i0 = banks_bf[bb][:, 0:1024].rearrange("p (a b) -> p a b", a=8)
                    i1 = bc(g3[:, hf * 8:(hf + 1) * 8, :], [128, 8, 128])
                    P.op("dve", lambda e, o=o, i0=i0, i1=i1: e.tensor_tensor(out=o, in0=i0, in1=i1, op=ALU.mult),
                         reads=[bankR[bb]], writes=[])

        def mm_group(b, bcols, lhs_list, rhs_list, reads):
            n = len(lhs_list)

            def fn(e):
                ins = None
                for i in range(n):
                    ins = e.matmul(banks[b][:, bcols[0]:bcols[1]], lhs_list[i], rhs_list[i], start=(i == 0), stop=(i == n - 1))
                return ins
            return P.op("pe", fn, reads=reads, writes=[bankR[b]])

        def hgrn_tile_common(fs_t, kT_t, T, TR):
            a = T["a"]
            P.op("dve", lambda e: e.tensor_scalar(out=kT_t, in0=fs_t, scalar1=-1.0, scalar2=1.0, op0=ALU.mult, op1=ALU.add),
                 reads=[TR["fs"]], writes=[TR["k"]])
            P.op("act", lambda e: e.activation(out=fs_t, in_=fs_t, func=AF.Ln), reads=[TR["fs"], TR["k"]], writes=[TR["fs"]])
            a2 = a.rearrange("p a b -> p (a b)")
            lf2 = fs_t.rearrange("p a b -> p (a b)")
            P.op("dve", lambda e: e.tensor_tensor_scan(out=a2, data0=rmask, data1=lf2, initial=0.0, op0=ALU.mult, op1=ALU.add),
                 reads=[TR["fs"]], writes=[TR["a"]])

        def hgrn_state_update(kT_t, itok_t, T, TR):
            a = T["a"]
            tA, tC, kdecT, kdtok, dS = T["tA"], T["tC"], T["kdecT"], T["kdtok"], T["dS"]
            aend = a[:, :, 127:128]
            P.op("dve", lambda e: e.tensor_tensor(out=tA, in0=bc(aend, [128, 8, 128]), in1=a, op=ALU.subtract),
                 reads=[TR["a"]], writes=[TR["tA"]])
            P.op("act", lambda e: e.activation(out=tC, in_=tA, func=AF.Exp), reads=[TR["tA"]], writes=[TR["tC"]])
            P.op("dve", lambda e: e.tensor_tensor(out=kdecT, in0=kT_t, in1=tC, op=ALU.mult),
                 reads=[TR["k"], TR["tC"]], writes=[TR["kdecT"]])
            P.op("act", lambda e: e.activation(out=dS, in_=aend, func=AF.Exp), reads=[TR["a"]], writes=[TR["dS"]])
            b = nbank()

            def tr(e):
                ins = None
                for h in range(8):
                    ins = e.transpose(banks_bf[b][:, h * 128:(h + 1) * 128], kdecT[:, h, :], identb)
                return ins
            P.op("pe", tr, reads=[TR["kdecT"]], writes=[bankR[b]])
            P.op("act", lambda e: e.copy(out=kdtok, in_=banks_bf[b][:, 0:1024]), reads=[bankR[b]], writes=[TR["kdtok"]])
            for hb in range(2):
                b2 = nbank()

                def mmB(e, hb=hb, b2=b2):
                    ins = None
                    for hq in range(4):
                        h = hb * 4 + hq
                        ins = e.matmul(banks[b2][:, hq * 128:(hq + 1) * 128], kdtok[:, h * 128:(h + 1) * 128],
                                       itok_t[:, h * 128:(h + 1) * 128], start=True, stop=True)
                    return ins
                P.op("pe", mmB, reads=[TR["kdtok"], TR["itok"]], writes=[bankR[b2]])
                Sv = S_f[:, hb * 4:(hb + 1) * 4, :]
                dSv = bc(dS[:, hb * 4:(hb + 1) * 4, :], [128, 4, 128])
                Bv = banks[b2][:, 0:512].rearrange("p (a b) -> p a b", a=4)
                P.op("dve", lambda e, Sv=Sv, dSv=dSv: e.tensor_tensor(out=Sv, in0=Sv, in1=dSv, op=ALU.mult),
                     reads=[TR["dS"]], writes=[SR])
                P.op("dve", lambda e, Sv=Sv, Bv=Bv: e.tensor_tensor(out=Sv, in0=Sv, in1=Bv, op=ALU.add),
                     reads=[bankR[b2]], writes=[SR])
            P.op("act", lambda e: e.copy(out=S_b, in_=S_f), reads=[SR], writes=[SbR])

        def hgrn_temps(base_offs):
            T, TR = {}, {}
            return T, TR

        hT = view(RB, [128, 16, 1024], BF16)
        yaT = view(RA, [128, 8, 1024], BF16)
        ybT = view(RA + 16384, [128, 8, 1024], BF16)
        mT = view(RC, [128, 16, 1024], BF16)
        yacc = view(RA, [128, 8, D], F32)

        def proj_fm(W2d, c0, actT, nkc, ntok_blocks, tokw, consume, tok0=0):
            chunks = []
            for k0 in range(0, nkc, 8):
                chunks.append(wchunk(W2d, k0, c0))
            for cb in range(2):
                for tb in range(ntok_blocks):
                    b = nbank()
                    lhs = [chunks[kc // 8][0][:, kc % 8, cb * 128:(cb + 1) * 128] for kc in range(nkc)]
                    rhs = [actT[:, kc, tok0 + tb * tokw: tok0 + (tb + 1) * tokw] for kc in range(nkc)]
                    mm_group(b, (0, tokw), lhs, rhs, [c[1] for c in chunks])
                    consume(cb, tb, b)

        def proj_tm(W2d, c0, actT, nkc, tiles, consume):
            chunks = []
            for k0 in range(0, nkc, 8):
                chunks.append(wchunk(W2d, k0, c0))
            for t in tiles:
                b = nbank()
                lhs = [actT[:, kc, t * 128:(t + 1) * 128] for kc in range(nkc)]
                rhs = [chunks[kc // 8][0][:, kc % 8, 0:256] for kc in range(nkc)]
                mm_group(b, (0, 256), lhs, rhs, [c[1] for c in chunks])
                consume(t, b)

        def hgrn_alloc(off_list):
            pools = [[o, o + s] for o, s in off_list]

            def take(shape, dt):
                n = 1
                for s_ in shape[1:]:
                    n *= s_
                sz = n * (4 if dt == F32 else 2)
                sz = (sz + 63) // 64 * 64
                for p in pools:
                    if p[1] - p[0] >= sz:
                        o = p[0]
                        p[0] += sz
                        return view(o, shape, dt)
                raise RuntimeError("hgrn temp alloc failed")
            return take

        def prologue_half(ph):
            norm_transpose(lambda t: xp_d[ph * 1024 + t * 128: ph * 1024 + (t + 1) * 128, :], g1T, hT, RA)
            P.barrier()
            take = hgrn_alloc([(RA, 65536), (RC, 32768)])
            fs = take([128, 2, 8, 128], F32)
            kT = take([128, 2, 8, 128], BF16)
            itok = take([128, 2, 1024], BF16)
            T = {"a": take([128, 8, 128], F32), "tA": take([128, 8, 128], F32), "tC": take([128, 8, 128], F32),
                 "kdecT": take([128, 8, 128], BF16), "kdtok": take([128, 1024], BF16), "dS": take([128, 8, 1], F32)}
            TR = {k: Res(k) for k in ("fs", "k", "a", "tA", "tC", "kdecT", "kdtok", "dS", "itok")}
            for tb4 in range(4):
                tok0 = tb4 * 256
                hgrn_project_f_i(tok0, fs, itok, TR)
                hgrn_f_affine(fs, TR)
                for tl in range(2):
                    hgrn_tile_common(fs[:, tl], kT[:, tl], T, TR)
                    hgrn_state_update(kT[:, tl], itok[:, tl, :], T, TR)
            P.barrier()


        def hgrn_project_f_i(tok0, fs, itok, TR, qT=None, sgn=None, sgtmp=None):
            sections = [("f", 3072)]
            if qT is not None:
                sections = [("q", 2048), ("f", 3072), ("g", 5120)]
            for name, cbase in sections:
                for cg in range(4):
                    def consume(cb, tb, b, name=name, cg=cg):
                        h = cg * 2 + cb
                        src = banks[b][:, 0:256].rearrange("p (t c) -> p t c", t=2)
                        if name == "f":
                            o = fs[:, :, h, :]
                            P.op("act", lambda e: e.activation(out=o, in_=src, func=AF.Sigmoid), reads=[bankR[b]], writes=[TR["fs"]])
                        elif name == "q":
                            o = qT[:, :, h, :]
                            P.op("act", lambda e: e.copy(out=o, in_=src), reads=[bankR[b]], writes=[TR["q"]])
                        else:
                            P.op("act", lambda e: e.activation(out=sgtmp, in_=banks[b][:, 0:256], func=AF.Silu),
                                 reads=[bankR[b]], writes=[TR["sgtmp"]])
                            o = sgn[:, :, h, :]
                            sv = sgtmp.rearrange("p (t c) -> p t c", t=2)
                            P.op("dve", lambda e: e.tensor_scalar(out=o, in0=sv, scalar1=ngT[:, h:h + 1], scalar2=None, op0=ALU.mult),
                                 reads=[TR["sgtmp"]], writes=[TR["sgn"]])
                    proj_fm(win_d, cbase + cg * 256, hT, 16, 1, 256, consume, tok0=tok0)
            for cg in range(4):
                def consume_i(t, b, cg=cg):
                    tl = t - tok0 // 128
                    o = itok[:, tl, cg * 256:(cg + 1) * 256]
                    P.op("act", lambda e: e.copy(out=o, in_=banks[b][:, 0:256]), reads=[bankR[b]], writes=[TR["itok"]])
                proj_tm(win_d, 4096 + cg * 256, hT, 16, [tok0 // 128, tok0 // 128 + 1], consume_i)

        def hgrn_f_affine(fs, TR):
            oml3 = omlT.rearrange("p (o h c) -> p o h c", o=1, c=1)
            lb3 = lbT.rearrange("p (o h c) -> p o h c", o=1, c=1)
            P.op("dve", lambda e: e.tensor_tensor(out=fs, in0=fs, in1=bc(oml3, [128, 2, 8, 128]), op=ALU.mult),
                 reads=[TR["fs"]], writes=[TR["fs"]])
            P.op("dve", lambda e: e.tensor_tensor(out=fs, in0=fs, in1=bc(lb3, [128, 2, 8, 128]), op=ALU.add),
                 reads=[TR["fs"]], writes=[TR["fs"]])

        def dump(dst, src, name):
            P.op("sp", lambda e: e.dma_start(out=dst, in_=src), dma=name)
            P.barrier()

        def main_half(hf):
            xbase = hf * 1024
            dbg = DBG and hf == 0
            if dbg:
                dump(dbg_S, S_f.rearrange("p a b -> p (a b)"), "dbgS")
            norm_transpose(lambda t: x_d[xbase + t * 128: xbase + (t + 1) * 128, :], g1T, hT, RA)
            P.barrier()
            if dbg:
                dump(dbg_h, hT.rearrange("p a b -> p (a b)"), "dbgh")
            if STOP <= 1:
                return
            vg = view(RC, [128, 8, 1024], F32)
            vln = view(RA + 32768, [128, 8, 1024], BF16)
            lnG = view(RA + 49152, [128, 1024], F32)
            lnB = view(RA + 53248, [128, 1024], F32)
            tmpv = view(RA + 57344, [128, 1024], F32)
            junkv = view(RA + 61440, [128, 1024], BF16)
            lnR, lnR2 = Res(), Res()
            P.op("sp", lambda e: e.dma_start(out=lnG, in_=lng_d.partition_broadcast(128)), writes=[lnR], dma="lng")
            P.op("sp", lambda e: e.dma_start(out=lnB, in_=lnb_d.partition_broadcast(128)), writes=[lnR2], dma="lnb")
            uR = Res()
            for cg in range(4):
                def cons_u(cb, tb, b, cg=cg):
                    o = yaT[:, cg * 2 + cb, tb * 512:(tb + 1) * 512]
                    P.op("act", lambda e: e.activation(out=o, in_=banks[b][:, 0:512], func=AF.Gelu_apprx_tanh),
                         reads=[bankR[b]], writes=[uR])
                proj_fm(win_d, cg * 256, hT, 16, 2, 512, cons_u)
            vgR = [Res() for _ in range(8)]
            for cg in range(4):
                def cons_v(t, b, cg=cg):
                    o = vg[:, t, cg * 256:(cg + 1) * 256]
                    P.op("act", lambda e: e.activation(out=o, in_=banks[b][:, 0:256], func=AF.Gelu_apprx_tanh),
                         reads=[bankR[b]], writes=[vgR[t]])
                proj_tm(win_d, 1024 + cg * 256, hT, 16, range(8), cons_v)
            tmpR, junkR2, vlnR = Res(), Res(), [Res() for _ in range(8)]
            yaR = Res()
            for t in range(8):
                vt = vg[:, t, :]
                s1, s2, mean, msq, var = (stat[:, 8 + i:9 + i] for i in range(5))
                P.op("dve", lambda e, vt=vt: e.tensor_reduce(out=s1, in_=vt, axis=AX.X, op=ALU.add), reads=[vgR[t]], writes=[statR])
                P.op("act", lambda e, vt=vt: e.activation(out=junkv, in_=vt, func=AF.Square, accum_out=s2),
                     reads=[vgR[t]], writes=[junkR2, statR])
                P.op("dve", lambda e: e.tensor_scalar(out=mean, in0=s1, scalar1=1.0 / 1024, scalar2=None, op0=ALU.mult), reads=[statR], writes=[statR])
                P.op("dve", lambda e: e.tensor_tensor(out=msq, in0=mean, in1=mean, op=ALU.mult), reads=[statR], writes=[statR])
                P.op("dve", lambda e: e.tensor_scalar(out=var, in0=s2, scalar1=1.0 / 1024, scalar2=msq, op0=ALU.mult, op1=ALU.subtract),
                     reads=[statR], writes=[statR])
                P.op("act", lambda e: e.activation(out=var, in_=var, func=AF.Sqrt, bias=EPS, scale=1.0), reads=[statR], writes=[statR])
                P.op("dve", lambda e: e.reciprocal(out=var, in_=var), reads=[statR], writes=[statR])
                P.op("dve", lambda e, vt=vt: e.tensor_scalar(out=tmpv, in0=vt, scalar1=mean, scalar2=var, op0=ALU.subtract, op1=ALU.mult),
                     reads=[vgR[t], statR], writes=[tmpR])
                P.op("dve", lambda e: e.tensor_tensor(out=tmpv, in0=tmpv, in1=lnG, op=ALU.mult), reads=[tmpR, lnR], writes=[tmpR])
                vl = vln[:, t, :]
                P.op("dve", lambda e, vl=vl: e.tensor_tensor(out=vl, in0=tmpv, in1=lnB, op=ALU.add), reads=[tmpR, lnR2], writes=[vlnR[t]])
                for gb in range(2):
                    b = nbank()

                    def sp_mm(e, t=t, gb=gb, b=b):
                        ins = None
                        for gq in range(4):
                            g = gb * 4 + gq
                            o = banks[b][:, gq * 128:(gq + 1) * 128]
                            e.matmul(o, vln[:, t, g * 128:(g + 1) * 128], wsT[:, g, :], start=True, stop=False)
                            e.matmul(o, onesb[0:1, :], bshi[0:1, g * 128:(g + 1) * 128], start=False, stop=False)
                            ins = e.matmul(o, onesb[0:1, :], bslo[0:1, g * 128:(g + 1) * 128], start=False, stop=True)
                        return ins
                    P.op("pe", sp_mm, reads=[vlnR[t]], writes=[bankR[b]])
                    o = yaT[:, gb * 4:(gb + 1) * 4, t * 128:(t + 1) * 128]
                    pv = banks[b][:, 0:512].rearrange("p (a b) -> p a b", a=4)
                    P.op("dve", lambda e, o=o, pv=pv: e.tensor_tensor(out=o, in0=pv, in1=o, op=ALU.mult),
                         reads=[bankR[b], uR], writes=[yaR])
            P.barrier()
            if dbg:
                dump(dbg_ya, yaT.rearrange("p a b -> p (a b)"), "dbgya")
            if STOP <= 2:
                return
            take = hgrn_alloc([(RA + 32768, 32768), (RC, 32768)])
            fs = take([128, 2, 8, 128], F32)
            kT = take([128, 2, 8, 128], BF16)
            qT = take([128, 2, 8, 128], BF16)
            sgn = take([128, 2, 8, 128], BF16)
            itok = take([128, 2, 1024], BF16)
            sgtmp = take([128, 256], F32)
            T = {"a": take([128, 8, 128], F32), "tA": take([128, 8, 128], F32), "tB": take([128, 8, 128], F32),
                 "tC": take([128, 8, 128], F32), "qd": take([128, 8, 128], BF16), "qdt": take([128, 8, 128], BF16),
                 "kinv": take([128, 8, 128], BF16), "kdecT": take([128, 8, 128], BF16), "kdtok": take([128, 1024], BF16),
                 "dS": take([128, 8, 1], F32), "Ef": take([128, 8, 8, 8], F32), "E": take([128, 8, 8, 8], BF16),
                 "kk0": take([128, 8, 8, 16], BF16), "kk1": take([128, 8, 8, 16], BF16),
                 "scm": take([128, 8, 128], BF16), "osq": take([128, 8, 128], BF16)}
            TR = {k: Res(k) for k in list(T.keys()) + ["fs", "k", "q", "sgn", "sgtmp", "itok", "scm0", "scm1", "osq0", "osq1"]}
            ybR = Res()
            for tb4 in range(4):
                tok0 = tb4 * 256
                hgrn_project_f_i(tok0, fs, itok, TR, qT=qT, sgn=sgn, sgtmp=sgtmp)
                hgrn_f_affine(fs, TR)
                for tl in range(2):
                    tokt = tok0 + tl * 128
                    fs_t, kT_t, qT_t, sgn_t, itok_t = fs[:, tl], kT[:, tl], qT[:, tl], sgn[:, tl], itok[:, tl, :]
                    hgrn_tile_common(fs_t, kT_t, T, TR)
                    a, tA, tB, tC = T["a"], T["tA"], T["tB"], T["tC"]
                    a4 = a.rearrange("p h (j r) -> p h j r", r=16)
                    Aj4 = Aj.rearrange("p h (j o) -> p h j o", o=1)
                    P.op("dve", lambda e, a4=a4, Aj4=Aj4: e.tensor_copy(out=Aj4[:, :, 1:8, :], in_=a4[:, :, 0:7, 15:16]),
                         reads=[TR["a"]], writes=[TR["Ef"]])
                    tA4 = tA.rearrange("p h (j r) -> p h j r", r=16)
                    P.op("dve", lambda e, a4=a4, Aj4=Aj4, tA4=tA4: e.tensor_tensor(out=tA4, in0=a4, in1=bc(Aj4, [128, 8, 8, 16]), op=ALU.subtract),
                         reads=[TR["a"], TR["Ef"]], writes=[TR["tA"]])
                    P.op("act", lambda e, tA=tA, tB=tB: e.activation(out=tB, in_=tA, func=AF.Exp), reads=[TR["tA"]], writes=[TR["tB"]])
                    P.op("dve", lambda e, qT_t=qT_t, tB=tB: e.tensor_tensor(out=T["qd"], in0=qT_t, in1=tB, op=ALU.mult),
                         reads=[TR["q"], TR["tB"]], writes=[TR["qd"]])
                    P.op("act", lambda e, tA=tA, tC=tC: e.activation(out=tC, in_=tA, func=AF.Exp, scale=-1.0), reads=[TR["tA"]], writes=[TR["tC"]])
                    P.op("dve", lambda e, kT_t=kT_t, tC=tC: e.tensor_tensor(out=T["kinv"], in0=kT_t, in1=tC, op=ALU.mult),
                         reads=[TR["k"], TR["tC"]], writes=[TR["kinv"]])
                    P.op("act", lambda e, a=a, tB=tB: e.activation(out=tB, in_=a, func=AF.Exp), reads=[TR["a"]], writes=[TR["tB"]])
                    P.op("dve", lambda e, qT_t=qT_t, tB=tB: e.tensor_tensor(out=T["qdt"], in0=qT_t, in1=tB, op=ALU.mult),
                         reads=[TR["q"], TR["tB"]], writes=[TR["qdt"]])
                    Ef, Eb = T["Ef"], T["E"]
                    Ajj = Aj.rearrange("p h (j o) -> p h j o", o=1)
                    Ajc = Aj.rearrange("p h (o c) -> p h o c", o=1)
                    P.op("dve", lambda e, Ef=Ef: e.tensor_tensor(out=Ef, in0=bc(Ajj, [128, 8, 8, 8]), in1=bc(Ajc, [128, 8, 8, 8]), op=ALU.subtract),
                         reads=[TR["Ef"]], writes=[TR["E"]])
                    P.op("dve", lambda e, Ef=Ef: e.tensor_scalar(out=Ef, in0=Ef, scalar1=0.0, scalar2=None, op0=ALU.min),
                         reads=[TR["E"]], writes=[TR["E"]])
                    P.op("act", lambda e, Ef=Ef, Eb=Eb: e.activation(out=Eb, in_=Ef, func=AF.Exp), reads=[TR["E"]], writes=[TR["E"]])
                    obanks = []
                    for hb in range(2):
                        bs_ = nbank()
                        for hq in range(4):
                            h = hb * 4 + hq
                            kk = T["kk%d" % (h % 2)]
                            kkR = TR["kk%d" % (h % 2)]
                            kin = T["kinv"][:, h, :].rearrange("p (o c r) -> p o c r", o=1, r=16)
                            Eh = Eb[:, h].rearrange("p j (c o) -> p j c o", o=1)
                            P.op("dve", lambda e, kk=kk, kin=kin, Eh=Eh: e.tensor_tensor(out=kk, in0=bc(kin, [128, 8, 8, 16]), in1=bc(Eh, [128, 8, 8, 16]), op=ALU.mult),
                                 reads=[TR["kinv"], TR["E"]], writes=[kkR])

                            def sc_mm(e, kk=kk, h=h, hq=hq, bs_=bs_):
                                ins = None
                                for j in range(8):
                                    ins = e.matmul(banks[bs_][:, hq * 128 + j * 16: hq * 128 + (j + 1) * 16],
                                                   kk[:, j].rearrange("p c r -> p (c r)"), T["qd"][:, h, j * 16:(j + 1) * 16],
                                                   start=True, stop=True)
                                return ins
                            P.op("pe", sc_mm, reads=[kkR, TR["qd"]], writes=[bankR[bs_]])
                        scm = T["scm"][:, hb * 4:(hb + 1) * 4, :]
                        scR = TR["scm%d" % hb]
                        UT3 = bc(UT.rearrange("p (o f) -> p o f", o=1), [128, 4, 128])
                        sv = banks[bs_][:, 0:512].rearrange("p (a b) -> p a b", a=4)
                        P.op("dve", lambda e, scm=scm, sv=sv, UT3=UT3: e.tensor_tensor(out=scm, in0=sv, in1=UT3, op=ALU.mult),
                             reads=[bankR[bs_]], writes=[scR])
                        bo = nbank()
                        obanks.append(bo)

                        def o_mm(e, hb=hb, bo=bo, itok_t=itok_t):
                            ins = None
                            for hq in range(4):
                                h = hb * 4 + hq
                                o = banks[bo][:, hq * 128:(hq + 1) * 128]
                                e.matmul(o, itok_t[:, h * 128:(h + 1) * 128], T["scm"][:, h, :], start=True, stop=False)
                                ins = e.matmul(o, S_b[:, h, :], T["qdt"][:, h, :], start=False, stop=True)
                            return ins
                        P.op("pe", o_mm, reads=[scR, TR["itok"], SbR, TR["qdt"]], writes=[bankR[bo]])
                    hgrn_state_update(kT_t, itok_t, T, TR)
                    for hb in range(2):
                        bo = obanks[hb]
                        osq = T["osq"][:, hb * 4:(hb + 1) * 4, :]
                        oR = TR["osq%d" % hb]
                        ov = banks[bo][:, 0:512].rearrange("p (a b) -> p a b", a=4)
                        P.op("act", lambda e, osq=osq, ov=ov: e.activation(out=osq, in_=ov, func=AF.Square), reads=[bankR[bo]], writes=[oR])
                        bq = nbank()
                        mm_group(bq, (0, 512), [onesb], [osq.rearrange("p a b -> p (a b)")], [oR])
                        tq = (tA if hb == 0 else tC)[:, 0:4, :]
                        tqR = TR["tA"] if hb == 0 else TR["tC"]
                        qv = banks[bq][:, 0:512].rearrange("p (a b) -> p a b", a=4)
                        P.op("act", lambda e, tq=tq, qv=qv: e.activation(out=tq, in_=qv, func=AF.Sqrt, bias=EPS, scale=1.0 / 128),
                             reads=[bankR[bq]], writes=[tqR])
                        P.op("dve", lambda e, tq=tq: e.reciprocal(out=tq, in_=tq), reads=[tqR], writes=[tqR])
                        P.op("dve", lambda e, tq=tq, ov=ov: e.tensor_tensor(out=tq, in0=ov, in1=tq, op=ALU.mult),
                             reads=[bankR[bo], tqR], writes=[tqR])
                        yo = ybT[:, hb * 4:(hb + 1) * 4, tokt:tokt + 128]
                        sg4 = sgn_t[:, hb * 4:(hb + 1) * 4, :]
                        P.op("dve", lambda e, yo=yo, tq=tq, sg4=sg4: e.tensor_tensor(out=yo, in0=tq, in1=sg4, op=ALU.mult),
                             reads=[tqR, TR["sgn"]], writes=[ybR])
            P.barrier()
            if dbg:
                dump(dbg_yb, ybT.rearrange("p a b -> p (a b)"), "dbgyb")
            if STOP <= 3:
                return
            sA = [view(RA + 32768 + i * 2048, [128, 512], F32) for i in range(4)]
            sAR = [Res() for _ in range(4)]
            sctr = [0]
            for cg in range(8):
                parts = []
                for (goff, Wp, yT) in ((6144, wpa_d, yaT), (8192, wpb_d, ybT)):
                    cw = [wchunk(win_d, 0, goff + cg * 256), wchunk(win_d, 8, goff + cg * 256)]
                    cp = wchunk(Wp, 0, cg * 256)
                    parts.append((cw, cp, yT))
                for cb in range(2):
                    for tb in range(2):
                        tsl = slice(tb * 512, (tb + 1) * 512)
                        prods = []
                        for (cw, cp, yT) in parts:
                            bg = nbank()
                            mm_group(bg, (0, 512), [cw[kc // 8][0][:, kc % 8, cb * 128:(cb + 1) * 128] for kc in range(16)],
                                     [hT[:, kc, tsl] for kc in range(16)], [cw[0][1], cw[1][1]])
                            si = sctr[0] % 4
                            sctr[0] += 1
                            sg_ = sA[si]
                            P.op("act", lambda e, sg_=sg_, bg=bg: e.activation(out=sg_, in_=banks[bg][:, 0:512], func=AF.Sigmoid),
                                 reads=[bankR[bg]], writes=[sAR[si]])
                            bp = nbank()
                            mm_group(bp, (0, 512), [cp[0][:, kc, cb * 128:(cb + 1) * 128] for kc in range(8)],
                                     [yT[:, kc, tsl] for kc in range(8)], [cp[1]])
                            P.op("dve", lambda e, sg_=sg_, bp=bp: e.tensor_tensor(out=sg_, in0=sg_, in1=banks[bp][:, 0:512], op=ALU.mult),
                                 reads=[bankR[bp], sAR[si]], writes=[sAR[si]])
                            prods.append((sg_, sAR[si]))
                        o = mT[:, cg * 2 + cb, tsl]
                        P.op("dve", lambda e, o=o, p0=prods[0][0], p1=prods[1][0]: e.tensor_tensor(out=o, in0=p0, in1=p1, op=ALU.add),
                             reads=[prods[0][1], prods[1][1]], writes=[])
            P.barrier()
            if dbg:
                dump(dbg_m, mT.rearrange("p a b -> p (a b)"), "dbgm")
            if STOP <= 4:
                return
            yR = [Res() for _ in range(8)]
            for t in range(8):
                yt = yacc[:, t, :]
                src = x_d[xbase + t * 128: xbase + (t + 1) * 128, :]
                P.op("sp", lambda e, yt=yt, src=src: e.dma_start(out=yt, in_=src), writes=[yR[t]], dma="xres%d" % t)
            for cg in range(8):
                def cons_o(t, b, cg=cg):
                    o = yacc[:, t, cg * 256:(cg + 1) * 256]
                    P.op("dve", lambda e: e.tensor_tensor(out=o, in0=o, in1=banks[b][:, 0:256], op=ALU.add),
                         reads=[bankR[b]], writes=[yR[t]])
                proj_tm(wout_d, cg * 256, mT, 16, range(8), cons_o)
            P.barrier()
            if dbg:
                dump(dbg_x2.rearrange("(t p) d -> p t d", p=128), yacc, "dbgx2")
            if STOP <= 5:
                return
            xs32 = view(RC + 4096, [128, D], F32)
            hi = view(RC + 12288, [128, D], BF16)
            lo = view(RC + 16384, [128, D], BF16)
            junk = view(RC + 20480, [128, D], BF16)
            hiT = view(RC + 24576, [128, 16, 128], BF16)
            loT = view(RC + 28672, [128, 16, 128], BF16)
            g2b = view(RB, [128, D], F32)
            h2row = [view(RB + 8192 + i * 4096, [128, D], BF16) for i in range(2)]
            h2R = [Res(), Res()]
            g2bR = Res()
            xsR, hiR, loR, jkR, hiTR, loTR, rsR = (Res() for _ in range(7))
            P.op("sp", lambda e: e.dma_start(out=g2b, in_=g2_d.partition_broadcast(128)), writes=[g2bR], dma="g2b")
            for t in range(8):
                gt = hf * 8 + t
                yt = yacc[:, t, :]
                ssq, rt = stat[:, 16:17], stat[:, 17:18]
                P.op("act", lambda e, yt=yt: e.activation(out=junk, in_=yt, func=AF.Square, accum_out=ssq), reads=[yR[t]], writes=[jkR, statR])
                P.op("act", lambda e: e.activation(out=rt, in_=ssq, func=AF.Sqrt, bias=EPS, scale=1.0 / D), reads=[statR], writes=[statR])
                P.op("dve", lambda e: e.reciprocal(out=rt, in_=rt), reads=[statR], writes=[statR])
                P.op("dve", lambda e, yt=yt: e.tensor_scalar(out=xs32, in0=yt, scalar1=rt, scalar2=None, op0=ALU.mult),
                     reads=[yR[t], statR], writes=[xsR])
                dstx = x2s_d[xbase + t * 128: xbase + (t + 1) * 128, :]
                P.op("sp", lambda e, yt=yt, dstx=dstx: e.dma_start(out=dstx, in_=yt), reads=[yR[t]], writes=[x2sR], dma="x2s")
                P.op("dve", lambda e: e.tensor_copy(out=hi, in_=xs32), reads=[xsR], writes=[hiR])
                P.op("dve", lambda e: e.tensor_tensor(out=xs32, in0=xs32, in1=hi, op=ALU.subtract), reads=[xsR, hiR], writes=[xsR])
                P.op("dve", lambda e: e.tensor_copy(out=lo, in_=xs32), reads=[xsR], writes=[loR])
                s_ = t % 2
                hr = h2row[s_]
                P.op("dve", lambda e, hr=hr: e.tensor_tensor(out=hr, in0=hi, in1=g2b, op=ALU.mult), reads=[hiR, g2bR], writes=[h2R[s_]])
                dsth = h2s_d[xbase + t * 128: xbase + (t + 1) * 128, :]
                P.op("sp", lambda e, hr=hr, dsth=dsth: e.dma_start(out=dsth, in_=hr), reads=[h2R[s_]], writes=[h2sR], dma="h2s")
                for (srcb, srcR, dstT_, dR) in ((hi, hiR, hiT, hiTR), (lo, loR, loT, loTR)):
                    b0, b1 = nbank(), nbank()

                    def tr(e, srcb=srcb, b0=b0, b1=b1):
                        ins = None
                        for j in range(16):
                            bb = b0 if j < 8 else b1
                            ins = e.transpose(banks_bf[bb][:, (j % 8) * 128:(j % 8 + 1) * 128], srcb[:, j * 128:(j + 1) * 128], identb)
                        return ins
                    P.op("pe", tr, reads=[srcR], writes=[bankR[b0], bankR[b1]])
                    for hf_, bb in ((0, b0), (1, b1)):
                        pv = banks_bf[bb][:, 0:1024].rearrange("p (a b) -> p a b", a=8)
                        o = dstT_[:, hf_ * 8:(hf_ + 1) * 8, :]
                        P.op("act", lambda e, o=o, pv=pv: e.copy(out=o, in_=pv), reads=[bankR[bb]], writes=[dR])
                br_ = nbank()
                lhs = [hiT[:, kc, :] for kc in range(16)] + [loT[:, kc, :] for kc in range(16)] + [hiT[:, kc, :] for kc in range(16)]
                rhs = [wrhi[:, kc, :] for kc in range(16)] + [wrhi[:, kc, :] for kc in range(16)] + [wrlo[:, kc, :] for kc in range(16)]
                mm_group(br_, (0, NR), lhs, rhs, [hiTR, loTR])
                lg = rs[:, 0:NR]
                gl = rs[:, 0:NG]
                el = rs[:, NG:NR]
                msk = rs[:, 128:128 + NE]
                ohg = rs[:, 256:256 + NG]
                oh1 = rs[:, 320:320 + NE]
                oh2 = rs[:, 384:384 + NE]
                sm = rs[:, 448:464]
                gmax, ngmax, sume, pg, m1, m2, dd, w1, w2 = (sm[:, i:i + 1] for i in range(9))
                ejunk = rs[:, 464:464 + NG]

                def R(fn, eng="dve"):
                    P.op(eng, fn, reads=[rsR], writes=[rsR])
                P.op("dve", lambda e, br_=br_: e.tensor_tensor(out=lg, in0=banks[br_][:, 0:NR], in1=brt, op=ALU.add), reads=[bankR[br_], rsR], writes=[rsR])
                R(lambda e: e.tensor_reduce(out=gmax, in_=gl, axis=AX.X, op=ALU.max))
                R(lambda e: e.tensor_scalar(out=ohg, in0=gl, scalar1=gmax, scalar2=None, op0=ALU.is_equal))
                R(lambda e: e.tensor_scalar(out=ngmax, in0=gmax, scalar1=-1.0, scalar2=None, op0=ALU.mult))
                R(lambda e: e.activation(out=ejunk, in_=gl, func=AF.Exp, bias=ngmax, scale=1.0, accum_out=sume), "act")
                R(lambda e: e.reciprocal(out=pg, in_=sume))
                R(lambda e: e.tensor_scalar(out=ohg, in0=ohg, scalar1=-1.0, scalar2=1e30, op0=ALU.add, op1=ALU.mult))
                el3 = el.rearrange("p (g c) -> p g c", g=NG)
                msk3 = msk.rearrange("p (g c) -> p g c", g=NG)
                ohg3 = ohg.rearrange("p (g o) -> p g o", o=1)
                R(lambda e: e.tensor_tensor(out=msk3, in0=el3, in1=bc(ohg3, [128, NG, EPG]), op=ALU.add))
                R(lambda e: e.tensor_reduce(out=m1, in_=msk, axis=AX.X, op=ALU.max))
                R(lambda e: e.tensor_scalar(out=oh1, in0=msk, scalar1=m1, scalar2=None, op0=ALU.is_equal))
                R(lambda e: e.scalar_tensor_tensor(out=msk, in0=oh1, scalar=-1e30, in1=msk, op0=ALU.mult, op1=ALU.add))
                R(lambda e: e.tensor_reduce(out=m2, in_=msk, axis=AX.X, op=ALU.max))
                R(lambda e: e.tensor_scalar(out=oh2, in0=msk, scalar1=m2, scalar2=None, op0=ALU.is_equal))
                R(lambda e: e.tensor_tensor(out=dd, in0=m2, in1=m1, op=ALU.subtract))
                R(lambda e: e.activation(out=dd, in_=dd, func=AF.Exp), "act")
                R(lambda e: e.tensor_scalar(out=w1, in0=dd, scalar1=1.0, scalar2=None, op0=ALU.add))
                R(lambda e: e.reciprocal(out=w1, in_=w1))
                R(lambda e: e.tensor_tensor(out=w2, in0=dd, in1=w1, op=ALU.mult))
                gv0, gv1 = GT[:, gt, 0:1], GT[:, gt, 1:2]
                P.op("dve", lambda e, gv0=gv0: e.tensor_tensor(out=gv0, in0=w1, in1=pg, op=ALU.mult), reads=[rsR], writes=[routeR])
                P.op("dve", lambda e, gv1=gv1: e.tensor_tensor(out=gv1, in0=w2, in1=pg, op=ALU.mult), reads=[rsR], writes=[routeR])
                o0, o1 = OH[:, gt, 0, :], OH[:, gt, 1, :]
                P.op("dve", lambda e, o0=o0: e.tensor_copy(out=o0, in_=oh1), reads=[rsR], writes=[routeR])
                P.op("dve", lambda e, o1=o1: e.tensor_copy(out=o1, in_=oh2), reads=[rsR], writes=[routeR])
            P.barrier()

        bregs = {}

        def breg(e, val):
            if val not in bregs:
                bregs[val] = e.to_reg(val)
            return bregs[val]

        zf_ev = [None]

        def zero_fill_xsort():
            z = view(RA, [128, D], BF16)
            zR = Res()
            P.op("dve", lambda e: e.memset(z, 0.0), writes=[zR])
            for blk in range(NE + NOV):
                rows = xsort_d[blk * 128:(blk + 1) * 128, :]
                zf_ev[0] = P.op("sp", lambda e, rows=rows: e.dma_start(out=rows, in_=z), reads=[zR], dma="zf")
            P.barrier()

        def moe_sorted():
            OS = view(RC, [128, NT, NE], BF16)
            big = view(RC + 4096, [128, NA, NE], F32)
            cmpb = view(RC + 4096 + NA * NE * 4, [128, NOV, NE], F32)
            tmpi = view(RC + 4096 + NA * NE * 4 + NOV * NE * 4, [128, NOV, 16], F32)
            mR = Res()

            def M(fn, eng="dve", extra_r=()):
                P.op(eng, fn, reads=[mR, routeR] + list(extra_r), writes=[mR])
            M(lambda e: e.tensor_tensor(out=OS, in0=OH[:, :, 0, :], in1=OH[:, :, 1, :], op=ALU.add))
            M(lambda e: e.memset(ones64, 1.0))
            for t in range(NT):
                b = nbank()
                lhs = [sutb] + [onesb] * t
                rhs = [OS[:, t, :]] + [OS[:, t2, :] for t2 in range(t)]
                mm_group(b, (0, NE), lhs, rhs, [mR])
                for k in range(2):
                    ohk = OH[:, t, k, :]
                    rk = rank[:, 2 * t + k: 2 * t + k + 1]
                    P.op("dve", lambda e, b=b, ohk=ohk: e.tensor_tensor(out=tmp64, in0=banks[b][:, 0:NE], in1=ohk, op=ALU.mult),
                         reads=[bankR[b], mR, routeR], writes=[mR])
                    M(lambda e, rk=rk: e.tensor_reduce(out=rk, in_=tmp64, axis=AX.X, op=ALU.add))
            b = nbank()
            mm_group(b, (0, NE), [onesb] * NT, [OS[:, t, :] for t in range(NT)], [mR])
            P.op("dve", lambda e, b=b: e.tensor_copy(out=cntall, in_=banks[b][:, 0:NE]), reads=[bankR[b], mR], writes=[mR])
            M(lambda e: e.tensor_scalar(out=opad, in0=cntall, scalar1=-128.0, scalar2=0.0, op0=ALU.add, op1=ALU.max))
            cmpa = cmpb.rearrange("p j e -> p (j e)").rearrange("p (e j) -> p e j", j=NOV)
            ov3 = opad.rearrange("p (e o) -> p e o", o=1)
            th3 = cst[:, 1600:1600 + NOV].rearrange("p (o j) -> p o j", o=1)
            M(lambda e: e.tensor_tensor(out=cmpa, in0=bc(ov3, [128, NE, NOV]), in1=bc(th3, [128, NE, NOV]), op=ALU.is_gt))
            M(lambda e: e.tensor_reduce(out=tmp64, in_=cmpa, axis=AX.X, op=ALU.add))
            M(lambda e: e.tensor_scalar(out=opad, in0=tmp64, scalar1=128.0, scalar2=None, op0=ALU.mult))
            M(lambda e: e.tensor_tensor_scan(out=oend, data0=ones64, data1=opad, initial=0.0, op0=ALU.mult, op1=ALU.add))
            M(lambda e: e.tensor_tensor(out=ostart, in0=oend, in1=opad, op=ALU.subtract))
            OHf = OH.rearrange("p t k e -> p (t k) e")
            e128 = cst[:, 1536:1536 + NE].rearrange("p (o e) -> p o e", o=1)
            os3 = ostart.rearrange("p (o e) -> p o e", o=1)
            M(lambda e: e.tensor_tensor(out=big, in0=OHf, in1=bc(e128, [128, NA, NE]), op=ALU.mult))
            M(lambda e: e.tensor_reduce(out=basev, in_=big, axis=AX.X, op=ALU.add))
            M(lambda e: e.tensor_tensor(out=big, in0=OHf, in1=bc(os3, [128, NA, NE]), op=ALU.mult))
            M(lambda e: e.tensor_reduce(out=osv, in_=big, axis=AX.X, op=ALU.add))
            M(lambda e: e.tensor_scalar(out=isov, in0=rank, scalar1=128.0, scalar2=None, op0=ALU.is_ge))
            M(lambda e: e.tensor_tensor(out=osv, in0=osv, in1=basev, op=ALU.subtract))
            M(lambda e: e.tensor_scalar(out=osv, in0=osv, scalar1=float(NE * 128 - 128), scalar2=None, op0=ALU.add))
            M(lambda e: e.tensor_tensor(out=osv, in0=osv, in1=isov, op=ALU.mult))
            M(lambda e: e.tensor_tensor(out=basev, in0=basev, in1=rank, op=ALU.add))
            M(lambda e: e.tensor_tensor(out=basev, in0=basev, in1=osv, op=ALU.add))
            M(lambda e: e.tensor_copy(out=desti, in_=basev))
            oe3 = oend.rearrange("p (o e) -> p o e", o=1)
            ot3 = cst[:, 1600:1600 + NOV].rearrange("p (j o) -> p j o", o=1)
            M(lambda e: e.tensor_tensor(out=cmpb, in0=bc(oe3, [128, NOV, NE]), in1=bc(ot3, [128, NOV, NE]), op=ALU.is_le))
            M(lambda e: e.tensor_reduce(out=obe, in_=cmpb, axis=AX.X, op=ALU.add))
            obe3 = obe.rearrange("p (j o) -> p j o", o=1)
            kcb3 = cst[:, 1632:1648].rearrange("p (o k) -> p o k", o=1)
            M(lambda e: e.tensor_scalar(out=tmpi, in0=bc(obe3, [128, NOV, 16]), scalar1=float(D), scalar2=None, op0=ALU.mult))
            M(lambda e: e.tensor_tensor(out=tmpi, in0=tmpi, in1=bc(kcb3, [128, NOV, 16]), op=ALU.add))
            M(lambda e: e.tensor_copy(out=oig, in_=tmpi))
            tmpd = tmpi[:, :, 0:FFC]
            M(lambda e: e.tensor_scalar(out=tmpd, in0=bc(obe3, [128, NOV, FFC]), scalar1=float(DE), scalar2=None, op0=ALU.mult))
            M(lambda e: e.tensor_tensor(out=tmpd, in0=tmpd, in1=bc(kcb3[:, :, 0:FFC], [128, NOV, FFC]), op=ALU.add))
            M(lambda e: e.tensor_copy(out=oid, in_=tmpd))
            hrow = [view(RA + i * 4096, [128, D], BF16) for i in range(2)]
            hrowR = [Res(), Res()]
            for t in range(NT):
                s_ = t % 2
                hr = hrow[s_]
                src = h2s_d[t * 128:(t + 1) * 128, :]
                P.op("sp", lambda e, hr=hr, src=src: e.dma_start(out=hr, in_=src), reads=[h2sR], writes=[hrowR[s_]], dma="hrow%d" % s_)
                for k in range(2):
                    ix = desti[:, 2 * t + k: 2 * t + k + 1]
                    P.op("pool", lambda e, hr=hr, ix=ix: e.indirect_dma_start(
                        out=xsort_d[:, :], out_offset=bass.IndirectOffsetOnAxis(ap=ix, axis=0), in_=hr, in_offset=None),
                        reads=[hrowR[s_], mR], writes=[xsortR], dma="sc", extra=[zf_ev[0]])
            P.barrier()
            xb = [view(RA + 8192 + i * 4096, [128, D], BF16) for i in range(2)]
            xbT = [view(RA + 16384 + i * 4096, [128, 16, 128], BF16) for i in range(2)]
            sil = view(RA + 24576, [128, 1024], F32)
            actT = [view(RA + 28672 + i * 2048, [128, 8, 128], BF16) for i in range(2)]
            ybs = [view(RA + 32768 + i * 8192, [128, D], F32) for i in range(2)]
            wf_gu = view(RB, [128, 16, DE], BF16)
            wf_d = view(RC, [128, FFC, D], BF16)
            xbR, xbTR, actR = [Res(), Res()], [Res(), Res()], [Res(), Res()]
            silR = Res()
            ybsR = [[Res() for _ in range(8)] for _ in range(2)]
            wfR = [Res() for _ in range(16)]
            wdR = [Res() for _ in range(FFC)]
            NFFB = 2 * NFB
            for blk in range(NE + NOV):
                s_ = blk % 2
                static = blk < NE
                ex = blk
                j = blk - NE
                rows = xsort_d[blk * 128:(blk + 1) * 128, :]
                xbs = xb[s_]
                P.op("sp", lambda e, xbs=xbs, rows=rows: e.dma_start(out=xbs, in_=rows), reads=[xsortR], writes=[xbR[s_]], dma="xb%d" % s_)
                b0, b1 = nbank(), nbank()

                def tr(e, xbs=xbs, b0=b0, b1=b1):
                    ins = None
                    for q in range(16):
                        bb = b0 if q < 8 else b1
                        ins = e.transpose(banks_bf[bb][:, (q % 8) * 128:(q % 8 + 1) * 128], xbs[:, q * 128:(q + 1) * 128], identb)
                    return ins
                P.op("pe", tr, reads=[xbR[s_]], writes=[bankR[b0], bankR[b1]])
                for hf_, bb in ((0, b0), (1, b1)):
                    pv = banks_bf[bb][:, 0:1024].rearrange("p (a b) -> p a b", a=8)
                    o = xbT[s_][:, hf_ * 8:(hf_ + 1) * 8, :]
                    if hf_ == 0:
                        P.op("act", lambda e, o=o, pv=pv: e.copy(out=o, in_=pv), reads=[bankR[bb]], writes=[xbTR[s_]])
                    else:
                        P.op("dve", lambda e, o=o, pv=pv: e.tensor_copy(out=o, in_=pv), reads=[bankR[bb]], writes=[xbTR[s_]])
                Ab = [nbank(), nbank()]
                Ub = [nbank(), nbank()]
                rhs = [xbT[s_][:, kc, :] for kc in range(16)]

                def cols(ffb):
                    return ((ffb % 4) * 128, (ffb % 4) * 128 + 128)
                if static:
                    for fb in range(NFB):
                        cg = [wchunk(weg_d, ex * 16 + 0, fb * 256), wchunk(weg_d, ex * 16 + 8, fb * 256)]
                        cu = [wchunk(weu_d, ex * 16 + 0, fb * 256), wchunk(weu_d, ex * 16 + 8, fb * 256)]
                        for cb in range(2):
                            ffb = fb * 2 + cb
                            mm_group(Ab[ffb // 4], cols(ffb), [cg[kc // 8][0][:, kc % 8, cb * 128:(cb + 1) * 128] for kc in range(16)],
                                     rhs, [cg[0][1], cg[1][1], xbTR[s_]])
                            mm_group(Ub[ffb // 4], cols(ffb), [cu[kc // 8][0][:, kc % 8, cb * 128:(cb + 1) * 128] for kc in range(16)],
                                     rhs, [cu[0][1], cu[1][1], xbTR[s_]])
                else:
                    for (Wd_, bks) in ((weg_d, Ab), (weu_d, Ub)):
                        for kc in range(16):
                            ix = oig[:, j, kc:kc + 1]
                            o = wf_gu[:, kc, :]
                            P.op("pool", lambda e, o=o, ix=ix, Wd_=Wd_: e.indirect_dma_start(
                                out=o, out_offset=None, in_=Wd_[:, :], in_offset=bass.IndirectOffsetOnAxis(ap=ix, axis=0),
                                bounds_check=breg(e, NE * D - 1), oob_is_err=False),
                                reads=[mR], writes=[wfR[kc]], dma="ow%d" % kc)
                        for ffb in range(NFFB):
                            mm_group(bks[ffb // 4], cols(ffb), [wf_gu[:, kc, ffb * 128:(ffb + 1) * 128] for kc in range(16)],
                                     rhs, wfR + [xbTR[s_]])
                for q in range(2):
                    nf = min(4, NFFB - 4 * q)
                    if nf <= 0:
                        continue
                    sl = sil[:, q * 512:q * 512 + nf * 128]
                    P.op("act", lambda e, sl=sl, q=q, nf=nf, Ab=Ab: e.activation(out=sl, in_=banks[Ab[q]][:, 0:nf * 128], func=AF.Silu),
                         reads=[bankR[Ab[q]]], writes=[silR])
                    o = actT[s_][:, 4 * q:4 * q + nf, :].rearrange("p a b -> p (a b)")
                    P.op("dve", lambda e, o=o, sl=sl, q=q, nf=nf, Ub=Ub: e.tensor_tensor(out=o, in0=sl, in1=banks[Ub[q]][:, 0:nf * 128], op=ALU.mult),
                         reads=[bankR[Ub[q]], silR], writes=[actR[s_]])
                if not static:
                    for ffc in range(FFC):
                        ix = oid[:, j, ffc:ffc + 1]
                        o = wf_d[:, ffc, :]
                        P.op("pool", lambda e, o=o, ix=ix: e.indirect_dma_start(
                            out=o, out_offset=None, in_=wed_d[:, :], in_offset=bass.IndirectOffsetOnAxis(ap=ix, axis=0),
                            bounds_check=breg(e, NE * DE - 1), oob_is_err=False),
                            reads=[mR], writes=[wdR[ffc]], dma="od%d" % ffc)
                for cg8 in range(8):
                    b = nbank()
                    lhs = [actT[s_][:, ffc, :] for ffc in range(FFC)]
                    if static:
                        cd = wchunk(wed_d, ex * FFC, cg8 * 256, nk=FFC)
                        mm_group(b, (0, 256), lhs, [cd[0][:, ffc, 0:256] for ffc in range(FFC)], [cd[1], actR[s_]])
                    else:
                        mm_group(b, (0, 256), lhs, [wf_d[:, ffc, cg8 * 256:(cg8 + 1) * 256] for ffc in range(FFC)], wdR + [actR[s_]])
                    o = ybs[s_][:, cg8 * 256:(cg8 + 1) * 256]
                    if cg8 % 2 == 0:
                        P.op("act", lambda e, o=o, b=b: e.copy(out=o, in_=banks[b][:, 0:256]), reads=[bankR[b]], writes=[ybsR[s_][cg8]])
                    else:
                        P.op("dve", lambda e, o=o, b=b: e.tensor_copy(out=o, in_=banks[b][:, 0:256]), reads=[bankR[b]], writes=[ybsR[s_][cg8]])
                yrows = ysort_d[blk * 128:(blk + 1) * 128, :]
                yb_ = ybs[s_]
                P.op("sp", lambda e, yb_=yb_, yrows=yrows: e.dma_start(out=yrows, in_=yb_), reads=ybsR[s_], writes=[ysortR], dma="ys")
            P.barrier()
            gfin = view(RC, [128, D], F32)
            y0 = [view(RA + i * 8192, [128, D], F32) for i in range(2)]
            y1 = [view(RA + 16384 + i * 8192, [128, D], F32) for i in range(2)]
            xt2 = [view(RA + 32768 + i * 8192, [128, D], F32) for i in range(2)]
            ot = [view(RA + 49152 + i * 8192, [128, D], F32) for i in range(2)]
            junk7 = view(RC + 8192, [128, D], BF16)
            y0R, y1R, xtR, otR = ([Res(), Res()] for _ in range(4))
            gfR, j7R = Res(), Res()
            P.op("sp", lambda e: e.dma_start(out=gfin, in_=gf_d.partition_broadcast(128)), writes=[gfR], dma="gfin")
            for t in range(NT):
                s_ = t % 2
                for k, (yb, yR_) in enumerate(((y0[s_], y0R[s_]), (y1[s_], y1R[s_]))):
                    ix = desti[:, 2 * t + k: 2 * t + k + 1]
                    P.op("pool", lambda e, yb=yb, ix=ix: e.indirect_dma_start(
                        out=yb, out_offset=None, in_=ysort_d[:, :], in_offset=bass.IndirectOffsetOnAxis(ap=ix, axis=0)),
                        reads=[ysortR, mR], writes=[yR_], dma="yg%d%d" % (k, s_))
                xx = xt2[s_]
                src = x2s_d[t * 128:(t + 1) * 128, :]
                P.op("sp", lambda e, xx=xx, src=src: e.dma_start(out=xx, in_=src), reads=[x2sR], writes=[xtR[s_]], dma="xt%d" % s_)
                ya, yb2 = y0[s_], y1[s_]
                gv0, gv1 = GT[:, t, 0:1], GT[:, t, 1:2]
                P.op("dve", lambda e, xx=xx, ya=ya, gv0=gv0: e.scalar_tensor_tensor(out=xx, in0=ya, scalar=gv0, in1=xx, op0=ALU.mult, op1=ALU.add),
                     reads=[y0R[s_], routeR], writes=[xtR[s_]])
                P.op("dve", lambda e, xx=xx, yb2=yb2, gv1=gv1: e.scalar_tensor_tensor(out=xx, in0=yb2, scalar=gv1, in1=xx, op0=ALU.mult, op1=ALU.add),
                     reads=[y1R[s_], routeR], writes=[xtR[s_]])
                ssq, rt = stat[:, 20 + 2 * s_:21 + 2 * s_], stat[:, 21 + 2 * s_:22 + 2 * s_]
                P.op("act", lambda e, xx=xx, ssq=ssq: e.activation(out=junk7, in_=xx, func=AF.Square, accum_out=ssq), reads=[xtR[s_]], writes=[j7R, statR])
                P.op("act", lambda e, ssq=ssq, rt=rt: e.activation(out=rt, in_=ssq, func=AF.Sqrt, bias=EPS, scale=1.0 / D), reads=[statR], writes=[statR])
                P.op("dve", lambda e, rt=rt: e.reciprocal(out=rt, in_=rt), reads=[statR], writes=[statR])
                o = ot[s_]
                P.op("dve", lambda e, o=o, xx=xx, rt=rt: e.scalar_tensor_tensor(out=o, in0=xx, scalar=rt, in1=gfin, op0=ALU.mult, op1=ALU.mult),
                     reads=[xtR[s_], statR, gfR], writes=[otR[s_]])
                dst = out_d[t * 128:(t + 1) * 128, :]
                P.op("sp", lambda e, o=o, dst=dst: e.dma_start(out=dst, in_=o), reads=[otR[s_]], dma="out%d" % s_)
            P.barrier()

        setup()
        zero_fill_xsort()
        for ph in range(NPH if STOP > 0 else 0):
            prologue_half(ph)
        for hf in range(NH if STOP > 0 else 0):
            main_half(hf)
        if STOP > 6:
            moe_sorted()
        P.barrier()
        P.op("sp", None)
        P.emit(nc, es)
    return nc


CFG = dict(tpc=2048, nph=14, ng=8, epg=8, de=1024)
N_CORES = 8


def make_consts():
    c = np.zeros((128, 1648), np.float32)
    p = np.arange(128)[:, None]
    f = np.arange(128)[None, :]
    c[:, 0:128] = (p == f)
    c[:, 128:256] = (f <= p)
    c[:, 256:384] = (f >= p)
    rm = np.ones((128, 1024), np.float32)
    rm[:, ::128] = 0.0
    c[:, 384:1408] = rm
    c[:, 1408:1536] = (f > p)
    c[:, 1536:1600] = 128.0 * np.arange(64)[None, :]
    c[:, 1600:1632] = 128.0 * np.arange(32)[None, :]
    c[:, 1632:1648] = 128.0 * np.arange(16)[None, :] + np.arange(128)[:, None]
    return c


def make_in_maps(cfg, n_cores, x, norm_mix_g, w_in, gmlp_ln_g, gmlp_ln_b, w_spatial, b_spatial, hgrn_lb_logits,
                 hgrn_norm_g, w_branch_a, w_branch_b, w_out, norm_ffn_g, w_router_group, b_router_group,
                 w_router_expert, b_router_expert, w_expert_gate, w_expert_up, w_expert_down, norm_final_g):
    f = lambda a: np.ascontiguousarray(np.asarray(a, dtype=np.float32))
    TPC = cfg["tpc"]
    NPH = cfg["nph"]
    NE = cfg["ng"] * cfg["epg"]
    DE = cfg["de"]
    xf = f(x).reshape(-1, D)
    shared = dict(
        norm_mix_g=f(norm_mix_g).reshape(D), w_in=f(w_in).reshape(D, 10240),
        gmlp_ln_g=f(gmlp_ln_g).reshape(1024), gmlp_ln_b=f(gmlp_ln_b).reshape(1024),
        w_spatial=f(w_spatial).reshape(8, 128, 128), b_spatial=f(b_spatial).reshape(1, 1024),
        hgrn_lb=f(hgrn_lb_logits).reshape(2, 1024), hgrn_norm_g=f(hgrn_norm_g).reshape(1024),
        w_branch_a=f(w_branch_a).reshape(1024, D), w_branch_b=f(w_branch_b).reshape(1024, D),
        w_out=f(w_out).reshape(D, D), norm_ffn_g=f(norm_ffn_g).reshape(D),
        w_router=np.ascontiguousarray(np.concatenate([f(w_router_group).reshape(D, -1), f(w_router_expert).reshape(D, -1)], axis=1)),
        b_router=np.ascontiguousarray(np.concatenate([f(b_router_group).reshape(-1), f(b_router_expert).reshape(-1)])),
        w_eg=f(w_expert_gate).reshape(NE * D, DE), w_eu=f(w_expert_up).reshape(NE * D, DE),
        w_ed=f(w_expert_down).reshape(NE * DE, D), norm_final_g=f(norm_final_g).reshape(D),
        consts=make_consts(),
    )
    maps = []
    npt = max(NPH, 1) * 1024
    for c in range(n_cores):
        m = dict(shared)
        m["x"] = xf[c * TPC:(c + 1) * TPC]
        xp = np.zeros((npt, D), np.float32)
        prev = xf[0:c * TPC]
        if NPH > 0 and prev.shape[0] > 0:
            xp[npt - prev.shape[0]:] = prev
        m["xprev"] = xp
        maps.append(m)
    return maps


def kernel(**inputs):
    nc = build(CFG)
    maps = make_in_maps(CFG, N_CORES, **inputs)
    res = run_bass_kernel_spmd(nc, maps, core_ids=list(range(N_CORES)))
    out = np.concatenate([r["out"] for r in res.results], axis=0)
    return out.reshape(1, N_CORES * CFG["tpc"], D).astype(np.float32)
```

```python
import numpy as np
from contextlib import ExitStack
import concourse.bass as bass
import concourse.mybir as mybir
from concourse.bass_utils import run_bass_kernel_spmd

F32 = mybir.dt.float32
BF16 = mybir.dt.bfloat16
AF = mybir.ActivationFunctionType
ALU = mybir.AluOpType
AX = mybir.AxisListType

D = 2048
KC = 16
EPS = 1e-6
ENGS = ("pe", "act", "dve", "pool", "sp")


class Res:
    __slots__ = ("lw", "rd", "name", "excl")

    def __init__(self, name="", excl=False):
        self.lw = None
        self.rd = []
        self.name = name
        self.excl = excl


class Op:
    __slots__ = ("eng", "fn", "waits", "dma", "needed", "val")

    def __init__(self, eng, fn, waits, dma):
        self.eng = eng
        self.fn = fn
        self.waits = waits
        self.dma = dma
        self.needed = False
        self.val = 0


class Prog:
    def __init__(self):
        self.q = {e: [] for e in ENGS}
        self.waited = {e: {} for e in ENGS}
        self.fence = {e: [] for e in ENGS}
        self.dma_cnt = {}
        self.last_dma = {}
        self.last_c = {}

    def op(self, eng, fn, reads=(), writes=(), dma=None, nofence=False, extra=()):
        deps = list(extra)
        ex = [r for r in reads if r.excl]
        if ex:
            reads = [r for r in reads if not r.excl]
            writes = list(writes) + ex
        for r in reads:
            if r.lw is not None:
                deps.append(r.lw)
        for w in writes:
            if w.lw is not None:
                deps.append(w.lw)
            deps.extend(w.rd)
        if not nofence and self.fence[eng]:
            deps.extend(self.fence[eng])
            self.fence[eng] = []
        deps.sort(key=lambda d: -d[2])
        waits = []
        wd = self.waited[eng]
        for d in deps:
            if d[0] == "c":
                _, pe_, pidx = d
                if pe_ == "pe" and eng == "pe":
                    continue
                key = ("c", pe_)
                if wd.get(key, -1) >= pidx:
                    continue
                wd[key] = pidx
                self.q[pe_][pidx].needed = True
                waits.append(d)
            else:
                _, stream, val = d
                key = ("d", stream)
                if wd.get(key, 0) >= val:
                    continue
                wd[key] = val
                waits.append(d)
        idx = len(self.q[eng])
        o = Op(eng, fn, waits, dma)
        if dma is not None:
            cnt = self.dma_cnt.get(dma, 0) + 1
            self.dma_cnt[dma] = cnt
            ev = ("d", dma, cnt * 16)
            self.last_dma[dma] = ev
        else:
            ev = ("c", eng, idx)
            self.last_c[eng] = ev
        self.q[eng].append(o)
        for r in reads:
            r.rd.append(ev)
        for w in writes:
            w.lw = ev
            w.rd = []
        return ev

    def barrier(self):
        evs = list(self.last_c.values()) + list(self.last_dma.values())
        for e in ENGS:
            self.fence[e] = list(evs)

    def emit(self, nc, es):
        for e in ENGS:
            c = 0
            for o in self.q[e]:
                if o.dma is None and o.needed:
                    c += 1
                    o.val = c
        sem_c = {e: es.enter_context(nc.semaphore("c_" + e)) for e in ENGS}
        sem_d = {s: es.enter_context(nc.semaphore("d_" + s)) for s in self.dma_cnt}
        q = self.q

        def run(name, e):
            for o in q[name]:
                for d in o.waits:
                    if d[0] == "c":
                        e.wait_ge(sem_c[d[1]], q[d[1]][d[2]].val)
                    else:
                        e.wait_ge(sem_d[d[1]], d[2])
                if o.fn is None:
                    continue
                ins = o.fn(e)
                if o.dma is not None:
                    ins.then_inc(sem_d[o.dma], 16)
                elif o.needed:
                    ins.then_inc(sem_c[name], 1)

        with nc.Block() as block:
            @block.tensor
            def _(e):
                run("pe", e)

            @block.scalar
            def _(e):
                run("act", e)

            @block.vector
            def _(e):
                run("dve", e)

            @block.gpsimd
            def _(e):
                run("pool", e)

            @block.sync
            def _(e):
                run("sp", e)


def bc(ap, shape):
    return ap.broadcast_to(list(shape))


def build(cfg):
    TPC = cfg["tpc"]
    NH = TPC // 1024
    NPH = cfg["nph"]
    NG, EPG, DE = cfg["ng"], cfg["epg"], cfg["de"]
    NE = NG * EPG
    NR = NG + NE
    FFC = DE // 128
    NFB = DE // 256

    nc = bass.Bass("TRN2", target_bir_lowering=False)

    def din(name, shape, dt=F32):
        return nc.dram_tensor(name, list(shape), dt, kind="ExternalInput").ap()

    x_d = din("x", [TPC, D])
    xp_d = din("xprev", [max(NPH, 1) * 1024, D])
    g1_d = din("norm_mix_g", [D])
    win_d = din("w_in", [D, 10240])
    lng_d = din("gmlp_ln_g", [1024])
    lnb_d = din("gmlp_ln_b", [1024])
    wsp_d = din("w_spatial", [8, 128, 128])
    bsp_d = din("b_spatial", [1, 1024])
    lbl_d = din("hgrn_lb", [2, 1024])
    hng_d = din("hgrn_norm_g", [1024])
    wpa_d = din("w_branch_a", [1024, D])
    wpb_d = din("w_branch_b", [1024, D])
    wout_d = din("w_out", [D, D])
    g2_d = din("norm_ffn_g", [D])
    wr_d = din("w_router", [D, NR])
    br_d = din("b_router", [NR])
    weg_d = din("w_eg", [NE * D, DE])
    weu_d = din("w_eu", [NE * D, DE])
    wed_d = din("w_ed", [NE * DE, D])
    gf_d = din("norm_final_g", [D])
    cst_d = din("consts", [128, 1648])
    out_d = nc.dram_tensor("out", [TPC, D], F32, kind="ExternalOutput").ap()
    DBG = cfg.get("dbg", False)
    STOP = cfg.get("stop", 99)
    NT = TPC // 128
    NA = 2 * NT
    NOV = NA
    dint = lambda n, sh, dt: nc.dram_tensor(n, list(sh), dt, kind="Internal").ap()
    x2s_d = dint("x2s", [TPC, D], F32)
    h2s_d = dint("h2s", [TPC, D], BF16)
    xsort_d = dint("xsort", [(NE + NOV) * 128, D], BF16)
    ysort_d = dint("ysort", [(NE + NOV) * 128, D], F32)
    if DBG:
        dout = lambda n, sh, dt: nc.dram_tensor(n, list(sh), dt, kind="ExternalOutput").ap()
        dbg_h = dout("dbg_h", [128, 16384], BF16)
        dbg_ya = dout("dbg_ya", [128, 8192], BF16)
        dbg_yb = dout("dbg_yb", [128, 8192], BF16)
        dbg_m = dout("dbg_m", [128, 16384], BF16)
        dbg_x2 = dout("dbg_x2", [1024, D], F32)
        dbg_G = dout("dbg_G", [128, 8 * NE], F32)
        dbg_S = dout("dbg_S", [128, 1024], F32)

    P = Prog()
    es = ExitStack()
    with es:
        ARENA_BYTES = 211968
        arena = es.enter_context(nc.sbuf_tensor("arena", [128, ARENA_BYTES // 4], F32))
        arena_bf = arena.bitcast(BF16)
        banks = [es.enter_context(nc.psum_tensor("bank%d" % i, [128, 512], F32)) for i in range(8)]
        banks_bf = [b.bitcast(BF16) for b in banks]
        bankR = [Res("bank%d" % i, excl=True) for i in range(8)]

        def view(off, shape, dt):
            n = 1
            for s in shape[1:]:
                n *= s
            if dt == F32:
                assert off % 4 == 0
                ap = arena[:, off // 4: off // 4 + n]
            else:
                assert off % 2 == 0
                ap = arena_bf[:, off // 2: off // 2 + n]
            if shape[0] != 128:
                ap = ap[0:shape[0], :]
            if len(shape) == 3:
                ap = ap.rearrange("p (a b) -> p a b", a=shape[1])
            elif len(shape) == 4:
                ap = ap.rearrange("p (a b c) -> p a b c", a=shape[1], b=shape[2])
            return ap

        cur = [0]

        def alloc(shape, dt, nbytes=None):
            n = 1
            for s in shape[1:]:
                n *= s
            sz = n * (4 if dt == F32 else 2)
            sz = (sz + 63) // 64 * 64
            off = cur[0]
            cur[0] += sz if nbytes is None else nbytes
            return view(off, shape, dt), off

        cst, _ = alloc([128, 1648], F32)
        ident_f = cst[:, 0:128]
        LT = cst[:, 128:256]
        UT = cst[:, 256:384]
        rmask = cst[:, 384:1408]
        SUT = cst[:, 1408:1536]
        identb, _ = alloc([128, 128], BF16)
        sutb, _ = alloc([128, 128], BF16)
        onesb, _ = alloc([128, 128], BF16)
        g1T, _ = alloc([128, 16], F32)
        g2T, _ = alloc([128, 16], F32)
        ngT, _ = alloc([128, 8], F32)
        lbT, _ = alloc([128, 8], F32)
        omlT, _ = alloc([128, 8], F32)
        brt, _ = alloc([128, NR], F32)
        bshi, _ = alloc([1, 1024], BF16)
        bslo, _ = alloc([1, 1024], BF16)
        wsT, _ = alloc([128, 8, 128], BF16)
        wrhi, _ = alloc([128, 16, NR], BF16)
        wrlo, _ = alloc([128, 16, NR], BF16)
        S_f, _ = alloc([128, 8, 128], F32)
        S_b, _ = alloc([128, 8, 128], BF16)
        Aj, _ = alloc([128, 8, 8], F32)
        stat, _ = alloc([128, 64], F32)
        SPARE_SZ = 12288
        _, SPARE = alloc([128, SPARE_SZ // 4], F32)
        NRB = 10
        ring = [alloc([128, 8, 256], BF16)[0] for _ in range(NRB)]
        ringR = [Res("ring%d" % i) for i in range(NRB)]
        _, RA = alloc([128, 16384], F32)
        _, RB = alloc([128, 8192], F32)
        _, RC = alloc([128, 8192], F32)
        assert cur[0] <= ARENA_BYTES, cur[0]

        I32 = mybir.dt.int32
        rs = view(SPARE, [128, 512], F32)
        OH = view(SPARE + 2048, [128, NT, 2, NE], BF16)
        o_ = SPARE + 2048 + NT * 2 * NE * 2
        GT = view(o_, [128, NT, 2], F32)
        rank = view(o_ + 128, [128, NA], F32)
        desti = arena.bitcast(I32)[:, (o_ + 256) // 4:(o_ + 256) // 4 + NA]
        basev = view(o_ + 384, [128, NA], F32)
        osv = view(o_ + 512, [128, NA], F32)
        isov = view(o_ + 640, [128, NA], F32)
        cntall = view(o_ + 768, [128, NE], F32)
        opad = view(o_ + 1024, [128, NE], F32)
        oend = view(o_ + 1280, [128, NE], F32)
        ostart = view(o_ + 1536, [128, NE], F32)
        tmp64 = view(o_ + 1792, [128, NE], F32)
        ones64 = view(o_ + 2048, [128, NE], F32)
        obe = view(o_ + 2304, [128, NOV], F32)
        o2_ = o_ + 2304 + 128
        oig = arena.bitcast(I32)[:, o2_ // 4:o2_ // 4 + NOV * 16].rearrange("p (j k) -> p j k", k=16)
        o3_ = o2_ + NOV * 16 * 4
        oid = arena.bitcast(I32)[:, o3_ // 4:o3_ // 4 + NOV * FFC].rearrange("p (j k) -> p j k", k=FFC)
        assert o3_ + NOV * FFC * 4 <= SPARE + SPARE_SZ, (o3_ + NOV * FFC * 4 - SPARE)
        routeR, x2sR, h2sR, xsortR, ysortR = (Res() for _ in range(5))
        constR = Res("const")
        SR = Res("S")
        SbR = Res("Sb")
        statR = Res("stat")

        wctr = [0]

        def wchunk(W2d, kc0, c0, nk=8, ncols=256):
            b = wctr[0] % NRB
            wctr[0] += 1
            src = W2d[kc0 * 128:(kc0 + nk) * 128, c0:c0 + ncols].rearrange("(kc p) c -> p kc c", p=128)
            rv = ring[b][:, 0:nk, 0:ncols]
            P.op("pool", lambda e, rv=rv, src=src: e.dma_start(out=rv, in_=src),
                 writes=[ringR[b]], dma="rg%d" % b, nofence=True)
            return ring[b], ringR[b]

        bctr = [0]

        def nbank():
            b = bctr[0] % 8
            bctr[0] += 1
            return b

        def setup():
            tmpC_f = view(RC, [128, 16, NR], F32)
            tmpC_w = view(RC + 8192, [128, 8, 128], F32)
            tmpC_wb = view(RC + 8192 + 4096, [128, 8, 128], BF16)
            bsrow = view(RC + 16384, [1, 1024], F32)
            bstmp = view(RC + 16384 + 4096, [1, 1024], F32)
            l01 = view(RC + 30720, [128, 2, 8], F32)
            g2b = g2T.rearrange("p (a o) -> p a o", o=1)
            sp = "cst"

            def ld(dst, src, slow=False):
                if slow:
                    P.op("sp", lambda e: e.dma_start(out=dst, in_=src, allow_slow_non_contiguous=True),
                         writes=[constR], dma=sp)
                else:
                    P.op("sp", lambda e: e.dma_start(out=dst, in_=src), writes=[constR], dma=sp)

            ld(cst, cst_d[:, :])
            ld(g1T, g1_d.rearrange("(kc p) -> p kc", p=128), True)
            ld(g2T, g2_d.rearrange("(kc p) -> p kc", p=128), True)
            ld(ngT, hng_d.rearrange("(h v) -> v h", v=128), True)
            ld(l01, lbl_d.rearrange("l (h k) -> k l h", k=128), True)
            ld(brt, br_d.partition_broadcast(128))
            ld(bsrow, bsp_d[:, :])
            ld(tmpC_w, wsp_d.rearrange("g t s -> t g s"))
            ld(tmpC_f, wr_d.rearrange("(kc p) c -> p kc c", p=128))
            P.op("dve", lambda e: e.tensor_copy(out=identb, in_=ident_f), reads=[constR], writes=[constR])
            P.op("dve", lambda e: e.memset(onesb, 1.0), writes=[constR])
            P.op("dve", lambda e: e.tensor_copy(out=sutb, in_=SUT), reads=[constR], writes=[constR])
            P.op("dve", lambda e: e.memset(S_f, 0.0), writes=[SR])
            P.op("dve", lambda e: e.memset(S_b, 0.0), writes=[SbR])
            P.op("dve", lambda e: e.memset(Aj, 0.0), writes=[constR])
            P.op("dve", lambda e: e.tensor_tensor(out=lbT, in0=l01[:, 0, :], in1=l01[:, 1, :], op=ALU.subtract),
                 reads=[constR], writes=[constR])
            P.op("act", lambda e: e.activation(out=lbT, in_=lbT, func=AF.Sigmoid), reads=[constR], writes=[constR])
            P.op("dve", lambda e: e.tensor_scalar(out=omlT, in0=lbT, scalar1=-1.0, scalar2=1.0, op0=ALU.mult, op1=ALU.add),
                 reads=[constR], writes=[constR])
            P.op("dve", lambda e: e.tensor_copy(out=bshi, in_=bsrow), reads=[constR], writes=[constR])
            P.op("dve", lambda e: e.tensor_tensor(out=bstmp, in0=bsrow, in1=bshi, op=ALU.subtract), reads=[constR], writes=[constR])
            P.op("dve", lambda e: e.tensor_copy(out=bslo, in_=bstmp), reads=[constR], writes=[constR])
            P.op("dve", lambda e: e.tensor_tensor(out=tmpC_wb, in0=tmpC_w, in1=bc(LT.rearrange("p (o f) -> p o f", o=1), [128, 8, 128]), op=ALU.mult),
                 reads=[constR], writes=[constR])
            b = nbank()

            def tr8(e):
                ins = None
                for g in range(8):
                    ins = e.transpose(banks_bf[b][:, g * 128:(g + 1) * 128], tmpC_wb[:, g, :], identb)
                return ins
            P.op("pe", tr8, reads=[constR], writes=[bankR[b]])
            P.op("dve", lambda e: e.tensor_copy(out=wsT, in_=banks_bf[b][:, 0:1024].rearrange("p (g t) -> p g t", g=8)),
                 reads=[bankR[b]], writes=[constR])
            P.op("dve", lambda e: e.tensor_tensor(out=tmpC_f, in0=tmpC_f, in1=bc(g2b, [128, 16, NR]), op=ALU.mult),
                 reads=[constR], writes=[constR])
            P.op("dve", lambda e: e.tensor_copy(out=wrhi, in_=tmpC_f), reads=[constR], writes=[constR])
            tmp2 = view(RC + 24576, [128, 16, NR], F32) if 16 * NR * 4 <= 8192 else None
            P.op("dve", lambda e: e.tensor_tensor(out=tmp2, in0=tmpC_f, in1=wrhi, op=ALU.subtract), reads=[constR], writes=[constR])
            P.op("dve", lambda e: e.tensor_copy(out=wrlo, in_=tmp2), reads=[constR], writes=[constR])
            P.barrier()

        def norm_transpose(src_rows, gT, dstT, tmp_off, store_x=None):
            xin = [view(tmp_off + i * 8192, [128, D], F32) for i in range(2)]
            xs = [view(tmp_off + 16384 + i * 4096, [128, D], BF16) for i in range(2)]
            junk = view(tmp_off + 24576, [128, D], BF16)
            xinR = [Res(), Res()]
            xsR = [Res(), Res()]
            junkR = Res()
            g3 = gT.rearrange("p (a o) -> p a o", o=1)
            for t in range(8):
                s = t % 2
                xt = xin[s]
                src = src_rows(t)
                P.op("sp", lambda e, xt=xt, src=src: e.dma_start(out=xt, in_=src), writes=[xinR[s]], dma="xin%d" % s)
                ssq = stat[:, 2 * s:2 * s + 1]
                rt = stat[:, 2 * s + 1:2 * s + 2]
                P.op("act", lambda e, xt=xt, ssq=ssq: e.activation(out=junk, in_=xt, func=AF.Square, accum_out=ssq),
                     reads=[xinR[s]], writes=[junkR, statR])
                P.op("act", lambda e, ssq=ssq, rt=rt: e.activation(out=rt, in_=ssq, func=AF.Sqrt, bias=EPS, scale=1.0 / D),
                     reads=[statR], writes=[statR])
                P.op("dve", lambda e, rt=rt: e.reciprocal(out=rt, in_=rt), reads=[statR], writes=[statR])
                xst = xs[s]
                P.op("dve", lambda e, xt=xt, xst=xst, rt=rt: e.tensor_scalar(out=xst, in0=xt, scalar1=rt, scalar2=None, op0=ALU.mult),
                     reads=[xinR[s], statR], writes=[xsR[s]])
                b0, b1 = nbank(), nbank()

                def tr(e, xst=xst, b0=b0, b1=b1):
                    ins = None
                    for j in range(16):
                        bb = b0 if j < 8 else b1
                        ins = e.transpose(banks_bf[bb][:, (j % 8) * 128:(j % 8 + 1) * 128], xst[:, j * 128:(j + 1) * 128], identb)
                    return ins
                P.op("pe", tr, reads=[xsR[s]], writes=[bankR[b0], bankR[b1]])
                for hf, bb in ((0, b0), (1, b1)):
                    o = dstT[:, hf * 8:(hf + 1) * 8, t * 128:(t + 1) * 128]
                    i0 = banks_bf[bb][:, 0:1024].rearrange("p (a b) -> p a b", a=8)
                    i1 = bc(g3[:, hf * 8:(hf + 1) * 8, :], [128, 8, 128])
                    P.op("dve", lambda e, o=o, i0=i0, i1=i1: e.tensor_tensor(out=o, in0=i0, in1=i1, op=ALU.mult),
                         reads=[bankR[bb]], writes=[])

        def mm_group(b, bcols, lhs_list, rhs_list, reads):
            n = len(lhs_list)

            def fn(e):
                ins = None
                for i in range(n):
                    ins = e.matmul(banks[b][:, bcols[0]:bcols[1]], lhs_list[i], rhs_list[i], start=(i == 0), stop=(i == n - 1))
                return ins
            return P.op("pe", fn, reads=reads, writes=[bankR[b]])

        def hgrn_tile_common(fs_t, kT_t, T, TR):
            a = T["a"]
            P.op("dve", lambda e: e.tensor_scalar(out=kT_t, in0=fs_t, scalar1=-1.0, scalar2=1.0, op0=ALU.mult, op1=ALU.add),
                 reads=[TR["fs"]], writes=[TR["k"]])
            P.op("act", lambda e: e.activation(out=fs_t, in_=fs_t, func=AF.Ln), reads=[TR["fs"], TR["k"]], writes=[TR["fs"]])
            a2 = a.rearrange("p a b -> p (a b)")
            lf2 = fs_t.rearrange("p a b -> p (a b)")
            P.op("dve", lambda e: e.tensor_tensor_scan(out=a2, data0=rmask, data1=lf2, initial=0.0, op0=ALU.mult, op1=ALU.add),
                 reads=[TR["fs"]], writes=[TR["a"]])

        def hgrn_state_update(kT_t, itok_t, T, TR):
            a = T["a"]
            tA, tC, kdecT, kdtok, dS = T["tA"], T["tC"], T["kdecT"], T["kdtok"], T["dS"]
            aend = a[:, :, 127:128]
            P.op("dve", lambda e: e.tensor_tensor(out=tA, in0=bc(aend, [128, 8, 128]), in1=a, op=ALU.subtract),
                 reads=[TR["a"]], writes=[TR["tA"]])
            P.op("act", lambda e: e.activation(out=tC, in_=tA, func=AF.Exp), reads=[TR["tA"]], writes=[TR["tC"]])
            P.op("dve", lambda e: e.tensor_tensor(out=kdecT, in0=kT_t, in1=tC, op=ALU.mult),
                 reads=[TR["k"], TR["tC"]], writes=[TR["kdecT"]])
            P.op("act", lambda e: e.activation(out=dS, in_=aend, func=AF.Exp), reads=[TR["a"]], writes=[TR["dS"]])
            b = nbank()

            def tr(e):
                ins = None
                for h in range(8):
                    ins = e.transpose(banks_bf[b][:, h * 128:(h + 1) * 128], kdecT[:, h, :], identb)
                return ins
            P.op("pe", tr, reads=[TR["kdecT"]], writes=[bankR[b]])
            P.op("act", lambda e: e.copy(out=kdtok, in_=banks_bf[b][:, 0:1024]), reads=[bankR[b]], writes=[TR["kdtok"]])
            for hb in range(2):
                b2 = nbank()

                def mmB(e, hb=hb, b2=b2):
                    ins = None
                    for hq in range(4):
                        h = hb * 4 + hq
                        ins = e.matmul(banks[b2][:, hq * 128:(hq + 1) * 128], kdtok[:, h * 128:(h + 1) * 128],
                                       itok_t[:, h * 128:(h + 1) * 128], start=True, stop=True)
                    return ins
                P.op("pe", mmB, reads=[TR["kdtok"], TR["itok"]], writes=[bankR[b2]])
                Sv = S_f[:, hb * 4:(hb + 1) * 4, :]
                dSv = bc(dS[:, hb * 4:(hb + 1) * 4, :], [128, 4, 128])
                Bv = banks[b2][:, 0:512].rearrange("p (a b) -> p a b", a=4)
                P.op("dve", lambda e, Sv=Sv, dSv=dSv: e.tensor_tensor(out=Sv, in0=Sv, in1=dSv, op=ALU.mult),
                     reads=[TR["dS"]], writes=[SR])
                P.op("dve", lambda e, Sv=Sv, Bv=Bv: e.tensor_tensor(out=Sv, in0=Sv, in1=Bv, op=ALU.add),
                     reads=[bankR[b2]], writes=[SR])
            P.op("act", lambda e: e.copy(out=S_b, in_=S_f), reads=[SR], writes=[SbR])

        def hgrn_temps(base_offs):
            T, TR = {}, {}
            return T, TR

        hT = view(RB, [128, 16, 1024], BF16)
        yaT = view(RA, [128, 8, 1024], BF16)
        ybT = view(RA + 16384, [128, 8, 1024], BF16)
        mT = view(RC, [128, 16, 1024], BF16)
        yacc = view(RA, [128, 8, D], F32)

        def proj_fm(W2d, c0, actT, nkc, ntok_blocks, tokw, consume, tok0=0, wsrc=None):
            chunks = []
            for k0 in range(0, nkc, 8):
                chunks.append(wchunk(W2d, k0, c0) if wsrc is None else wsrc(k0, c0))
            for cb in range(2):
                for tb in range(ntok_blocks):
                    b = nbank()
                    lhs = [chunks[kc // 8][0][:, kc % 8, cb * 128:(cb + 1) * 128] for kc in range(nkc)]
                    rhs = [actT[:, kc, tok0 + tb * tokw: tok0 + (tb + 1) * tokw] for kc in range(nkc)]
                    mm_group(b, (0, tokw), lhs, rhs, [c[1] for c in chunks])
                    consume(cb, tb, b)

        def proj_tm(W2d, c0, actT, nkc, tiles, consume, wsrc=None):
            chunks = []
            for k0 in range(0, nkc, 8):
                chunks.append(wchunk(W2d, k0, c0) if wsrc is None else wsrc(k0, c0))
            for t in tiles:
                b = nbank()
                lhs = [actT[:, kc, t * 128:(t + 1) * 128] for kc in range(nkc)]
                rhs = [chunks[kc // 8][0][:, kc % 8, 0:256] for kc in range(nkc)]
                mm_group(b, (0, 256), lhs, rhs, [c[1] for c in chunks])
                consume(t, b)

        def hgrn_alloc(off_list):
            pools = [[o, o + s] for o, s in off_list]

            def take(shape, dt):
                n = 1
                for s_ in shape[1:]:
                    n *= s_
                sz = n * (4 if dt == F32 else 2)
                sz = (sz + 63) // 64 * 64
                for p in pools:
                    if p[1] - p[0] >= sz:
                        o = p[0]
                        p[0] += sz
                        return view(o, shape, dt)
                raise RuntimeError("hgrn temp alloc failed")
            return take

        def prologue_half(ph):
            norm_transpose(lambda t: xp_d[ph * 1024 + t * 128: ph * 1024 + (t + 1) * 128, :], g1T, hT, RC)
            P.barrier()
            take = hgrn_alloc([(RC, 32768), (SPARE, SPARE_SZ)])
            fs = take([128, 2, 8, 128], F32)
            kT = take([128, 2, 8, 128], BF16)
            itok = take([128, 2, 1024], BF16)
            T = {"a": take([128, 8, 128], F32), "tA": take([128, 8, 128], F32), "tC": take([128, 8, 128], F32),
                 "kdecT": take([128, 8, 128], BF16), "kdtok": take([128, 1024], BF16), "dS": take([128, 8, 1], F32)}
            TR = {k: Res(k) for k in ("fs", "k", "a", "tA", "tC", "kdecT", "kdtok", "dS", "itok")}
            for tb4 in range(4):
                tok0 = tb4 * 256
                hgrn_project_f_i(tok0, fs, itok, TR, wsrc=wcache_src)
                hgrn_f_affine(fs, TR)
                for tl in range(2):
                    hgrn_tile_common(fs[:, tl], kT[:, tl], T, TR)
                    hgrn_state_update(kT[:, tl], itok[:, tl, :], T, TR)
            P.barrier()

        wcache = view(RA, [128, 16, 2048], BF16)
        wcR = Res("wcache")

        def wcache_src(k0, c0):
            cc = c0 - 3072
            return wcache[:, k0:k0 + 8, cc:cc + 256], wcR

        def load_wcache():
            for cc in range(0, 2048, 256):
                for k0 in (0, 8):
                    src = win_d[k0 * 128:(k0 + 8) * 128, 3072 + cc:3072 + cc + 256].rearrange("(kc p) c -> p kc c", p=128)
                    dst = wcache[:, k0:k0 + 8, cc:cc + 256]
                    P.op("pool", lambda e, dst=dst, src=src: e.dma_start(out=dst, in_=src), writes=[wcR], dma="wc")


        def hgrn_project_f_i(tok0, fs, itok, TR, qT=None, sgn=None, sgtmp=None, wsrc=None):
            sections = [("f", 3072)]
            if qT is not None:
                sections = [("q", 2048), ("f", 3072), ("g", 5120)]
            for name, cbase in sections:
                for cg in range(4):
                    def consume(cb, tb, b, name=name, cg=cg):
                        h = cg * 2 + cb
                        src = banks[b][:, 0:256].rearrange("p (t c) -> p t c", t=2)
                        if name == "f":
                            o = fs[:, :, h, :]
                            P.op("act", lambda e: e.activation(out=o, in_=src, func=AF.Sigmoid), reads=[bankR[b]], writes=[TR["fs"]])
                        elif name == "q":
                            o = qT[:, :, h, :]
                            P.op("act", lambda e: e.copy(out=o, in_=src), reads=[bankR[b]], writes=[TR["q"]])
                        else:
                            P.op("act", lambda e: e.activation(out=sgtmp, in_=banks[b][:, 0:256], func=AF.Silu),
                                 reads=[bankR[b]], writes=[TR["sgtmp"]])
                            o = sgn[:, :, h, :]
                            sv = sgtmp.rearrange("p (t c) -> p t c", t=2)
                            P.op("dve", lambda e: e.tensor_scalar(out=o, in0=sv, scalar1=ngT[:, h:h + 1], scalar2=None, op0=ALU.mult),
                                 reads=[TR["sgtmp"]], writes=[TR["sgn"]])
                    proj_fm(win_d, cbase + cg * 256, hT, 16, 1, 256, consume, tok0=tok0, wsrc=wsrc)
            for cg in range(4):
                def consume_i(t, b, cg=cg):
                    tl = t - tok0 // 128
                    o = itok[:, tl, cg * 256:(cg + 1) * 256]
                    P.op("act", lambda e: e.copy(out=o, in_=banks[b][:, 0:256]), reads=[bankR[b]], writes=[TR["itok"]])
                proj_tm(win_d, 4096 + cg * 256, hT, 16, [tok0 // 128, tok0 // 128 + 1], consume_i, wsrc=wsrc)

        def hgrn_f_affine(fs, TR):
            oml3 = omlT.rearrange("p (o h c) -> p o h c", o=1, c=1)
            lb3 = lbT.rearrange("p (o h c) -> p o h c", o=1, c=1)
            P.op("dve", lambda e: e.tensor_tensor(out=fs, in0=fs, in1=bc(oml3, [128, 2, 8, 128]), op=ALU.mult),
                 reads=[TR["fs"]], writes=[TR["fs"]])
            P.op("dve", lambda e: e.tensor_tensor(out=fs, in0=fs, in1=bc(lb3, [128, 2, 8, 128]), op=ALU.add),
                 reads=[TR["fs"]], writes=[TR["fs"]])

        def dump(dst, src, name):
            P.op("sp", lambda e: e.dma_start(out=dst, in_=src), dma=name)
            P.barrier()

        def main_half(hf):
            xbase = hf * 1024
            dbg = DBG and hf == 0
            if dbg:
                dump(dbg_S, S_f.rearrange("p a b -> p (a b)"), "dbgS")
            norm_transpose(lambda t: x_d[xbase + t * 128: xbase + (t + 1) * 128, :], g1T, hT, RA)
            P.barrier()
            if dbg:
                dump(dbg_h, hT.rearrange("p a b -> p (a b)"), "dbgh")
            if STOP <= 1:
                return
            vg = view(RC, [128, 8, 1024], F32)
            vln = view(RA + 32768, [128, 8, 1024], BF16)
            lnG = view(RA + 49152, [128, 1024], F32)
            lnB = view(RA + 53248, [128, 1024], F32)
            tmpv = view(RA + 57344, [128, 1024], F32)
            junkv = view(RA + 61440, [128, 1024], BF16)
            lnR, lnR2 = Res(), Res()
            P.op("sp", lambda e: e.dma_start(out=lnG, in_=lng_d.partition_broadcast(128)), writes=[lnR], dma="lng")
            P.op("sp", lambda e: e.dma_start(out=lnB, in_=lnb_d.partition_broadcast(128)), writes=[lnR2], dma="lnb")
            uR = Res()
            for cg in range(4):
                def cons_u(cb, tb, b, cg=cg):
                    o = yaT[:, cg * 2 + cb, tb * 512:(tb + 1) * 512]
                    P.op("act", lambda e: e.activation(out=o, in_=banks[b][:, 0:512], func=AF.Gelu_apprx_tanh),
                         reads=[bankR[b]], writes=[uR])
                proj_fm(win_d, cg * 256, hT, 16, 2, 512, cons_u)
            vgR = [Res() for _ in range(8)]
            for cg in range(4):
                def cons_v(t, b, cg=cg):
                    o = vg[:, t, cg * 256:(cg + 1) * 256]
                    P.op("act", lambda e: e.activation(out=o, in_=banks[b][:, 0:256], func=AF.Gelu_apprx_tanh),
                         reads=[bankR[b]], writes=[vgR[t]])
                proj_tm(win_d, 1024 + cg * 256, hT, 16, range(8), cons_v)
            tmpR, junkR2, vlnR = Res(), Res(), [Res() for _ in range(8)]
            yaR = Res()
            for t in range(8):
                vt = vg[:, t, :]
                s1, s2, mean, msq, var = (stat[:, 8 + i:9 + i] for i in range(5))
                P.op("dve", lambda e, vt=vt: e.tensor_reduce(out=s1, in_=vt, axis=AX.X, op=ALU.add), reads=[vgR[t]], writes=[statR])
                P.op("act", lambda e, vt=vt: e.activation(out=junkv, in_=vt, func=AF.Square, accum_out=s2),
                     reads=[vgR[t]], writes=[junkR2, statR])
                P.op("dve", lambda e: e.tensor_scalar(out=mean, in0=s1, scalar1=1.0 / 1024, scalar2=None, op0=ALU.mult), reads=[statR], writes=[statR])
                P.op("dve", lambda e: e.tensor_tensor(out=msq, in0=mean, in1=mean, op=ALU.mult), reads=[statR], writes=[statR])
                P.op("dve", lambda e: e.tensor_scalar(out=var, in0=s2, scalar1=1.0 / 1024, scalar2=msq, op0=ALU.mult, op1=ALU.subtract),
                     reads=[statR], writes=[statR])
                P.op("act", lambda e: e.activation(out=var, in_=var, func=AF.Sqrt, bias=EPS, scale=1.0), reads=[statR], writes=[statR])
                P.op("dve", lambda e: e.reciprocal(out=var, in_=var), reads=[statR], writes=[statR])
                P.op("dve", lambda e, vt=vt: e.tensor_scalar(out=tmpv, in0=vt, scalar1=mean, scalar2=var, op0=ALU.subtract, op1=ALU.mult),
                     reads=[vgR[t], statR], writes=[tmpR])
                P.op("dve", lambda e: e.tensor_tensor(out=tmpv, in0=tmpv, in1=lnG, op=ALU.mult), reads=[tmpR, lnR], writes=[tmpR])
                vl = vln[:, t, :]
                P.op("dve", lambda e, vl=vl: e.tensor_tensor(out=vl, in0=tmpv, in1=lnB, op=ALU.add), reads=[tmpR, lnR2], writes=[vlnR[t]])
                for gb in range(2):
                    b = nbank()

                    def sp_mm(e, t=t, gb=gb, b=b):
                        ins = None
                        for gq in range(4):
                            g = gb * 4 + gq
                            o = banks[b][:, gq * 128:(gq + 1) * 128]
                            e.matmul(o, vln[:, t, g * 128:(g + 1) * 128], wsT[:, g, :], start=True, stop=False)
                            e.matmul(o, onesb[0:1, :], bshi[0:1, g * 128:(g + 1) * 128], start=False, stop=False)
                            ins = e.matmul(o, onesb[0:1, :], bslo[0:1, g * 128:(g + 1) * 128], start=False, stop=True)
                        return ins
                    P.op("pe", sp_mm, reads=[vlnR[t]], writes=[bankR[b]])
                    o = yaT[:, gb * 4:(gb + 1) * 4, t * 128:(t + 1) * 128]
                    pv = banks[b][:, 0:512].rearrange("p (a b) -> p a b", a=4)
                    P.op("dve", lambda e, o=o, pv=pv: e.tensor_tensor(out=o, in0=pv, in1=o, op=ALU.mult),
                         reads=[bankR[b], uR], writes=[yaR])
            P.barrier()
            if dbg:
                dump(dbg_ya, yaT.rearrange("p a b -> p (a b)"), "dbgya")
            if STOP <= 2:
                return
            take = hgrn_alloc([(RA + 32768, 32768), (RC, 32768)])
            fs = take([128, 2, 8, 128], F32)
            kT = take([128, 2, 8, 128], BF16)
            qT = take([128, 2, 8, 128], BF16)
            sgn = take([128, 2, 8, 128], BF16)
            itok = take([128, 2, 1024], BF16)
            sgtmp = take([128, 256], F32)
            T = {"a": take([128, 8, 128], F32), "tA": take([128, 8, 128], F32), "tB": take([128, 8, 128], F32),
                 "tC": take([128, 8, 128], F32), "qd": take([128, 8, 128], BF16), "qdt": take([128, 8, 128], BF16),
                 "kinv": take([128, 8, 128], BF16), "kdecT": take([128, 8, 128], BF16), "kdtok": take([128, 1024], BF16),
                 "dS": take([128, 8, 1], F32), "Ef": take([128, 8, 8, 8], F32), "E": take([128, 8, 8, 8], BF16),
                 "kk0": take([128, 8, 8, 16], BF16), "kk1": take([128, 8, 8, 16], BF16),
                 "scm": take([128, 8, 128], BF16), "osq": take([128, 8, 128], BF16)}
            TR = {k: Res(k) for k in list(T.keys()) + ["fs", "k", "q", "sgn", "sgtmp", "itok", "scm0", "scm1", "osq0", "osq1"]}
            ybR = Res()
            for tb4 in range(4):
                tok0 = tb4 * 256
                hgrn_project_f_i(tok0, fs, itok, TR, qT=qT, sgn=sgn, sgtmp=sgtmp)
                hgrn_f_affine(fs, TR)
                for tl in range(2):
                    tokt = tok0 + tl * 128
                    fs_t, kT_t, qT_t, sgn_t, itok_t = fs[:, tl], kT[:, tl], qT[:, tl], sgn[:, tl], itok[:, tl, :]
                    hgrn_tile_common(fs_t, kT_t, T, TR)
                    a, tA, tB, tC = T["a"], T["tA"], T["tB"], T["tC"]
                    a4 = a.rearrange("p h (j r) -> p h j r", r=16)
                    Aj4 = Aj.rearrange("p h (j o) -> p h j o", o=1)
                    P.op("dve", lambda e, a4=a4, Aj4=Aj4: e.tensor_copy(out=Aj4[:, :, 1:8, :], in_=a4[:, :, 0:7, 15:16]),
                         reads=[TR["a"]], writes=[TR["Ef"]])
                    tA4 = tA.rearrange("p h (j r) -> p h j r", r=16)
                    P.op("dve", lambda e, a4=a4, Aj4=Aj4, tA4=tA4: e.tensor_tensor(out=tA4, in0=a4, in1=bc(Aj4, [128, 8, 8, 16]), op=ALU.subtract),
                         reads=[TR["a"], TR["Ef"]], writes=[TR["tA"]])
                    P.op("act", lambda e, tA=tA, tB=tB: e.activation(out=tB, in_=tA, func=AF.Exp), reads=[TR["tA"]], writes=[TR["tB"]])
                    P.op("dve", lambda e, qT_t=qT_t, tB=tB: e.tensor_tensor(out=T["qd"], in0=qT_t, in1=tB, op=ALU.mult),
                         reads=[TR["q"], TR["tB"]], writes=[TR["qd"]])
                    P.op("act", lambda e, tA=tA, tC=tC: e.activation(out=tC, in_=tA, func=AF.Exp, scale=-1.0), reads=[TR["tA"]], writes=[TR["tC"]])
                    P.op("dve", lambda e, kT_t=kT_t, tC=tC: e.tensor_tensor(out=T["kinv"], in0=kT_t, in1=tC, op=ALU.mult),
                         reads=[TR["k"], TR["tC"]], writes=[TR["kinv"]])
                    P.op("act", lambda e, a=a, tB=tB: e.activation(out=tB, in_=a, func=AF.Exp), reads=[TR["a"]], writes=[TR["tB"]])
                    P.op("dve", lambda e, qT_t=qT_t, tB=tB: e.tensor_tensor(out=T["qdt"], in0=qT_t, in1=tB, op=ALU.mult),
                         reads=[TR["q"], TR["tB"]], writes=[TR["qdt"]])
                    Ef, Eb = T["Ef"], T["E"]
                    Ajj = Aj.rearrange("p h (j o) -> p h j o", o=1)
                    Ajc = Aj.rearrange("p h (o c) -> p h o c", o=1)
                    P.op("dve", lambda e, Ef=Ef: e.tensor_tensor(out=Ef, in0=bc(Ajj, [128, 8, 8, 8]), in1=bc(Ajc, [128, 8, 8, 8]), op=ALU.subtract),
                         reads=[TR["Ef"]], writes=[TR["E"]])
                    P.op("dve", lambda e, Ef=Ef: e.tensor_scalar(out=Ef, in0=Ef, scalar1=0.0, scalar2=None, op0=ALU.min),
                         reads=[TR["E"]], writes=[TR["E"]])
                    P.op("act", lambda e, Ef=Ef, Eb=Eb: e.activation(out=Eb, in_=Ef, func=AF.Exp), reads=[TR["E"]], writes=[TR["E"]])
                    obanks = []
                    for hb in range(2):
                        bs_ = nbank()
                        for hq in range(4):
                            h = hb * 4 + hq
                            kk = T["kk%d" % (h % 2)]
                            kkR = TR["kk%d" % (h % 2)]
                            kin = T["kinv"][:, h, :].rearrange("p (o c r) -> p o c r", o=1, r=16)
                            Eh = Eb[:, h].rearrange("p j (c o) -> p j c o", o=1)
                            P.op("dve", lambda e, kk=kk, kin=kin, Eh=Eh: e.tensor_tensor(out=kk, in0=bc(kin, [128, 8, 8, 16]), in1=bc(Eh, [128, 8, 8, 16]), op=ALU.mult),
                                 reads=[TR["kinv"], TR["E"]], writes=[kkR])

                            def sc_mm(e, kk=kk, h=h, hq=hq, bs_=bs_):
                                ins = None
                                for j in range(8):
                                    ins = e.matmul(banks[bs_][:, hq * 128 + j * 16: hq * 128 + (j + 1) * 16],
                                                   kk[:, j].rearrange("p c r -> p (c r)"), T["qd"][:, h, j * 16:(j + 1) * 16],
                                                   start=True, stop=True)
                                return ins
                            P.op("pe", sc_mm, reads=[kkR, TR["qd"]], writes=[bankR[bs_]])
                        scm = T["scm"][:, hb * 4:(hb + 1) * 4, :]
                        scR = TR["scm%d" % hb]
                        UT3 = bc(UT.rearrange("p (o f) -> p o f", o=1), [128, 4, 128])
                        sv = banks[bs_][:, 0:512].rearrange("p (a b) -> p a b", a=4)
                        P.op("dve", lambda e, scm=scm, sv=sv, UT3=UT3: e.tensor_tensor(out=scm, in0=sv, in1=UT3, op=ALU.mult),
                             reads=[bankR[bs_]], writes=[scR])
                        bo = nbank()
                        obanks.append(bo)

                        def o_mm(e, hb=hb, bo=bo, itok_t=itok_t):
                            ins = None
                            for hq in range(4):
                                h = hb * 4 + hq
                                o = banks[bo][:, hq * 128:(hq + 1) * 128]
                                e.matmul(o, itok_t[:, h * 128:(h + 1) * 128], T["scm"][:, h, :], start=True, stop=False)
                                ins = e.matmul(o, S_b[:, h, :], T["qdt"][:, h, :], start=False, stop=True)
                            return ins
                        P.op("pe", o_mm, reads=[scR, TR["itok"], SbR, TR["qdt"]], writes=[bankR[bo]])
                    hgrn_state_update(kT_t, itok_t, T, TR)
                    for hb in range(2):
                        bo = obanks[hb]
                        osq = T["osq"][:, hb * 4:(hb + 1) * 4, :]
                        oR = TR["osq%d" % hb]
                        ov = banks[bo][:, 0:512].rearrange("p (a b) -> p a b", a=4)
                        P.op("act", lambda e, osq=osq, ov=ov: e.activation(out=osq, in_=ov, func=AF.Square), reads=[bankR[bo]], writes=[oR])
                        bq = nbank()
                        mm_group(bq, (0, 512), [onesb], [osq.rearrange("p a b -> p (a b)")], [oR])
                        tq = (tA if hb == 0 else tC)[:, 0:4, :]
                        tqR = TR["tA"] if hb == 0 else TR["tC"]
                        qv = banks[bq][:, 0:512].rearrange("p (a b) -> p a b", a=4)
                        P.op("act", lambda e, tq=tq, qv=qv: e.activation(out=tq, in_=qv, func=AF.Sqrt, bias=EPS, scale=1.0 / 128),
                             reads=[bankR[bq]], writes=[tqR])
                        P.op("dve", lambda e, tq=tq: e.reciprocal(out=tq, in_=tq), reads=[tqR], writes=[tqR])
                        P.op("dve", lambda e, tq=tq, ov=ov: e.tensor_tensor(out=tq, in0=ov, in1=tq, op=ALU.mult),
                             reads=[bankR[bo], tqR], writes=[tqR])
                        yo = ybT[:, hb * 4:(hb + 1) * 4, tokt:tokt + 128]
                        sg4 = sgn_t[:, hb * 4:(hb + 1) * 4, :]
                        P.op("dve", lambda e, yo=yo, tq=tq, sg4=sg4: e.tensor_tensor(out=yo, in0=tq, in1=sg4, op=ALU.mult),
                             reads=[tqR, TR["sgn"]], writes=[ybR])
            P.barrier()
            if dbg:
                dump(dbg_yb, ybT.rearrange("p a b -> p (a b)"), "dbgyb")
            if STOP <= 3:
                return
            sA = [view(RA + 32768 + i * 2048, [128, 512], F32) for i in range(4)]
            sAR = [Res() for _ in range(4)]
            sctr = [0]
            for cg in range(8):
                parts = []
                for (goff, Wp, yT) in ((6144, wpa_d, yaT), (8192, wpb_d, ybT)):
                    cw = [wchunk(win_d, 0, goff + cg * 256), wchunk(win_d, 8, goff + cg * 256)]
                    cp = wchunk(Wp, 0, cg * 256)
                    parts.append((cw, cp, yT))
                for cb in range(2):
                    for tb in range(2):
                        tsl = slice(tb * 512, (tb + 1) * 512)
                        prods = []
                        for (cw, cp, yT) in parts:
                            bg = nbank()
                            mm_group(bg, (0, 512), [cw[kc // 8][0][:, kc % 8, cb * 128:(cb + 1) * 128] for kc in range(16)],
                                     [hT[:, kc, tsl] for kc in range(16)], [cw[0][1], cw[1][1]])
                            si = sctr[0] % 4
                            sctr[0] += 1
                            sg_ = sA[si]
                            P.op("act", lambda e, sg_=sg_, bg=bg: e.activation(out=sg_, in_=banks[bg][:, 0:512], func=AF.Sigmoid),
                                 reads=[bankR[bg]], writes=[sAR[si]])
                            bp = nbank()
                            mm_group(bp, (0, 512), [cp[0][:, kc, cb * 128:(cb + 1) * 128] for kc in range(8)],
                                     [yT[:, kc, tsl] for kc in range(8)], [cp[1]])
                            P.op("dve", lambda e, sg_=sg_, bp=bp: e.tensor_tensor(out=sg_, in0=sg_, in1=banks[bp][:, 0:512], op=ALU.mult),
                                 reads=[bankR[bp], sAR[si]], writes=[sAR[si]])
                            prods.append((sg_, sAR[si]))
                        o = mT[:, cg * 2 + cb, tsl]
                        P.op("dve", lambda e, o=o, p0=prods[0][0], p1=prods[1][0]: e.tensor_tensor(out=o, in0=p0, in1=p1, op=ALU.add),
                             reads=[prods[0][1], prods[1][1]], writes=[])
            P.barrier()
            if dbg:
                dump(dbg_m, mT.rearrange("p a b -> p (a b)"), "dbgm")
            if STOP <= 4:
                return
            yR = [Res() for _ in range(8)]
            for t in range(8):
                yt = yacc[:, t, :]
                src = x_d[xbase + t * 128: xbase + (t + 1) * 128, :]
                P.op("sp", lambda e, yt=yt, src=src: e.dma_start(out=yt, in_=src), writes=[yR[t]], dma="xres%d" % t)
            for cg in range(8):
                def cons_o(t, b, cg=cg):
                    o = yacc[:, t, cg * 256:(cg + 1) * 256]
                    P.op("dve", lambda e: e.tensor_tensor(out=o, in0=o, in1=banks[b][:, 0:256], op=ALU.add),
                         reads=[bankR[b]], writes=[yR[t]])
                proj_tm(wout_d, cg * 256, mT, 16, range(8), cons_o)
            P.barrier()
            if dbg:
                dump(dbg_x2.rearrange("(t p) d -> p t d", p=128), yacc, "dbgx2")
            if STOP <= 5:
                return
            xs32 = view(RC + 4096, [128, D], F32)
            hi = view(RC + 12288, [128, D], BF16)
            lo = view(RC + 16384, [128, D], BF16)
            junk = view(RC + 20480, [128, D], BF16)
            hiT = view(RC + 24576, [128, 16, 128], BF16)
            loT = view(RC + 28672, [128, 16, 128], BF16)
            g2b = view(RB, [128, D], F32)
            h2row = [view(RB + 8192 + i * 4096, [128, D], BF16) for i in range(2)]
            h2R = [Res(), Res()]
            g2bR = Res()
            xsR, hiR, loR, jkR, hiTR, loTR, rsR = (Res() for _ in range(7))
            P.op("sp", lambda e: e.dma_start(out=g2b, in_=g2_d.partition_broadcast(128)), writes=[g2bR], dma="g2b")
            for t in range(8):
                gt = hf * 8 + t
                yt = yacc[:, t, :]
                ssq, rt = stat[:, 16:17], stat[:, 17:18]
                P.op("act", lambda e, yt=yt: e.activation(out=junk, in_=yt, func=AF.Square, accum_out=ssq), reads=[yR[t]], writes=[jkR, statR])
                P.op("act", lambda e: e.activation(out=rt, in_=ssq, func=AF.Sqrt, bias=EPS, scale=1.0 / D), reads=[statR], writes=[statR])
                P.op("dve", lambda e: e.reciprocal(out=rt, in_=rt), reads=[statR], writes=[statR])
                P.op("dve", lambda e, yt=yt: e.tensor_scalar(out=xs32, in0=yt, scalar1=rt, scalar2=None, op0=ALU.mult),
                     reads=[yR[t], statR], writes=[xsR])
                dstx = x2s_d[xbase + t * 128: xbase + (t + 1) * 128, :]
                P.op("sp", lambda e, yt=yt, dstx=dstx: e.dma_start(out=dstx, in_=yt), reads=[yR[t]], writes=[x2sR], dma="x2s")
                P.op("dve", lambda e: e.tensor_copy(out=hi, in_=xs32), reads=[xsR], writes=[hiR])
                P.op("dve", lambda e: e.tensor_tensor(out=xs32, in0=xs32, in1=hi, op=ALU.subtract), reads=[xsR, hiR], writes=[xsR])
                P.op("dve", lambda e: e.tensor_copy(out=lo, in_=xs32), reads=[xsR], writes=[loR])
                s_ = t % 2
                hr = h2row[s_]
                P.op("dve", lambda e, hr=hr: e.tensor_tensor(out=hr, in0=hi, in1=g2b, op=ALU.mult), reads=[hiR, g2bR], writes=[h2R[s_]])
                dsth = h2s_d[xbase + t * 128: xbase + (t + 1) * 128, :]
                P.op("sp", lambda e, hr=hr, dsth=dsth: e.dma_start(out=dsth, in_=hr), reads=[h2R[s_]], writes=[h2sR], dma="h2s")
                for (srcb, srcR, dstT_, dR) in ((hi, hiR, hiT, hiTR), (lo, loR, loT, loTR)):
                    b0, b1 = nbank(), nbank()

                    def tr(e, srcb=srcb, b0=b0, b1=b1):
                        ins = None
                        for j in range(16):
                            bb = b0 if j < 8 else b1
                            ins = e.transpose(banks_bf[bb][:, (j % 8) * 128:(j % 8 + 1) * 128], srcb[:, j * 128:(j + 1) * 128], identb)
                        return ins
                    P.op("pe", tr, reads=[srcR], writes=[bankR[b0], bankR[b1]])
                    for hf_, bb in ((0, b0), (1, b1)):
                        pv = banks_bf[bb][:, 0:1024].rearrange("p (a b) -> p a b", a=8)
                        o = dstT_[:, hf_ * 8:(hf_ + 1) * 8, :]
                        P.op("act", lambda e, o=o, pv=pv: e.copy(out=o, in_=pv), reads=[bankR[bb]], writes=[dR])
                br_ = nbank()
                lhs = [hiT[:, kc, :] for kc in range(16)] + [loT[:, kc, :] for kc in range(16)] + [hiT[:, kc, :] for kc in range(16)]
                rhs = [wrhi[:, kc, :] for kc in range(16)] + [wrhi[:, kc, :] for kc in range(16)] + [wrlo[:, kc, :] for kc in range(16)]
                mm_group(br_, (0, NR), lhs, rhs, [hiTR, loTR])
                lg = rs[:, 0:NR]
                gl = rs[:, 0:NG]
                el = rs[:, NG:NR]
                msk = rs[:, 128:128 + NE]
                ohg = rs[:, 256:256 + NG]
                oh1 = rs[:, 320:320 + NE]
                oh2 = rs[:, 384:384 + NE]
                sm = rs[:, 448:464]
                gmax, ngmax, sume, pg, m1, m2, dd, w1, w2 = (sm[:, i:i + 1] for i in range(9))
                ejunk = rs[:, 464:464 + NG]

                def R(fn, eng="dve"):
                    P.op(eng, fn, reads=[rsR], writes=[rsR])
                P.op("dve", lambda e, br_=br_: e.tensor_tensor(out=lg, in0=banks[br_][:, 0:NR], in1=brt, op=ALU.add), reads=[bankR[br_], rsR], writes=[rsR])
                R(lambda e: e.tensor_reduce(out=gmax, in_=gl, axis=AX.X, op=ALU.max))
                R(lambda e: e.tensor_scalar(out=ohg, in0=gl, scalar1=gmax, scalar2=None, op0=ALU.is_equal))
                R(lambda e: e.tensor_scalar(out=ngmax, in0=gmax, scalar1=-1.0, scalar2=None, op0=ALU.mult))
                R(lambda e: e.activation(out=ejunk, in_=gl, func=AF.Exp, bias=ngmax, scale=1.0, accum_out=sume), "act")
                R(lambda e: e.reciprocal(out=pg, in_=sume))
                R(lambda e: e.tensor_scalar(out=ohg, in0=ohg, scalar1=-1.0, scalar2=1e30, op0=ALU.add, op1=ALU.mult))
                el3 = el.rearrange("p (g c) -> p g c", g=NG)
                msk3 = msk.rearrange("p (g c) -> p g c", g=NG)
                ohg3 = ohg.rearrange("p (g o) -> p g o", o=1)
                R(lambda e: e.tensor_tensor(out=msk3, in0=el3, in1=bc(ohg3, [128, NG, EPG]), op=ALU.add))
                R(lambda e: e.tensor_reduce(out=m1, in_=msk, axis=AX.X, op=ALU.max))
                R(lambda e: e.tensor_scalar(out=oh1, in0=msk, scalar1=m1, scalar2=None, op0=ALU.is_equal))
                R(lambda e: e.scalar_tensor_tensor(out=msk, in0=oh1, scalar=-1e30, in1=msk, op0=ALU.mult, op1=ALU.add))
                R(lambda e: e.tensor_reduce(out=m2, in_=msk, axis=AX.X, op=ALU.max))
                R(lambda e: e.tensor_scalar(out=oh2, in0=msk, scalar1=m2, scalar2=None, op0=ALU.is_equal))
                R(lambda e: e.tensor_tensor(out=dd, in0=m2, in1=m1, op=ALU.subtract))
                R(lambda e: e.activation(out=dd, in_=dd, func=AF.Exp), "act")
                R(lambda e: e.tensor_scalar(out=w1, in0=dd, scalar1=1.0, scalar2=None, op0=ALU.add))
                R(lambda e: e.reciprocal(out=w1, in_=w1))
                R(lambda e: e.tensor_tensor(out=w2, in0=dd, in1=w1, op=ALU.mult))
                gv0, gv1 = GT[:, gt, 0:1], GT[:, gt, 1:2]
                P.op("dve", lambda e, gv0=gv0: e.tensor_tensor(out=gv0, in0=w1, in1=pg, op=ALU.mult), reads=[rsR], writes=[routeR])
                P.op("dve", lambda e, gv1=gv1: e.tensor_tensor(out=gv1, in0=w2, in1=pg, op=ALU.mult), reads=[rsR], writes=[routeR])
                o0, o1 = OH[:, gt, 0, :], OH[:, gt, 1, :]
                P.op("dve", lambda e, o0=o0: e.tensor_copy(out=o0, in_=oh1), reads=[rsR], writes=[routeR])
                P.op("dve", lambda e, o1=o1: e.tensor_copy(out=o1, in_=oh2), reads=[rsR], writes=[routeR])
            P.barrier()

        bregs = {}

        def breg(e, val):
            if val not in bregs:
                bregs[val] = e.to_reg(val)
            return bregs[val]

        zf_ev = [None]

        def zero_fill_xsort():
            z = view(RA, [128, D], BF16)
            zR = Res()
            P.op("dve", lambda e: e.memset(z, 0.0), writes=[zR])
            for blk in range(NE + NOV):
                rows = xsort_d[blk * 128:(blk + 1) * 128, :]
                zf_ev[0] = P.op("sp", lambda e, rows=rows: e.dma_start(out=rows, in_=z), reads=[zR], dma="zf")
            P.barrier()

        def moe_sorted():
            OS = view(RC, [128, NT, NE], BF16)
            big = view(RC + 4096, [128, NA, NE], F32)
            cmpb = view(RC + 4096 + NA * NE * 4, [128, NOV, NE], F32)
            tmpi = view(RC + 4096 + NA * NE * 4 + NOV * NE * 4, [128, NOV, 16], F32)
            mR = Res()

            def M(fn, eng="dve", extra_r=()):
                P.op(eng, fn, reads=[mR, routeR] + list(extra_r), writes=[mR])
            M(lambda e: e.tensor_tensor(out=OS, in0=OH[:, :, 0, :], in1=OH[:, :, 1, :], op=ALU.add))
            M(lambda e: e.memset(ones64, 1.0))
            for t in range(NT):
                b = nbank()
                lhs = [sutb] + [onesb] * t
                rhs = [OS[:, t, :]] + [OS[:, t2, :] for t2 in range(t)]
                mm_group(b, (0, NE), lhs, rhs, [mR])
                for k in range(2):
                    ohk = OH[:, t, k, :]
                    rk = rank[:, 2 * t + k: 2 * t + k + 1]
                    P.op("dve", lambda e, b=b, ohk=ohk: e.tensor_tensor(out=tmp64, in0=banks[b][:, 0:NE], in1=ohk, op=ALU.mult),
                         reads=[bankR[b], mR, routeR], writes=[mR])
                    M(lambda e, rk=rk: e.tensor_reduce(out=rk, in_=tmp64, axis=AX.X, op=ALU.add))
            b = nbank()
            mm_group(b, (0, NE), [onesb] * NT, [OS[:, t, :] for t in range(NT)], [mR])
            P.op("dve", lambda e, b=b: e.tensor_copy(out=cntall, in_=banks[b][:, 0:NE]), reads=[bankR[b], mR], writes=[mR])
            M(lambda e: e.tensor_scalar(out=opad, in0=cntall, scalar1=-128.0, scalar2=0.0, op0=ALU.add, op1=ALU.max))
            cmpa = cmpb.rearrange("p j e -> p (j e)").rearrange("p (e j) -> p e j", j=NOV)
            ov3 = opad.rearrange("p (e o) -> p e o", o=1)
            th3 = cst[:, 1600:1600 + NOV].rearrange("p (o j) -> p o j", o=1)
            M(lambda e: e.tensor_tensor(out=cmpa, in0=bc(ov3, [128, NE, NOV]), in1=bc(th3, [128, NE, NOV]), op=ALU.is_gt))
            M(lambda e: e.tensor_reduce(out=tmp64, in_=cmpa, axis=AX.X, op=ALU.add))
            M(lambda e: e.tensor_scalar(out=opad, in0=tmp64, scalar1=128.0, scalar2=None, op0=ALU.mult))
            M(lambda e: e.tensor_tensor_scan(out=oend, data0=ones64, data1=opad, initial=0.0, op0=ALU.mult, op1=ALU.add))
            M(lambda e: e.tensor_tensor(out=ostart, in0=oend, in1=opad, op=ALU.subtract))
            OHf = OH.rearrange("p t k e -> p (t k) e")
            e128 = cst[:, 1536:1536 + NE].rearrange("p (o e) -> p o e", o=1)
            os3 = ostart.rearrange("p (o e) -> p o e", o=1)
            M(lambda e: e.tensor_tensor(out=big, in0=OHf, in1=bc(e128, [128, NA, NE]), op=ALU.mult))
            M(lambda e: e.tensor_reduce(out=basev, in_=big, axis=AX.X, op=ALU.add))
            M(lambda e: e.tensor_tensor(out=big, in0=OHf, in1=bc(os3, [128, NA, NE]), op=ALU.mult))
            M(lambda e: e.tensor_reduce(out=osv, in_=big, axis=AX.X, op=ALU.add))
            M(lambda e: e.tensor_scalar(out=isov, in0=rank, scalar1=128.0, scalar2=None, op0=ALU.is_ge))
            M(lambda e: e.tensor_tensor(out=osv, in0=osv, in1=basev, op=ALU.subtract))
            M(lambda e: e.tensor_scalar(out=osv, in0=osv, scalar1=float(NE * 128 - 128), scalar2=None, op0=ALU.add))
            M(lambda e: e.tensor_tensor(out=osv, in0=osv, in1=isov, op=ALU.mult))
            M(lambda e: e.tensor_tensor(out=basev, in0=basev, in1=rank, op=ALU.add))
            M(lambda e: e.tensor_tensor(out=basev, in0=basev, in1=osv, op=ALU.add))
            M(lambda e: e.tensor_copy(out=desti, in_=basev))
            oe3 = oend.rearrange("p (o e) -> p o e", o=1)
            ot3 = cst[:, 1600:1600 + NOV].rearrange("p (j o) -> p j o", o=1)
            M(lambda e: e.tensor_tensor(out=cmpb, in0=bc(oe3, [128, NOV, NE]), in1=bc(ot3, [128, NOV, NE]), op=ALU.is_le))
            M(lambda e: e.tensor_reduce(out=obe, in_=cmpb, axis=AX.X, op=ALU.add))
            obe3 = obe.rearrange("p (j o) -> p j o", o=1)
            kcb3 = cst[:, 1632:1648].rearrange("p (o k) -> p o k", o=1)
            M(lambda e: e.tensor_scalar(out=tmpi, in0=bc(obe3, [128, NOV, 16]), scalar1=float(D), scalar2=None, op0=ALU.mult))
            M(lambda e: e.tensor_tensor(out=tmpi, in0=tmpi, in1=bc(kcb3, [128, NOV, 16]), op=ALU.add))
            M(lambda e: e.tensor_copy(out=oig, in_=tmpi))
            tmpd = tmpi[:, :, 0:FFC]
            M(lambda e: e.tensor_scalar(out=tmpd, in0=bc(obe3, [128, NOV, FFC]), scalar1=float(DE), scalar2=None, op0=ALU.mult))
            M(lambda e: e.tensor_tensor(out=tmpd, in0=tmpd, in1=bc(kcb3[:, :, 0:FFC], [128, NOV, FFC]), op=ALU.add))
            M(lambda e: e.tensor_copy(out=oid, in_=tmpd))
            hrow = [view(RA + i * 4096, [128, D], BF16) for i in range(2)]
            hrowR = [Res(), Res()]
            for t in range(NT):
                s_ = t % 2
                hr = hrow[s_]
                src = h2s_d[t * 128:(t + 1) * 128, :]
                P.op("sp", lambda e, hr=hr, src=src: e.dma_start(out=hr, in_=src), reads=[h2sR], writes=[hrowR[s_]], dma="hrow%d" % s_)
                for k in range(2):
                    ix = desti[:, 2 * t + k: 2 * t + k + 1]
                    P.op("pool", lambda e, hr=hr, ix=ix: e.indirect_dma_start(
                        out=xsort_d[:, :], out_offset=bass.IndirectOffsetOnAxis(ap=ix, axis=0), in_=hr, in_offset=None),
                        reads=[hrowR[s_], mR], writes=[xsortR], dma="sc", extra=[zf_ev[0]])
            P.barrier()
            xb = [view(RA + 8192 + i * 4096, [128, D], BF16) for i in range(2)]
            xbT = [view(RA + 16384 + i * 4096, [128, 16, 128], BF16) for i in range(2)]
            sil = view(RA + 24576, [128, 1024], F32)
            actT = [view(RA + 28672 + i * 2048, [128, 8, 128], BF16) for i in range(2)]
            ybs = [view(RA + 32768 + i * 8192, [128, D], F32) for i in range(2)]
            wf_gu = view(RB, [128, 16, DE], BF16)
            wf_d = view(RC, [128, FFC, D], BF16)
            xbR, xbTR, actR = [Res(), Res()], [Res(), Res()], [Res(), Res()]
            silR = Res()
            ybsR = [[Res() for _ in range(8)] for _ in range(2)]
            wfR = [Res() for _ in range(16)]
            wdR = [Res() for _ in range(FFC)]
            NFFB = 2 * NFB
            for blk in range(NE + NOV):
                s_ = blk % 2
                static = blk < NE
                ex = blk
                j = blk - NE
                rows = xsort_d[blk * 128:(blk + 1) * 128, :]
                xbs = xb[s_]
                P.op("sp", lambda e, xbs=xbs, rows=rows: e.dma_start(out=xbs, in_=rows), reads=[xsortR], writes=[xbR[s_]], dma="xb%d" % s_)
                b0, b1 = nbank(), nbank()

                def tr(e, xbs=xbs, b0=b0, b1=b1):
                    ins = None
                    for q in range(16):
                        bb = b0 if q < 8 else b1
                        ins = e.transpose(banks_bf[bb][:, (q % 8) * 128:(q % 8 + 1) * 128], xbs[:, q * 128:(q + 1) * 128], identb)
                    return ins
                P.op("pe", tr, reads=[xbR[s_]], writes=[bankR[b0], bankR[b1]])
                for hf_, bb in ((0, b0), (1, b1)):
                    pv = banks_bf[bb][:, 0:1024].rearrange("p (a b) -> p a b", a=8)
                    o = xbT[s_][:, hf_ * 8:(hf_ + 1) * 8, :]
                    if hf_ == 0:
                        P.op("act", lambda e, o=o, pv=pv: e.copy(out=o, in_=pv), reads=[bankR[bb]], writes=[xbTR[s_]])
                    else:
                        P.op("dve", lambda e, o=o, pv=pv: e.tensor_copy(out=o, in_=pv), reads=[bankR[bb]], writes=[xbTR[s_]])
                Ab = [nbank(), nbank()]
                Ub = [nbank(), nbank()]
                rhs = [xbT[s_][:, kc, :] for kc in range(16)]

                def cols(ffb):
                    return ((ffb % 4) * 128, (ffb % 4) * 128 + 128)
                if static:
                    for fb in range(NFB):
                        cg = [wchunk(weg_d, ex * 16 + 0, fb * 256), wchunk(weg_d, ex * 16 + 8, fb * 256)]
                        cu = [wchunk(weu_d, ex * 16 + 0, fb * 256), wchunk(weu_d, ex * 16 + 8, fb * 256)]
                        for cb in range(2):
                            ffb = fb * 2 + cb
                            mm_group(Ab[ffb // 4], cols(ffb), [cg[kc // 8][0][:, kc % 8, cb * 128:(cb + 1) * 128] for kc in range(16)],
                                     rhs, [cg[0][1], cg[1][1], xbTR[s_]])
                            mm_group(Ub[ffb // 4], cols(ffb), [cu[kc // 8][0][:, kc % 8, cb * 128:(cb + 1) * 128] for kc in range(16)],
                                     rhs, [cu[0][1], cu[1][1], xbTR[s_]])
                else:
                    for (Wd_, bks) in ((weg_d, Ab), (weu_d, Ub)):
                        for kc in range(16):
                            ix = oig[:, j, kc:kc + 1]
                            o = wf_gu[:, kc, :]
                            P.op("pool", lambda e, o=o, ix=ix, Wd_=Wd_: e.indirect_dma_start(
                                out=o, out_offset=None, in_=Wd_[:, :], in_offset=bass.IndirectOffsetOnAxis(ap=ix, axis=0),
                                bounds_check=breg(e, NE * D - 1), oob_is_err=False),
                                reads=[mR], writes=[wfR[kc]], dma="ow%d" % kc)
                        for ffb in range(NFFB):
                            mm_group(bks[ffb // 4], cols(ffb), [wf_gu[:, kc, ffb * 128:(ffb + 1) * 128] for kc in range(16)],
                                     rhs, wfR + [xbTR[s_]])
                for q in range(2):
                    nf = min(4, NFFB - 4 * q)
                    if nf <= 0:
                        continue
                    sl = sil[:, q * 512:q * 512 + nf * 128]
                    P.op("act", lambda e, sl=sl, q=q, nf=nf, Ab=Ab: e.activation(out=sl, in_=banks[Ab[q]][:, 0:nf * 128], func=AF.Silu),
                         reads=[bankR[Ab[q]]], writes=[silR])
                    o = actT[s_][:, 4 * q:4 * q + nf, :].rearrange("p a b -> p (a b)")
                    P.op("dve", lambda e, o=o, sl=sl, q=q, nf=nf, Ub=Ub: e.tensor_tensor(out=o, in0=sl, in1=banks[Ub[q]][:, 0:nf * 128], op=ALU.mult),
                         reads=[bankR[Ub[q]], silR], writes=[actR[s_]])
                if not static:
                    for ffc in range(FFC):
                        ix = oid[:, j, ffc:ffc + 1]
                        o = wf_d[:, ffc, :]
                        P.op("pool", lambda e, o=o, ix=ix: e.indirect_dma_start(
                            out=o, out_offset=None, in_=wed_d[:, :], in_offset=bass.IndirectOffsetOnAxis(ap=ix, axis=0),
                            bounds_check=breg(e, NE * DE - 1), oob_is_err=False),
                            reads=[mR], writes=[wdR[ffc]], dma="od%d" % ffc)
                for cg8 in range(8):
                    b = nbank()
                    lhs = [actT[s_][:, ffc, :] for ffc in range(FFC)]
                    if static:
                        cd = wchunk(wed_d, ex * FFC, cg8 * 256, nk=FFC)
                        mm_group(b, (0, 256), lhs, [cd[0][:, ffc, 0:256] for ffc in range(FFC)], [cd[1], actR[s_]])
                    else:
                        mm_group(b, (0, 256), lhs, [wf_d[:, ffc, cg8 * 256:(cg8 + 1) * 256] for ffc in range(FFC)], wdR + [actR[s_]])
                    o = ybs[s_][:, cg8 * 256:(cg8 + 1) * 256]
                    if cg8 % 2 == 0:
                        P.op("act", lambda e, o=o, b=b: e.copy(out=o, in_=banks[b][:, 0:256]), reads=[bankR[b]], writes=[ybsR[s_][cg8]])
                    else:
                        P.op("dve", lambda e, o=o, b=b: e.tensor_copy(out=o, in_=banks[b][:, 0:256]), reads=[bankR[b]], writes=[ybsR[s_][cg8]])
                yrows = ysort_d[blk * 128:(blk + 1) * 128, :]
                yb_ = ybs[s_]
                P.op("sp", lambda e, yb_=yb_, yrows=yrows: e.dma_start(out=yrows, in_=yb_), reads=ybsR[s_], writes=[ysortR], dma="ys")
            P.barrier()
            gfin = view(RC, [128, D], F32)
            y0 = [view(RA + i * 8192, [128, D], F32) for i in range(2)]
            y1 = [view(RA + 16384 + i * 8192, [128, D], F32) for i in range(2)]
            xt2 = [view(RA + 32768 + i * 8192, [128, D], F32) for i in range(2)]
            ot = [view(RA + 49152 + i * 8192, [128, D], F32) for i in range(2)]
            junk7 = view(RC + 8192, [128, D], BF16)
            y0R, y1R, xtR, otR = ([Res(), Res()] for _ in range(4))
            gfR, j7R = Res(), Res()
            P.op("sp", lambda e: e.dma_start(out=gfin, in_=gf_d.partition_broadcast(128)), writes=[gfR], dma="gfin")
            for t in range(NT):
                s_ = t % 2
                for k, (yb, yR_) in enumerate(((y0[s_], y0R[s_]), (y1[s_], y1R[s_]))):
                    ix = desti[:, 2 * t + k: 2 * t + k + 1]
                    P.op("pool", lambda e, yb=yb, ix=ix: e.indirect_dma_start(
                        out=yb, out_offset=None, in_=ysort_d[:, :], in_offset=bass.IndirectOffsetOnAxis(ap=ix, axis=0)),
                        reads=[ysortR, mR], writes=[yR_], dma="yg%d%d" % (k, s_))
                xx = xt2[s_]
                src = x2s_d[t * 128:(t + 1) * 128, :]
                P.op("sp", lambda e, xx=xx, src=src: e.dma_start(out=xx, in_=src), reads=[x2sR], writes=[xtR[s_]], dma="xt%d" % s_)
                ya, yb2 = y0[s_], y1[s_]
                gv0, gv1 = GT[:, t, 0:1], GT[:, t, 1:2]
                P.op("dve", lambda e, xx=xx, ya=ya, gv0=gv0: e.scalar_tensor_tensor(out=xx, in0=ya, scalar=gv0, in1=xx, op0=ALU.mult, op1=ALU.add),
                     reads=[y0R[s_], routeR], writes=[xtR[s_]])
                P.op("dve", lambda e, xx=xx, yb2=yb2, gv1=gv1: e.scalar_tensor_tensor(out=xx, in0=yb2, scalar=gv1, in1=xx, op0=ALU.mult, op1=ALU.add),
                     reads=[y1R[s_], routeR], writes=[xtR[s_]])
                ssq, rt = stat[:, 20 + 2 * s_:21 + 2 * s_], stat[:, 21 + 2 * s_:22 + 2 * s_]
                P.op("act", lambda e, xx=xx, ssq=ssq: e.activation(out=junk7, in_=xx, func=AF.Square, accum_out=ssq), reads=[xtR[s_]], writes=[j7R, statR])
                P.op("act", lambda e, ssq=ssq, rt=rt: e.activation(out=rt, in_=ssq, func=AF.Sqrt, bias=EPS, scale=1.0 / D), reads=[statR], writes=[statR])
                P.op("dve", lambda e, rt=rt: e.reciprocal(out=rt, in_=rt), reads=[statR], writes=[statR])
                o = ot[s_]
                P.op("dve", lambda e, o=o, xx=xx, rt=rt: e.scalar_tensor_tensor(out=o, in0=xx, scalar=rt, in1=gfin, op0=ALU.mult, op1=ALU.mult),
                     reads=[xtR[s_], statR, gfR], writes=[otR[s_]])
                dst = out_d[t * 128:(t + 1) * 128, :]
                P.op("sp", lambda e, o=o, dst=dst: e.dma_start(out=dst, in_=o), reads=[otR[s_]], dma="out%d" % s_)
            P.barrier()

        setup()
        zero_fill_xsort()
        if NPH > 0 and STOP > 0:
            load_wcache()
        for ph in range(NPH if STOP > 0 else 0):
            prologue_half(ph)
        for hf in range(NH if STOP > 0 else 0):
            main_half(hf)
        if STOP > 6:
            moe_sorted()
        P.barrier()
        P.op("sp", None)
        P.emit(nc, es)
    return nc


CFG = dict(tpc=2048, nph=14, ng=8, epg=8, de=1024)
N_CORES = 8


def make_consts():
    c = np.zeros((128, 1648), np.float32)
    p = np.arange(128)[:, None]
    f = np.arange(128)[None, :]
    c[:, 0:128] = (p == f)
    c[:, 128:256] = (f <= p)
    c[:, 256:384] = (f >= p)
    rm = np.ones((128, 1024), np.float32)
    rm[:, ::128] = 0.0
    c[:, 384:1408] = rm
    c[:, 1408:1536] = (f > p)
    c[:, 1536:1600] = 128.0 * np.arange(64)[None, :]
    c[:, 1600:1632] = 128.0 * np.arange(32)[None, :]
    c[:, 1632:1648] = 128.0 * np.arange(16)[None, :] + np.arange(128)[:, None]
    return c


def make_in_maps(cfg, n_cores, x, norm_mix_g, w_in, gmlp_ln_g, gmlp_ln_b, w_spatial, b_spatial, hgrn_lb_logits,
                 hgrn_norm_g, w_branch_a, w_branch_b, w_out, norm_ffn_g, w_router_group, b_router_group,
                 w_router_expert, b_router_expert, w_expert_gate, w_expert_up, w_expert_down, norm_final_g):
    f = lambda a: np.ascontiguousarray(np.asarray(a, dtype=np.float32))
    TPC = cfg["tpc"]
    NPH = cfg["nph"]
    NE = cfg["ng"] * cfg["epg"]
    DE = cfg["de"]
    xf = f(x).reshape(-1, D)
    shared = dict(
        norm_mix_g=f(norm_mix_g).reshape(D), w_in=f(w_in).reshape(D, 10240),
        gmlp_ln_g=f(gmlp_ln_g).reshape(1024), gmlp_ln_b=f(gmlp_ln_b).reshape(1024),
        w_spatial=f(w_spatial).reshape(8, 128, 128), b_spatial=f(b_spatial).reshape(1, 1024),
        hgrn_lb=f(hgrn_lb_logits).reshape(2, 1024), hgrn_norm_g=f(hgrn_norm_g).reshape(1024),
        w_branch_a=f(w_branch_a).reshape(1024, D), w_branch_b=f(w_branch_b).reshape(1024, D),
        w_out=f(w_out).reshape(D, D), norm_ffn_g=f(norm_ffn_g).reshape(D),
        w_router=np.ascontiguousarray(np.concatenate([f(w_router_group).reshape(D, -1), f(w_router_expert).reshape(D, -1)], axis=1)),
        b_router=np.ascontiguousarray(np.concatenate([f(b_router_group).reshape(-1), f(b_router_expert).reshape(-1)])),
        w_eg=f(w_expert_gate).reshape(NE * D, DE), w_eu=f(w_expert_up).reshape(NE * D, DE),
        w_ed=f(w_expert_down).reshape(NE * DE, D), norm_final_g=f(norm_final_g).reshape(D),
        consts=make_consts(),
    )
    maps = []
    npt = max(NPH, 1) * 1024
    for c in range(n_cores):
        m = dict(shared)
        m["x"] = xf[c * TPC:(c + 1) * TPC]
        xp = np.zeros((npt, D), np.float32)
        prev = xf[0:c * TPC]
        if NPH > 0 and prev.shape[0] > 0:
            xp[npt - prev.shape[0]:] = prev
        m["xprev"] = xp
        maps.append(m)
    return maps


def kernel(**inputs):
    nc = build(CFG)
    maps = make_in_maps(CFG, N_CORES, **inputs)
    res = run_bass_kernel_spmd(nc, maps, core_ids=list(range(N_CORES)))
    out = np.concatenate([r["out"] for r in res.results], axis=0)
    return out.reshape(1, N_CORES * CFG["tpc"], D).astype(np.float32)
```
